# Optimizing a Trainium2 kernel written in Bass

```python
import jax, jax.numpy as jnp
from jax import lax
import numpy as np

D_MODEL = 2048
BATCH = 1
SEQ = 16384
DEPTH = 4

HEAD_DIM = 128
N_HEADS_DSA = 8
N_HEADS_MOBA = 8
KV_LATENT = 256
IDX_HEADS = 16
IDX_DIM = 64
DSA_TOPK_MAX = 256
MOBA_BLOCK = 256
MOBA_TOPK_MAX = 3
N_MEM = 256
N_HEADS_MEM = 4
D_FF = -(-8 * D_MODEL // (3 * 256)) * 256
ATTN_Q_BLOCK = 128
MOBA_Q_BLOCK = 64
DSA_WIDTH = N_HEADS_DSA * HEAD_DIM
MOBA_WIDTH = N_HEADS_MOBA * HEAD_DIM
MEM_WIDTH = N_HEADS_MEM * HEAD_DIM
SPLIT_SIZES = (DSA_WIDTH, KV_LATENT, IDX_HEADS * IDX_DIM, IDX_DIM, IDX_HEADS,
               MOBA_WIDTH, MOBA_WIDTH, MOBA_WIDTH, D_MODEL, D_MODEL)
IN_WIDTH = sum(SPLIT_SIZES)
ALPHA = (2.0 * DEPTH) ** 0.25
BETA = (8.0 * DEPTH) ** -0.25
LN_EPS = 1e-5

kernel_name = 'hybrid_dsa_moba_gated_deepnorm'


def layer_norm(x, g, b):
    xf = x.astype(jnp.float32)
    mu = xf.mean(-1, keepdims=True)
    var = jnp.square(xf - mu).mean(-1, keepdims=True)
    return ((xf - mu) * lax.rsqrt(var + LN_EPS) * g.astype(jnp.float32) + b.astype(jnp.float32)).astype(x.dtype)


def rms_norm(x, g):
    xf = x.astype(jnp.float32)
    return (xf * lax.rsqrt(jnp.mean(xf * xf, -1, keepdims=True) + LN_EPS) * g.astype(jnp.float32)).astype(x.dtype)


def alibi_slopes(n):
    return jnp.asarray([2.0 ** (-8.0 * (i + 1) / n) for i in range(n)], dtype=jnp.float32)


def dsa_attention(q, c_kv, q_idx, k_idx, w_idx, w_uk, w_uv):
    S = q.shape[0]
    topk = min(DSA_TOPK_MAX, S // 4)
    slopes = alibi_slopes(N_HEADS_DSA)
    q_lat = jnp.einsum('shd,hdc->shc', q, w_uk)
    key_pos = jnp.arange(S)
    idx_scale = IDX_DIM ** -0.5
    w_scale = IDX_HEADS ** -0.5

    def block(i):
        t0 = i * ATTN_Q_BLOCK
        qi = lax.dynamic_slice_in_dim(q_idx, t0, ATTN_Q_BLOCK, 0)
        wi = lax.dynamic_slice_in_dim(w_idx, t0, ATTN_Q_BLOCK, 0)
        ql = lax.dynamic_slice_in_dim(q_lat, t0, ATTN_Q_BLOCK, 0)
        qpos = t0 + jnp.arange(ATTN_Q_BLOCK)
        logits = jnp.einsum('thd,sd->ths', qi, k_idx, preferred_element_type=jnp.float32) * idx_scale
        score = jnp.einsum('th,ths->ts', wi.astype(jnp.float32) * w_scale, jax.nn.relu(logits))
        score = jnp.where(key_pos[None, :] <= qpos[:, None], score, -jnp.inf)
        _, sel = lax.top_k(score, topk)
        valid = sel <= qpos[:, None]
        c_sel = c_kv[sel]
        s = jnp.einsum('thc,tkc->thk', ql, c_sel, preferred_element_type=jnp.float32) * (HEAD_DIM ** -0.5)
        dist = (qpos[:, None] - sel).astype(jnp.float32)
        s = s - slopes[None, :, None] * dist[:, None, :]
        s = jnp.where(valid[:, None, :], s, -jnp.inf)
        p = jax.nn.softmax(s, axis=-1).astype(c_kv.dtype)
        return jnp.einsum('thk,tkc->thc', p, c_sel)

    o_lat = lax.map(block, jnp.arange(S // ATTN_Q_BLOCK)).reshape(S, N_HEADS_DSA, KV_LATENT)
    o = jnp.einsum('shc,hcd->shd', o_lat, w_uv)
    return o.reshape(S, DSA_WIDTH)


def moba_attention(q, k, v):
    S, H, Dh = q.shape
    n_blk = -(-S // MOBA_BLOCK)
    pad = n_blk * MOBA_BLOCK - S
    kb = jnp.pad(k, ((0, pad), (0, 0), (0, 0))).reshape(n_blk, MOBA_BLOCK, H, Dh).transpose(2, 0, 1, 3)
    vb = jnp.pad(v, ((0, pad), (0, 0), (0, 0))).reshape(n_blk, MOBA_BLOCK, H, Dh).transpose(2, 0, 1, 3)
    k_mean = kb.astype(jnp.float32).mean(axis=2)
    n_sel = min(MOBA_TOPK_MAX, n_blk - 1)
    n_past = n_sel * MOBA_BLOCK
    slopes = alibi_slopes(H)
    scale = Dh ** -0.5
    offs = jnp.arange(MOBA_BLOCK)
    blk_ids = jnp.arange(n_blk)
    head_ix = jnp.arange(H)[None, :, None]

    def block(i):
        t0 = i * MOBA_Q_BLOCK
        qi = lax.dynamic_slice_in_dim(q, t0, MOBA_Q_BLOCK, 0)
        qpos = t0 + jnp.arange(MOBA_Q_BLOCK)
        own = t0 // MOBA_BLOCK
        k_own = lax.dynamic_index_in_dim(kb, own, axis=1, keepdims=False)
        v_own = lax.dynamic_index_in_dim(vb, own, axis=1, keepdims=False)
        own_pos = own * MOBA_BLOCK + offs
        s_own = jnp.einsum('thd,hkd->thk', qi, k_own, preferred_element_type=jnp.float32) * scale
        s_own = s_own - slopes[None, :, None] * (qpos[:, None] - own_pos[None, :]).astype(jnp.float32)[:, None, :]
        s_own = jnp.where((own_pos[None, :] <= qpos[:, None])[:, None, :], s_own, -jnp.inf)
        if n_sel == 0:
            p = jax.nn.softmax(s_own, axis=-1).astype(v.dtype)
            return jnp.einsum('thk,hkd->thd', p, v_own)
        gate = jnp.einsum('thd,hnd->thn', qi.astype(jnp.float32), k_mean)
        gate = jnp.where(blk_ids[None, None, :] < own, gate, -jnp.inf)
        _, sel = lax.top_k(gate, n_sel)
        k_sel = kb[head_ix, sel]
        v_sel = vb[head_ix, sel].reshape(MOBA_Q_BLOCK, H, n_past, Dh)
        pos_sel = sel[..., None] * MOBA_BLOCK + offs
        s_sel = jnp.einsum('thd,thnkd->thnk', qi, k_sel, preferred_element_type=jnp.float32) * scale
        s_sel = s_sel - slopes[None, :, None, None] * (qpos[:, None, None, None] - pos_sel).astype(jnp.float32)
        s_sel = jnp.where((sel < own)[..., None], s_sel, -jnp.inf).reshape(MOBA_Q_BLOCK, H, n_past)
        s = jnp.concatenate([s_sel, s_own], axis=-1)
        p = jax.nn.softmax(s, axis=-1).astype(v.dtype)
        return (jnp.einsum('thk,thkd->thd', p[..., :n_past], v_sel)
                + jnp.einsum('thk,hkd->thd', p[..., n_past:], v_own))

    o = lax.map(block, jnp.arange(S // MOBA_Q_BLOCK))
    return o.reshape(S, H * Dh)


def hybrid_mixer(x, w_in, b_gate, kv_norm_g, idx_k_norm_g, idx_k_norm_b, w_uk, w_uv, w_o_dsa, w_o_moba, w_out):
    B, S, _ = x.shape
    points = np.cumsum(SPLIT_SIZES)[:-1].tolist()
    proj = x @ w_in
    q_dsa, c_kv, q_idx, k_idx, w_idx, q_m, k_m, v_m, ga, gb = jnp.split(proj, points, axis=-1)
    c_kv = rms_norm(c_kv, kv_norm_g)
    k_idx = layer_norm(k_idx, idx_k_norm_g, idx_k_norm_b)
    q_dsa = q_dsa.reshape(B, S, N_HEADS_DSA, HEAD_DIM)
    q_idx = q_idx.reshape(B, S, IDX_HEADS, IDX_DIM)
    o_a = jax.vmap(dsa_attention, in_axes=(0, 0, 0, 0, 0, None, None))(q_dsa, c_kv, q_idx, k_idx, w_idx, w_uk, w_uv)
    hm = (B, S, N_HEADS_MOBA, HEAD_DIM)
    o_b = jax.vmap(moba_attention)(q_m.reshape(hm), k_m.reshape(hm), v_m.reshape(hm))
    g_a = jax.nn.sigmoid(ga + b_gate[0])
    g_b = jax.nn.sigmoid(gb + b_gate[1])
    merged = g_a * (o_a @ w_o_dsa) + g_b * (o_b @ w_o_moba)
    return merged @ w_out


def mem_cross_attention(x, mem, w_q, w_kv, w_o):
    B, S, _ = x.shape
    q = (x @ w_q).reshape(B, S, N_HEADS_MEM, HEAD_DIM)
    k, v = jnp.split(mem @ w_kv, 2, axis=-1)
    k = k.reshape(B, N_MEM, N_HEADS_MEM, HEAD_DIM)
    v = v.reshape(B, N_MEM, N_HEADS_MEM, HEAD_DIM)
    s = jnp.einsum('bshd,bmhd->bhsm', q, k, preferred_element_type=jnp.float32) * (HEAD_DIM ** -0.5)
    p = jax.nn.softmax(s, axis=-1).astype(x.dtype)
    o = jnp.einsum('bhsm,bmhd->bshd', p, v).reshape(B, S, MEM_WIDTH)
    return o @ w_o


def swiglu(x, w_in, w_out):
    g, u = jnp.split(x @ w_in, 2, axis=-1)
    return (jax.nn.silu(g) * u) @ w_out


def setup_inputs(seed: int = 0) -> dict:
    key = jax.random.key(seed)
    ks = jax.random.split(key, 20)
    f32 = jnp.float32

    def nrm(k, shape, scale):
        return jax.random.normal(k, shape, f32) * scale

    v_off = sum(SPLIT_SIZES[:7])
    col_scale = jnp.ones((IN_WIDTH,), f32).at[v_off:v_off + MOBA_WIDTH].set(BETA)
    kv_scale = jnp.ones((2 * MEM_WIDTH,), f32).at[MEM_WIDTH:].set(BETA)
    return {
        'x': nrm(ks[0], (BATCH, SEQ, D_MODEL), 1.0),
        'mem': nrm(ks[1], (BATCH, N_MEM, D_MODEL), 1.0),
        'w_in': nrm(ks[2], (DEPTH, D_MODEL, IN_WIDTH), D_MODEL ** -0.5) * col_scale,
        'b_gate': nrm(ks[3], (DEPTH, 2, D_MODEL), 0.02),
        'kv_norm_g': 1.0 + nrm(ks[4], (DEPTH, KV_LATENT), 0.02),
        'idx_k_norm_g': 1.0 + nrm(ks[5], (DEPTH, IDX_DIM), 0.02),
        'idx_k_norm_b': nrm(ks[6], (DEPTH, IDX_DIM), 0.02),
        'w_uk': nrm(ks[7], (DEPTH, N_HEADS_DSA, HEAD_DIM, KV_LATENT), HEAD_DIM ** -0.5),
        'w_uv': nrm(ks[8], (DEPTH, N_HEADS_DSA, KV_LATENT, HEAD_DIM), BETA * KV_LATENT ** -0.5),
        'w_o_dsa': nrm(ks[9], (DEPTH, DSA_WIDTH, D_MODEL), BETA * DSA_WIDTH ** -0.5),
        'w_o_moba': nrm(ks[10], (DEPTH, MOBA_WIDTH, D_MODEL), BETA * MOBA_WIDTH ** -0.5),
        'w_out': nrm(ks[11], (DEPTH, D_MODEL, D_MODEL), BETA * D_MODEL ** -0.5),
        'w_q_mem': nrm(ks[12], (DEPTH, D_MODEL, MEM_WIDTH), D_MODEL ** -0.5),
        'w_kv_mem': nrm(ks[13], (DEPTH, D_MODEL, 2 * MEM_WIDTH), D_MODEL ** -0.5) * kv_scale,
        'w_o_mem': nrm(ks[14], (DEPTH, MEM_WIDTH, D_MODEL), BETA * MEM_WIDTH ** -0.5),
        'w_ffn_in': nrm(ks[15], (DEPTH, D_MODEL, 2 * D_FF), BETA * D_MODEL ** -0.5),
        'w_ffn_out': nrm(ks[16], (DEPTH, D_FF, D_MODEL), BETA * D_FF ** -0.5),
        'ln_g': 1.0 + nrm(ks[17], (DEPTH, 3, D_MODEL), 0.02),
        'ln_b': nrm(ks[18], (DEPTH, 3, D_MODEL), 0.02),
    }


def reference(x, mem, w_in, b_gate, kv_norm_g, idx_k_norm_g, idx_k_norm_b, w_uk, w_uv, w_o_dsa, w_o_moba,
              w_out, w_q_mem, w_kv_mem, w_o_mem, w_ffn_in, w_ffn_out, ln_g, ln_b):
    for l in range(DEPTH):
        y = hybrid_mixer(x, w_in[l], b_gate[l], kv_norm_g[l], idx_k_norm_g[l], idx_k_norm_b[l], w_uk[l], w_uv[l],
                         w_o_dsa[l], w_o_moba[l], w_out[l])
        x = layer_norm(ALPHA * x + y, ln_g[l, 0], ln_b[l, 0])
        y = mem_cross_attention(x, mem, w_q_mem[l], w_kv_mem[l], w_o_mem[l])
        x = layer_norm(ALPHA * x + y, ln_g[l, 1], ln_b[l, 1])
        y = swiglu(x, w_ffn_in[l], w_ffn_out[l])
        x = layer_norm(ALPHA * x + y, ln_g[l, 2], ln_b[l, 2])
    return x
```

```python
import contextlib
import numpy as np
import concourse.bass as bass
import concourse.mybir as mybir

F32 = mybir.dt.float32
BF16 = mybir.dt.bfloat16
AF = mybir.ActivationFunctionType
ALU = mybir.AluOpType
AX = mybir.AxisListType


class Eng:
    def __init__(self, name, h, sem, is_pe=False):
        self.name = name
        self.h = h
        self.sem = sem
        self.count = 0
        self.seen = {}
        self.is_pe = is_pe


class Res:
    __slots__ = ("name", "last_w", "readers", "sem", "semval")

    def __init__(self, name=""):
        self.name = name
        self.last_w = None
        self.readers = {}
        self.sem = None
        self.semval = 0


class Fw:
    def __init__(self, nc, es):
        self.nc = nc
        self.es = es
        self.engs = {}
        for name, h, pe in [("pe", nc.tensor, True), ("act", nc.scalar, False),
                            ("dve", nc.vector, False), ("pool", nc.gpsimd, False),
                            ("sp", nc.sync, False)]:
            sem = es.enter_context(nc.semaphore("sem_" + name))
            self.engs[name] = Eng(name, h, sem, pe)
        self.nsem = 5
        self.dpool = []
        self.dnext = 0
        self.phase_slots = []
        self.pes = None

    def begin_phase(self):
        self.pes = contextlib.ExitStack()
        self.pes.__enter__()
        self.dnext = 0
        self.phase_slots = []

    def end_phase(self):
        self.barrier()
        self.pes.__exit__(None, None, None)
        self.pes = None

    def barrier(self):
        sp = self.engs["sp"]
        for r in self.phase_slots:
            if r.last_w is not None and r.last_w[0] == "d":
                self._wait(sp, r.last_w)
            for t in r.readers.values():
                if t[0] == "d":
                    self._wait(sp, t)
        for e2 in self.engs.values():
            if e2 is not sp and e2.count > 0:
                self._wait(sp, ("e", e2, e2.count))
        inst = sp.h.nop()
        sp.count += 1
        inst.then_inc(sp.sem, 1)
        for e2 in self.engs.values():
            if e2 is not sp:
                self._wait(e2, ("e", sp, sp.count))

    def sbuf(self, name, shape, dtype):
        self.uid = getattr(self, "uid", 0) + 1
        t = self.pes.enter_context(self.nc.sbuf_tensor("%s_u%d" % (name, self.uid), list(shape), dtype))
        return t

    def psum(self, name, shape, dtype):
        self.uid = getattr(self, "uid", 0) + 1
        return self.pes.enter_context(self.nc.psum_tensor("%s_u%d" % (name, self.uid), list(shape), dtype))

    def dram(self, name, shape, dtype, kind="Internal"):
        return self.nc.dram_tensor(name, list(shape), dtype, kind=kind).ap()

    def _dma_sem(self, r):
        if r.sem is None:
            if self.dnext >= len(self.dpool):
                h = self.es.enter_context(self.nc.semaphore("dsem_%d" % self.nsem))
                self.nsem += 1
                self.dpool.append([h, 0])
            r.sem = self.dpool[self.dnext]
            self.dnext += 1
            self.phase_slots.append(r)
        return r.sem

    def _wait(self, eng, tok):
        kind = tok[0]
        if kind == "e":
            _, e2, n = tok
            if e2 is eng and eng.is_pe:
                return
            if eng.seen.get(e2.name, 0) >= n:
                return
            eng.h.wait_ge(e2.sem, n)
            eng.seen[e2.name] = n
        else:
            _, r, v = tok
            key = ("d", id(r.sem))
            if eng.seen.get(key, 0) >= v:
                return
            eng.h.wait_ge(r.sem[0], v)
            eng.seen[key] = v

    def _deps(self, eng, reads, writes):
        for r in reads:
            if r.last_w is not None:
                self._wait(eng, r.last_w)
        for w in writes:
            if w.last_w is not None:
                self._wait(eng, w.last_w)
            for t in w.readers.values():
                self._wait(eng, t)

    def _commit(self, tok, reads, writes):
        key = tok[1].name if tok[0] == "e" else ("d", id(tok[1]))
        for r in reads:
            r.readers[key] = tok
        for w in writes:
            w.last_w = tok
            w.readers = {}

    def op(self, engname, fn, reads=(), writes=()):
        eng = self.engs[engname]
        self._deps(eng, reads, writes)
        inst = fn(eng.h)
        eng.count += 1
        inst.then_inc(eng.sem, 1)
        tok = ("e", eng, eng.count)
        self._commit(tok, reads, writes)
        return inst

    def dma(self, out, in_, slot, reads=(), writes=(), q="sp", **kw):
        eng = self.engs[q]
        writes = list(writes)
        if slot not in writes:
            writes.append(slot)
        reads = [r for r in reads if r is not slot]
        self._deps(eng, reads, writes)
        sem = self._dma_sem(slot)
        inst = eng.h.dma_start(out=out, in_=in_, **kw)
        sem[1] += 16
        inst.then_inc(sem[0], 16)
        tok = ("d", slot, sem[1])
        self._commit(tok, reads, writes)
        return tok

    def wait_all(self, q="sp"):
        eng = self.engs[q]
        for e2 in self.engs.values():
            if e2.count > 0 and e2 is not eng:
                self._wait(eng, ("e", e2, e2.count))

    def wait_tok(self, q, tok):
        self._wait(self.engs[q], tok)


D = 2048
S = 16384
NCORE = 8
TL = S // NCORE
DEPTH = 4
HD = 128
NH = 8
KVL = 256
IH = 16
ID_ = 64
DFF = 5632
INW = 9552
ALPHA = (2.0 * DEPTH) ** 0.25
EPS = 1e-5
QSCALE = HD ** -0.5
NEG = -30000.0
BIG = 1.0e30
O_QD, O_CKV, O_QI, O_KI, O_WI, O_QM, O_KM, O_VM, O_GA, O_GB = 0, 1024, 1280, 2304, 2368, 2384, 3408, 4432, 5456, 7504


class Ctx:
    pass


def mk_psum(fw, n=8):
    banks = []
    for i in range(n):
        t = fw.psum("ps%d" % i, [128, 512], F32)
        banks.append((t, Res("ps%d" % i)))
    return banks


class Rot:
    def __init__(self, items):
        self.items = items
        self.i = 0

    def next(self):
        it = self.items[self.i % len(self.items)]
        self.i += 1
        return it


def mk_slots(fw, name, shape, dtype, n):
    return Rot([(fw.sbuf("%s%d" % (name, i), shape, dtype), Res("%s%d" % (name, i))) for i in range(n)])


def load_weight_slab(fw, C, src_ap, K, ncols):
    kc = K
    st, st_r = C.wst.next()
    wb, wb_r = C.wbf.next()
    fw.dma(out=st[:, 0:kc, 0:ncols], in_=src_ap.rearrange("(kc p) n -> p kc n", p=128), slot=st_r)
    fw.op("pool", lambda e: e.tensor_copy(out=wb[:, 0:kc, 0:ncols], in_=st[:, 0:kc, 0:ncols]),
          reads=[st_r], writes=[wb_r])
    return wb, wb_r


def phase_proj(fw, C, L):
    nc = fw.nc
    fw.begin_phase()
    banks = mk_psum(fw, 7)
    psb = Rot(banks)
    pst = fw.psum("pst", [128, 1024], BF16)
    pst_r = Res("pst")
    xb = fw.sbuf("xb", [128, 16, TL], BF16)
    xb_r = Res("xb")
    C.wst = mk_slots(fw, "wst", [128, 16, 256], F32, 2)
    C.wbf = mk_slots(fw, "wbf", [128, 16, 256], BF16, 2)
    xst = mk_slots(fw, "xst", [128, 16, 256], F32, 2)
    ob = mk_slots(fw, "ob", [128, 512], BF16, 4)
    of = mk_slots(fw, "of", [128, 512], F32, 3)
    qd = mk_slots(fw, "qd", [128, 512], BF16, 2)
    small = mk_slots(fw, "small", [128, 16], F32, 4)
    junk = fw.sbuf("junk", [128, 256], BF16); junk_r = Res("junk")
    tmpf = mk_slots(fw, "tmpf", [128, 256], F32, 2)
    wuk_st = fw.sbuf("wuk_st", [128, 8, 256], F32); wuk_st_r = Res()
    wuk = fw.sbuf("wuk", [128, 8, 256], BF16); wuk_r = Res()
    kmean = fw.sbuf("kmean", [128, 8, 8], F32); kmean_r = Res()
    cst = C.cst
    ident = fw.sbuf("ident", [128, 128], BF16); ident_r = Res()
    kvg = fw.sbuf("kvg", [128, 256], F32); kvg_r = Res()
    kig = fw.sbuf("kig", [128, 64], F32); kig_r = Res()
    kib = fw.sbuf("kib", [128, 64], F32); kib_r = Res()
    bg = fw.sbuf("bg", [128, 2, 16], F32); bg_r = Res()
    fw.dma(out=ident[:], in_=C.d_ident[:, :], slot=ident_r)
    fw.dma(out=kvg[:], in_=C.d_kvg[L], slot=kvg_r)
    fw.dma(out=kig[:], in_=C.d_kig[L], slot=kig_r)
    fw.dma(out=kib[:], in_=C.d_kib[L], slot=kib_r)
    fw.dma(out=bg[:], in_=C.d_bg[L], slot=bg_r)
    fw.dma(out=wuk_st[:], in_=C.w_uk[L].rearrange("h d c -> d h c"), slot=wuk_st_r)
    fw.op("pool", lambda e: e.tensor_copy(out=wuk[:], in_=wuk_st[:]), reads=[wuk_st_r], writes=[wuk_r])

    for j in range(TL // 256):
        st, st_r = xst.next()
        fw.dma(out=st[:], in_=C.xT_res.rearrange("(kc p) t -> p kc t", p=128)[:, :, j * 256:(j + 1) * 256], slot=st_r)
        eng = "dve" if j % 2 == 0 else "act"
        if eng == "dve":
            fw.op("dve", lambda e: e.tensor_copy(out=xb[:, :, j * 256:(j + 1) * 256], in_=st[:]), reads=[st_r], writes=[xb_r])
        else:
            fw.op("act", lambda e: e.copy(out=xb[:, :, j * 256:(j + 1) * 256], in_=st[:]), reads=[st_r], writes=[xb_r])

    w_in = C.w_in[L]
    evac_i = [0]

    def evac_copy(dst, dst_r, src, src_r, scale=None):
        i = evac_i[0]; evac_i[0] += 1
        if i % 2 == 0:
            if scale is None:
                fw.op("act", lambda e: e.copy(out=dst, in_=src), reads=[src_r], writes=[dst_r])
            else:
                fw.op("act", lambda e: e.mul(out=dst, in_=src, mul=float(scale)), reads=[src_r], writes=[dst_r])
        else:
            if scale is None:
                fw.op("dve", lambda e: e.tensor_copy(out=dst, in_=src), reads=[src_r], writes=[dst_r])
            else:
                fw.op("dve", lambda e: e.tensor_scalar(out=dst, in0=src, scalar1=float(scale), scalar2=None, op0=ALU.mult),
                      reads=[src_r], writes=[dst_r])

    def fm_matmul(wb, wb_r, blk, j):
        ps, ps_r = psb.next()
        for kc in range(16):
            fw.op("pe", lambda e: e.matmul(ps[:], lhsT=wb[:, kc, blk * 128:(blk + 1) * 128], rhs=xb[:, kc, j * 512:(j + 1) * 512],
                                           start=(kc == 0), stop=(kc == 15)),
                  reads=[wb_r, xb_r], writes=[ps_r])
        return ps, ps_r

    def tm_matmul(wb, wb_r, ncols, ts):
        ps, ps_r = psb.next()
        for kc in range(16):
            fw.op("pe", lambda e: e.matmul(ps[:, 0:ncols], lhsT=xb[:, kc, ts * 128:(ts + 1) * 128], rhs=wb[:, kc, 0:ncols],
                                           start=(kc == 0), stop=(kc == 15)),
                  reads=[wb_r, xb_r], writes=[ps_r])
        return ps, ps_r

    def sec0():
        for sl in range(4):
            wb, wb_r = load_weight_slab(fw, C, w_in[:, O_QD + sl * 256:O_QD + (sl + 1) * 256], 16, 256)
            for j in range(4):
                for blk in range(2):
                    h = sl * 2 + blk
                    ps, ps_r = fm_matmul(wb, wb_r, blk, j)
                    q, q_r = qd.next()
                    evac_copy(q[:], q_r, ps[:], ps_r)
                    for cc in range(2):
                        ps2, ps2_r = psb.next()
                        fw.op("pe", lambda e: e.matmul(ps2[:], lhsT=wuk[:, h, cc * 128:(cc + 1) * 128], rhs=q[:], start=True, stop=True),
                              reads=[wuk_r, q_r], writes=[ps2_r])
                        o, o_r = ob.next()
                        evac_copy(o[:], o_r, ps2[:], ps2_r, scale=QSCALE)
                        fw.dma(out=C.q_latT[cc, :, h, j * 512:(j + 1) * 512], in_=o[:], slot=o_r)

    if getattr(C, 'stop', 99) >= 0:
        sec0()
    def sec1():
        wb, wb_r = load_weight_slab(fw, C, w_in[:, O_CKV:O_CKV + 256], 16, 256)
        for ts in range(16):
            ps, ps_r = tm_matmul(wb, wb_r, 256, ts)
            sm, sm_r = small.next()
            fw.op("act", lambda e: e.activation(out=junk[:], in_=ps[:, 0:256], func=AF.Square, accum_out=sm[:, 0:1]),
                  reads=[ps_r], writes=[junk_r, sm_r])
            fw.op("dve", lambda e: e.tensor_scalar(out=sm[:, 1:2], in0=sm[:, 0:1], scalar1=1.0 / 256, scalar2=EPS, op0=ALU.mult, op1=ALU.add),
                  reads=[sm_r], writes=[sm_r])
            fw.op("act", lambda e: e.activation(out=sm[:, 2:3], in_=sm[:, 1:2], func=AF.Sqrt), reads=[sm_r], writes=[sm_r])
            fw.op("dve", lambda e: e.reciprocal(out=sm[:, 3:4], in_=sm[:, 2:3]), reads=[sm_r], writes=[sm_r])
            tf, tf_r = tmpf.next()
            fw.op("dve", lambda e: e.tensor_scalar(out=tf[:], in0=ps[:, 0:256], scalar1=sm[:, 3:4], scalar2=None, op0=ALU.mult),
                  reads=[ps_r, sm_r], writes=[tf_r])
            o, o_r = ob.next()
            fw.op("dve", lambda e: e.tensor_tensor(out=o[:, 0:256], in0=tf[:], in1=kvg[:], op=ALU.mult), reads=[tf_r, kvg_r], writes=[o_r])
            for cc in range(2):
                fw.op("pe", lambda e: e.transpose(pst[:, cc * 128:(cc + 1) * 128], o[:, cc * 128:(cc + 1) * 128], ident[:]),
                      reads=[o_r, ident_r], writes=[pst_r])
            fw.op("act", lambda e: e.copy(out=o[:, 256:512], in_=pst[:, 0:256]), reads=[pst_r], writes=[o_r])
            fw.dma(out=C.c_kv_own[ts * 128:(ts + 1) * 128, :], in_=o[:, 0:256], slot=o_r)
            fw.dma(out=C.c_kvT_own[:, :, ts * 128:(ts + 1) * 128].rearrange("cc p t -> p cc t"),
                   in_=o[:, 256:512].rearrange("p (cc t) -> p cc t", cc=2), slot=o_r)

    if getattr(C, 'stop', 99) >= 1:
        sec1()
    def sec2():
        for sl in range(4):
            wb, wb_r = load_weight_slab(fw, C, w_in[:, O_QI + sl * 256:O_QI + (sl + 1) * 256], 16, 256)
            for j in range(4):
                for blk in range(2):
                    ps, ps_r = fm_matmul(wb, wb_r, blk, j)
                    o, o_r = ob.next()
                    evac_copy(o[:], o_r, ps[:], ps_r)
                    fw.dma(out=C.q_idxT[sl * 2 + blk, :, j * 512:(j + 1) * 512], in_=o[:], slot=o_r)

    if getattr(C, 'stop', 99) >= 2:
        sec2()
    def sec3():
        wb, wb_r = load_weight_slab(fw, C, w_in[:, O_KI:O_KI + 80], 16, 80)
        for ts in range(16):
            ps, ps_r = tm_matmul(wb, wb_r, 80, ts)
            sm, sm_r = small.next()
            fw.op("dve", lambda e: e.bn_stats(out=sm[:, 0:6], in_=ps[:, 0:64]), reads=[ps_r], writes=[sm_r])
            fw.op("dve", lambda e: e.bn_aggr(out=sm[:, 6:8], in_=sm[:, 0:6]), reads=[sm_r], writes=[sm_r])
            fw.op("dve", lambda e: e.tensor_scalar(out=sm[:, 8:9], in0=sm[:, 7:8], scalar1=EPS, scalar2=None, op0=ALU.add), reads=[sm_r], writes=[sm_r])
            fw.op("act", lambda e: e.activation(out=sm[:, 9:10], in_=sm[:, 8:9], func=AF.Sqrt), reads=[sm_r], writes=[sm_r])
            fw.op("dve", lambda e: e.reciprocal(out=sm[:, 10:11], in_=sm[:, 9:10]), reads=[sm_r], writes=[sm_r])
            tf, tf_r = tmpf.next()
            fw.op("dve", lambda e: e.tensor_scalar(out=tf[:, 0:64], in0=ps[:, 0:64], scalar1=sm[:, 6:7], scalar2=sm[:, 10:11],
                                                   op0=ALU.subtract, op1=ALU.mult), reads=[ps_r, sm_r], writes=[tf_r])
            fw.op("dve", lambda e: e.tensor_tensor(out=tf[:, 64:128], in0=tf[:, 0:64], in1=kig[:], op=ALU.mult), reads=[tf_r, kig_r], writes=[tf_r])
            o, o_r = ob.next()
            fw.op("dve", lambda e: e.tensor_tensor(out=o[:, 0:64], in0=tf[:, 64:128], in1=kib[:], op=ALU.add), reads=[tf_r, kib_r], writes=[o_r])
            fw.op("pe", lambda e: e.transpose(pst[0:64, 0:128], o[:, 0:64], ident[:]), reads=[o_r, ident_r], writes=[pst_r])
            fw.op("act", lambda e: e.copy(out=o[0:64, 128:256], in_=pst[0:64, 0:128]), reads=[pst_r], writes=[o_r])
            fw.dma(out=C.k_idxT_own[:, ts * 128:(ts + 1) * 128], in_=o[0:64, 128:256], slot=o_r)
            f, f_r = of.next()
            fw.op("act", lambda e: e.mul(out=f[:, 0:16], in_=ps[:, 64:80], mul=1.0 / 32), reads=[ps_r], writes=[f_r])
            fw.dma(out=C.w_idx[ts * 128:(ts + 1) * 128, :], in_=f[:, 0:16], slot=f_r)

    if getattr(C, 'stop', 99) >= 3:
        sec3()
    def sec4():
        for sl in range(4):
            wb, wb_r = load_weight_slab(fw, C, w_in[:, O_QM + sl * 256:O_QM + (sl + 1) * 256], 16, 256)
            for j in range(4):
                for blk in range(2):
                    ps, ps_r = fm_matmul(wb, wb_r, blk, j)
                    o, o_r = ob.next()
                    evac_copy(o[:], o_r, ps[:], ps_r, scale=QSCALE)
                    fw.dma(out=C.q_mT[sl * 2 + blk, :, j * 512:(j + 1) * 512], in_=o[:], slot=o_r)
        for sl in range(4):
            wb, wb_r = load_weight_slab(fw, C, w_in[:, O_KM + sl * 256:O_KM + (sl + 1) * 256], 16, 256)
            for j in range(4):
                for blk in range(2):
                    h = sl * 2 + blk
                    ps, ps_r = fm_matmul(wb, wb_r, blk, j)
                    o, o_r = ob.next()
                    for s2 in range(2):
                        fw.op("act", lambda e: e.activation(out=o[:, s2 * 256:(s2 + 1) * 256], in_=ps[:, s2 * 256:(s2 + 1) * 256], func=AF.Copy,
                                                            accum_out=kmean[:, h, 2 * j + s2:2 * j + s2 + 1]),
                              reads=[ps_r], writes=[o_r, kmean_r])
                    fw.dma(out=C.k_mT_own[h, :, j * 512:(j + 1) * 512], in_=o[:], slot=o_r)
        fw.op("dve", lambda e: e.tensor_scalar(out=kmean[:], in0=kmean[:], scalar1=1.0 / 256, scalar2=None, op0=ALU.mult), reads=[kmean_r], writes=[kmean_r])
        fw.dma(out=C.k_meanT_own[:, :, :], in_=kmean[:], slot=kmean_r)

    if getattr(C, 'stop', 99) >= 4:
        sec4()
    def sec5():
        for sl in range(4):
            wb, wb_r = load_weight_slab(fw, C, w_in[:, O_VM + sl * 256:O_VM + (sl + 1) * 256], 16, 256)
            for ts in range(16):
                ps, ps_r = tm_matmul(wb, wb_r, 256, ts)
                o, o_r = ob.next()
                evac_copy(o[:, 0:256], o_r, ps[:, 0:256], ps_r)
                fw.dma(out=C.v_m_own[ts * 128:(ts + 1) * 128, sl * 256:(sl + 1) * 256], in_=o[:, 0:256], slot=o_r)

    if getattr(C, 'stop', 99) >= 5:
        sec5()
    def sec6():
        for gi, (off, dst) in enumerate([(O_GA, C.g_aT), (O_GB, C.g_bT)]):
            for sl in range(8):
                wb, wb_r = load_weight_slab(fw, C, w_in[:, off + sl * 256:off + (sl + 1) * 256], 16, 256)
                for j in range(4):
                    for blk in range(2):
                        fb = sl * 2 + blk
                        ps, ps_r = fm_matmul(wb, wb_r, blk, j)
                        f, f_r = of.next()
                        fw.op("act", lambda e: e.activation(out=f[:], in_=ps[:], func=AF.Sigmoid, bias=bg[:, gi, fb:fb + 1]),
                              reads=[ps_r, bg_r], writes=[f_r])
                        fw.dma(out=dst[fb, :, j * 512:(j + 1) * 512], in_=f[:], slot=f_r)

    if getattr(C, 'stop', 99) >= 6:
        sec6()
    fw.end_phase()


import ml_dtypes
NPBF = ml_dtypes.bfloat16


def own_tokens(c):
    return np.concatenate([np.arange(256 * (8 * k + c), 256 * (8 * k + c) + 256) for k in range(8)])


def declare(fw, C, name, shape, dtype, ext):
    kind = "Internal"
    if name in ext:
        kind = ext[name]
    t = fw.nc.dram_tensor(name, list(shape), dtype, kind=kind).ap()
    setattr(C, name, t)
    return t


def declare_proj_io(fw, C, nl, ext):
    declare(fw, C, "xT_res", [D, TL], F32, ext)
    declare(fw, C, "w_in", [nl, D, INW], F32, ext)
    declare(fw, C, "w_uk", [nl, NH, HD, KVL], F32, ext)
    declare(fw, C, "d_ident", [128, 128], BF16, ext)
    declare(fw, C, "d_kvg", [nl, 128, KVL], F32, ext)
    declare(fw, C, "d_kig", [nl, 128, ID_], F32, ext)
    declare(fw, C, "d_kib", [nl, 128, ID_], F32, ext)
    declare(fw, C, "d_bg", [nl, 128, 2, 16], F32, ext)
    declare(fw, C, "q_latT", [2, 128, NH, TL], BF16, ext)
    declare(fw, C, "q_idxT", [8, 128, TL], BF16, ext)
    declare(fw, C, "w_idx", [TL, IH], F32, ext)
    declare(fw, C, "k_idxT_own", [ID_, TL], BF16, ext)
    declare(fw, C, "c_kv_own", [TL, KVL], BF16, ext)
    declare(fw, C, "c_kvT_own", [2, 128, TL], BF16, ext)
    declare(fw, C, "q_mT", [NH, 128, TL], BF16, ext)
    declare(fw, C, "k_mT_own", [NH, 128, TL], BF16, ext)
    declare(fw, C, "k_meanT_own", [128, NH, 8], F32, ext)
    declare(fw, C, "v_m_own", [TL, NH * HD], BF16, ext)
    declare(fw, C, "g_aT", [16, 128, TL], F32, ext)
    declare(fw, C, "g_bT", [16, 128, TL], F32, ext)


def host_consts_proj(inp, layers):
    out = {}
    out["d_ident"] = np.eye(128, dtype=np.float32).astype(NPBF)
    out["d_kvg"] = np.ascontiguousarray(np.broadcast_to(inp["kv_norm_g"][layers][:, None, :], (len(layers), 128, KVL))).astype(np.float32)
    out["d_kig"] = np.ascontiguousarray(np.broadcast_to(inp["idx_k_norm_g"][layers][:, None, :], (len(layers), 128, ID_))).astype(np.float32)
    out["d_kib"] = np.ascontiguousarray(np.broadcast_to(inp["idx_k_norm_b"][layers][:, None, :], (len(layers), 128, ID_))).astype(np.float32)
    bg = inp["b_gate"][layers]
    out["d_bg"] = np.ascontiguousarray(bg.reshape(len(layers), 2, 16, 128).transpose(0, 3, 1, 2)).astype(np.float32)
    return out


NIT = 22


def declare_attn_io(fw, C, ext):
    declare(fw, C, "k_idxT_g", [ID_, S], BF16, ext)
    declare(fw, C, "c_kvT_g", [2, 128, S], BF16, ext)
    declare(fw, C, "c_kv_g", [S, KVL], BF16, ext)
    declare(fw, C, "k_mT_g", [NH, 128, S], BF16, ext)
    declare(fw, C, "v_m_g", [S, NH * HD], BF16, ext)
    declare(fw, C, "k_meanT_g", [128, NH, 64], F32, ext)
    declare(fw, C, "maskb", [16, 128, S], BF16, ext)
    declare(fw, C, "ddall", [128, 16], F32, ext)
    declare(fw, C, "o_aT", [NH, 128, TL], BF16, ext)
    declare(fw, C, "o_bT", [NH, 128, TL], BF16, ext)
    declare(fw, C, "d_tloc", [128, 16], F32, ext)
    declare(fw, C, "d_iota", [128, 2048], F32, ext)
    declare(fw, C, "d_pow2", [128, NIT], F32, ext)
    declare(fw, C, "d_slopes", [128, NH], F32, ext)
    declare(fw, C, "d_kb", [128, 128, NH], F32, ext)
    declare(fw, C, "d_ones", [128, 128], BF16, ext)
    declare(fw, C, "w_uv", [C.nl, NH, KVL, HD], F32, ext)


def host_consts_attn(c):
    out = {}
    tl = np.zeros((128, 16), np.float32)
    for qb in range(16):
        tl[:, qb] = 256 * c + 128 * (qb % 2) + np.arange(128)
    out["d_tloc"] = tl
    out["d_iota"] = np.ascontiguousarray(np.broadcast_to(np.arange(2048, dtype=np.float32)[None, :], (128, 2048)))
    p2 = np.array([2.0 ** (-i) for i in range(NIT)], np.float32); p2[0] = 1.0 + 1e-6
    out["d_pow2"] = np.ascontiguousarray(np.broadcast_to(p2[None, :], (128, NIT)))
    sl = np.array([2.0 ** (-(i + 1)) for i in range(NH)], np.float32)
    out["d_slopes"] = np.ascontiguousarray(np.broadcast_to(sl[None, :], (128, NH)))
    pos = (128 * np.arange(128)[None, :, None] + np.arange(128)[:, None, None]).astype(np.float32)
    out["d_kb"] = np.ascontiguousarray(pos * sl[None, None, :]).astype(np.float32)
    out["d_ones"] = np.ones((128, 128), np.float32).astype(NPBF)
    return out


def phase_dsa_select(fw, C, L):
    fw.begin_phase()
    banks = mk_psum(fw, 8)
    psL = Rot(banks[0:5]); psS = Rot(banks[5:8])
    kid = fw.sbuf("kid", [128, S], BF16); kid_r = Res()
    score = fw.sbuf("score", [128, S], F32); score_r = Res()
    mb = fw.sbuf("mb", [128, S], BF16); mb_r = Res()
    Rt = mk_slots(fw, "Rt", [128, 16, 256], BF16, 2)
    dg = mk_slots(fw, "dg", [128, 16, 128], BF16, 2)
    qi = mk_slots(fw, "qi", [128, 8, 128], BF16, 2)
    wi = mk_slots(fw, "wi", [128, 16], F32, 2)
    iota = fw.sbuf("iota", [128, 2048], F32); iota_r = Res()
    tmpz = fw.sbuf("tmpz", [128, 2048], F32); tmpz_r = Res()
    ident = fw.sbuf("ident", [128, 128], BF16); ident_r = Res()
    tloc = fw.sbuf("tloc", [128, 16], F32); tloc_r = Res()
    pow2 = fw.sbuf("pow2", [128, NIT], F32); pow2_r = Res()
    ddall = fw.sbuf("ddall", [128, 16], F32); ddall_r = Res()
    a32 = fw.sbuf("a32", [128, 512], F32); a32_r = Res()
    sm = mk_slots(fw, "sm", [128, 8 + NIT], F32, 2)
    fw.dma(out=kid[0:64, :], in_=C.k_idxT_g[:, :], slot=kid_r)
    kid2_r = Res()
    fw.dma(out=kid[64:128, :], in_=C.k_idxT_g[:, :], slot=kid2_r)
    fw.dma(out=iota[:], in_=C.d_iota[:, :], slot=iota_r)
    fw.dma(out=ident[:], in_=C.d_ident[:, :], slot=ident_r)
    fw.dma(out=tloc[:], in_=C.d_tloc[:, :], slot=tloc_r)
    fw.dma(out=pow2[:], in_=C.d_pow2[:, :], slot=pow2_r)
    fw.op("dve", lambda e: e.memset(ddall[:], 0.0), writes=[ddall_r])
    ev = [0]
    for qb in range(C.nqb):
        k = qb // 2; hf = qb % 2
        tl0 = 256 * k + 128 * hf
        NK = 2048 * (k + 1)
        q, q_r = qi.next(); w, w_r = wi.next(); d, d_r = dg.next()
        fw.dma(out=q[:], in_=C.q_idxT[:, :, tl0:tl0 + 128].rearrange("b p t -> p b t"), slot=q_r)
        fw.dma(out=w[:], in_=C.w_idx[tl0:tl0 + 128, :], slot=w_r)
        for h in range(16):
            fw.op("pool", lambda e: e.tensor_scalar(out=d[:, h, :], in0=ident[:], scalar1=w[:, h:h + 1], scalar2=None, op0=ALU.mult),
                  reads=[ident_r, w_r], writes=[d_r])
        for ck in range(NK // 256):
            R, R_r = Rt.next()
            for h in range(16):
                blk = h // 2; po = (h % 2) * 64
                ps, ps_r = psL.next()
                fw.op("pe", lambda e: e.matmul(ps[:, 0:256], lhsT=q[po:po + 64, blk, :], rhs=kid[po:po + 64, ck * 256:(ck + 1) * 256], start=True, stop=True),
                      reads=[q_r, kid_r, kid2_r], writes=[ps_r])
                ev[0] += 1
                if ev[0] % 2 == 0:
                    fw.op("act", lambda e: e.activation(out=R[:, h, :], in_=ps[:, 0:256], func=AF.Relu), reads=[ps_r], writes=[R_r])
                else:
                    fw.op("dve", lambda e: e.tensor_scalar(out=R[:, h, :], in0=ps[:, 0:256], scalar1=0.0, scalar2=None, op0=ALU.max), reads=[ps_r], writes=[R_r])
            ps, ps_r = psS.next()
            for h in range(16):
                fw.op("pe", lambda e: e.matmul(ps[:, 0:256], lhsT=d[:, h, :], rhs=R[:, h, :], start=(h == 0), stop=(h == 15)),
                      reads=[d_r, R_r], writes=[ps_r])
            fw.op("act", lambda e: e.copy(out=score[:, ck * 256:(ck + 1) * 256], in_=ps[:, 0:256]), reads=[ps_r], writes=[score_r])
        s, s_r = sm.next()
        fw.op("dve", lambda e: e.tensor_reduce(out=s[:, 0:1], in_=score[:, 0:NK], axis=AX.X, op=ALU.max, apply_absolute_value=True), reads=[score_r], writes=[s_r])
        fw.op("dve", lambda e: e.tensor_scalar(out=s[:, 8:8 + NIT], in0=pow2[:], scalar1=s[:, 0:1], scalar2=None, op0=ALU.mult), reads=[pow2_r, s_r], writes=[s_r])
        fw.op("dve", lambda e: e.tensor_scalar(out=s[:, 1:2], in0=s[:, 0:1], scalar1=-1.0, scalar2=None, op0=ALU.mult), reads=[s_r], writes=[s_r])
        fw.op("dve", lambda e: e.tensor_scalar(out=tmpz[:], in0=iota[:], scalar1=tloc[:, qb:qb + 1], scalar2=-BIG, op0=ALU.is_gt, op1=ALU.mult),
              reads=[iota_r, tloc_r], writes=[tmpz_r])
        fw.op("dve", lambda e: e.tensor_tensor(out=score[:, NK - 2048:NK], in0=score[:, NK - 2048:NK], in1=tmpz[:], op=ALU.add), reads=[tmpz_r, score_r], writes=[score_r])
        for it in range(NIT):
            fw.op("dve", lambda e: e.tensor_tensor(out=s[:, 2:3], in0=s[:, 1:2], in1=s[:, 8 + it:9 + it], op=ALU.add), reads=[s_r], writes=[s_r])
            fw.op("dve", lambda e: e.tensor_scalar(out=mb[:, 0:NK], in0=score[:, 0:NK], scalar1=s[:, 2:3], scalar2=None, op0=ALU.is_ge, op1=ALU.add, accum_out=s[:, 3:4]),
                  reads=[score_r, s_r], writes=[mb_r, s_r])
            fw.op("dve", lambda e: e.scalar_tensor_tensor(out=s[:, 4:5], in0=s[:, 3:4], scalar=256.0, in1=s[:, 8 + it:9 + it], op0=ALU.is_ge, op1=ALU.mult), reads=[s_r], writes=[s_r])
            fw.op("dve", lambda e: e.tensor_tensor(out=s[:, 1:2], in0=s[:, 1:2], in1=s[:, 4:5], op=ALU.add), reads=[s_r], writes=[s_r])
        fw.op("dve", lambda e: e.tensor_scalar(out=mb[:, 0:NK], in0=score[:, 0:NK], scalar1=s[:, 1:2], scalar2=NEG, op0=ALU.is_lt, op1=ALU.mult),
              reads=[score_r, s_r], writes=[mb_r])
        fw.dma(out=C.maskb[qb, :, 0:NK], in_=mb[:, 0:NK], slot=mb_r)
        ng = NK // 32
        fw.op("dve", lambda e: e.tensor_reduce(out=a32[:, 0:ng], in_=mb[:, 0:NK].rearrange("p (g e) -> p g e", e=32), axis=AX.X, op=ALU.max), reads=[mb_r], writes=[a32_r])
        fw.op("dve", lambda e: e.tensor_scalar(out=tmpz[:, 0:ng], in0=iota[:, 0:ng], scalar1=1.0, scalar2=None, op0=ALU.add), reads=[iota_r], writes=[tmpz_r])
        fw.op("dve", lambda e: e.scalar_tensor_tensor(out=a32[:, 0:ng], in0=a32[:, 0:ng], scalar=-1.0, in1=tmpz[:, 0:ng], op0=ALU.is_ge, op1=ALU.mult), reads=[a32_r, tmpz_r], writes=[a32_r])
        fw.op("dve", lambda e: e.tensor_reduce(out=s[:, 5:6], in_=a32[:, 0:ng], axis=AX.X, op=ALU.max), reads=[a32_r], writes=[s_r])
        fw.op("dve", lambda e: e.tensor_scalar(out=ddall[:, qb:qb + 1], in0=s[:, 5:6], scalar1=-32.0, scalar2=1.0, op0=ALU.mult, op1=ALU.add), reads=[s_r], writes=[ddall_r])
    fw.dma(out=C.ddall[:, :], in_=ddall[:], slot=ddall_r)
    fw.end_phase()


def phase_dsa_attn(fw, C, L):
    fw.begin_phase()
    banks = mk_psum(fw, 8)
    psT = Rot(banks[0:2]); psO = [banks[2], banks[3]]; psD = banks[4]; psU = Rot(banks[5:7])
    ckT = fw.sbuf("ckT", [128, 2, S], BF16); ckT_r = Res()
    ckv = fw.sbuf("ckv", [128, 128, KVL], BF16); ckv_r = Res()
    ql = mk_slots(fw, "ql", [128, 2, NH, 128], BF16, 2)
    mbt = mk_slots(fw, "mbt", [128, 2048], BF16, 2)
    pt = mk_slots(fw, "pt", [128, 512], BF16, 3)
    Er = mk_slots(fw, "Er", [128, 512], BF16, 2)
    E4 = fw.sbuf("E4", [128, 512], BF16); E4_r = Res()
    ident = fw.sbuf("ident", [128, 128], BF16); ident_r = Res()
    ones = fw.sbuf("ones", [128, 128], BF16); ones_r = Res()
    kb = fw.sbuf("kb", [128, 128, NH], F32); kb_r = Res()
    slopes = fw.sbuf("slopes", [128, NH], F32); slopes_r = Res()
    ddall = fw.sbuf("ddall", [128, 16], F32); ddall_r = Res()
    rp = mk_slots(fw, "rp", [128, NH], F32, 2)
    rec = mk_slots(fw, "rec", [128, 512], F32, 2)
    olat = mk_slots(fw, "olat", [128, 2, 512], BF16, 2)
    oa = mk_slots(fw, "oa", [128, 512], BF16, 2)
    wuv_st = fw.sbuf("wuv_st", [128, NH, 2, 128], F32); wuv_st_r = Res()
    wuv = fw.sbuf("wuv", [128, NH, 2, 128], BF16); wuv_r = Res()
    fw.dma(out=ckT[:], in_=C.c_kvT_g.rearrange("cc p s -> p cc s"), slot=ckT_r)
    fw.dma(out=ckv[:], in_=C.c_kv_g.rearrange("(b p) c -> p b c", p=128), slot=ckv_r)
    fw.dma(out=ident[:], in_=C.d_ident[:, :], slot=ident_r)
    fw.dma(out=ones[:], in_=C.d_ones[:, :], slot=ones_r)
    fw.dma(out=kb[:], in_=C.d_kb[:, :, :], slot=kb_r)
    fw.dma(out=slopes[:], in_=C.d_slopes[:, :], slot=slopes_r)
    fw.dma(out=ddall[:], in_=C.ddall[:, :], slot=ddall_r)
    fw.dma(out=wuv_st[:], in_=C.w_uv[L].rearrange("h (cc p) d -> p h cc d", p=128), slot=wuv_st_r)
    fw.op("pool", lambda e: e.tensor_copy(out=wuv[:], in_=wuv_st[:]), reads=[wuv_st_r], writes=[wuv_r])
    for i in range(4):
        fw.op("pool", lambda e: e.tensor_copy(out=E4[:, i * 128:(i + 1) * 128], in_=ident[:]), reads=[ident_r], writes=[E4_r])
    for qb in range(C.nqb):
        k = qb // 2; hf = qb % 2
        tl0 = 256 * k + 128 * hf
        NK = 2048 * (k + 1)
        q, q_r = ql.next()
        for cc in range(2):
            fw.dma(out=q[:, cc, :, :], in_=C.q_latT[cc, :, :, tl0:tl0 + 128], slot=q_r)
        r, r_r = rp.next()
        fw.op("dve", lambda e: e.tensor_scalar(out=r[:], in0=slopes[:], scalar1=ddall[:, qb:qb + 1], scalar2=None, op0=ALU.mult), reads=[slopes_r, ddall_r], writes=[r_r])
        for g in range(2):
            er, er_r = Er.next()
            for hh in range(4):
                h = g * 4 + hh
                fw.op("dve", lambda e: e.tensor_scalar(out=er[:, hh * 128:(hh + 1) * 128], in0=ident[:], scalar1=r[:, h:h + 1], scalar2=None, op0=ALU.mult),
                      reads=[ident_r, r_r], writes=[er_r])
            nkb = NK // 128
            for jk in range(nkb):
                if jk % 16 == 0:
                    m, m_r = mbt.next()
                    fw.dma(out=m[:], in_=C.maskb[qb, :, jk * 128:jk * 128 + 2048], slot=m_r)
                ps, ps_r = psT.next()
                rhs_q = [q[:, cc, g * 4:(g + 1) * 4, :] for cc in range(2)]
                fw.op("pe", lambda e: e.matmul(ps[:], lhsT=ckT[:, 0, jk * 128:(jk + 1) * 128], rhs=rhs_q[0], start=True, stop=False), reads=[ckT_r, q_r], writes=[ps_r])
                fw.op("pe", lambda e: e.matmul(ps[:], lhsT=ckT[:, 1, jk * 128:(jk + 1) * 128], rhs=rhs_q[1], start=False, stop=False), reads=[ckT_r, q_r], writes=[ps_r])
                fw.op("pe", lambda e: e.matmul(ps[:], lhsT=m[:, (jk % 16) * 128:(jk % 16 + 1) * 128], rhs=E4[:], start=False, stop=False), reads=[m_r, E4_r], writes=[ps_r])
                fw.op("pe", lambda e: e.matmul(ps[:], lhsT=ones[:], rhs=er[:], start=False, stop=True), reads=[ones_r, er_r], writes=[ps_r])
                p, p_r = pt.next()
                for hh in range(4):
                    h = g * 4 + hh
                    fw.op("act", lambda e: e.activation(out=p[:, hh * 128:(hh + 1) * 128], in_=ps[:, hh * 128:(hh + 1) * 128], func=AF.Exp, bias=kb[:, jk, h:h + 1]),
                          reads=[ps_r, kb_r], writes=[p_r])
                for cc in range(2):
                    fw.op("pe", lambda e: e.matmul(psO[cc][0][:], lhsT=ckv[:, jk, cc * 128:(cc + 1) * 128], rhs=p[:], start=(jk == 0), stop=(jk == nkb - 1)),
                          reads=[ckv_r, p_r], writes=[psO[cc][1]])
                fw.op("pe", lambda e: e.matmul(psD[0][:], lhsT=ones[:], rhs=p[:], start=(jk == 0), stop=(jk == nkb - 1)), reads=[ones_r, p_r], writes=[psD[1]])
            rc, rc_r = rec.next()
            fw.op("dve", lambda e: e.reciprocal(out=rc[:], in_=psD[0][:]), reads=[psD[1]], writes=[rc_r])
            ol, ol_r = olat.next()
            for cc in range(2):
                fw.op("dve", lambda e: e.tensor_tensor(out=ol[:, cc, :], in0=psO[cc][0][:], in1=rc[:], op=ALU.mult), reads=[psO[cc][1], rc_r], writes=[ol_r])
            pu, pu_r = psU.next()
            for hh in range(4):
                h = g * 4 + hh
                for cc in range(2):
                    fw.op("pe", lambda e: e.matmul(pu[:, hh * 128:(hh + 1) * 128], lhsT=wuv[:, h, cc, :], rhs=ol[:, cc, hh * 128:(hh + 1) * 128], start=(cc == 0), stop=(cc == 1)),
                          reads=[wuv_r, ol_r], writes=[pu_r])
            o, o_r = oa.next()
            fw.op("act", lambda e: e.copy(out=o[:], in_=pu[:]), reads=[pu_r], writes=[o_r])
            fw.dma(out=C.o_aT[g * 4:(g + 1) * 4, :, tl0:tl0 + 128].rearrange("h p t -> p h t"), in_=o[:].rearrange("p (h t) -> p h t", h=4), slot=o_r)
    fw.end_phase()


def declare_moba_io(fw, C, ext):
    declare(fw, C, "d_pastneg", [128, 8, 64], F32, ext)
    declare(fw, C, "d_pastind", [128, 8, 64], F32, ext)
    declare(fw, C, "d_alq", [128, 2, NH], F32, ext)
    declare(fw, C, "d_zc", [128, 32, 128], BF16, ext)
    declare(fw, C, "d_kbm", [128, NH, 128], F32, ext)


def host_consts_moba(c):
    out = {}
    pn = np.zeros((128, 8, 64), np.float32); pi = np.zeros((128, 8, 64), np.float32)
    for k in range(8):
        own = 8 * k + c
        pn[:, k, own:] = -BIG
        pi[:, k, :own] = 1.0
    out["d_pastneg"] = pn; out["d_pastind"] = pi
    sl = np.array([2.0 ** (-(i + 1)) for i in range(NH)], np.float32)
    p = np.arange(128, dtype=np.float32)
    alq = np.zeros((128, 2, NH), np.float32)
    for qh in range(2):
        alq[:, qh, :] = (255 - 128 * qh - p)[:, None] * sl[None, :]
    out["d_alq"] = alq
    zc = np.zeros((128, 8, 2, 2, 128), np.float32)
    for cp in range(8):
        for qh in range(2):
            for j in range(2):
                if cp > c:
                    zc[:, cp, qh, j, :] = NEG
                elif cp == c:
                    kpos = 128 * j + np.arange(128)[None, :]
                    qpos = 128 * qh + np.arange(128)[:, None]
                    zc[:, cp, qh, j, :] = np.where(kpos <= qpos, 0.0, NEG)
    out["d_zc"] = zc.reshape(128, 32, 128).astype(NPBF)
    m = np.arange(128, dtype=np.float32)
    kbm = (p[:, None, None] + 128 * (m[None, None, :] - 112) - 256 * c - 255) * sl[None, :, None]
    out["d_kbm"] = kbm.astype(np.float32)
    return out


def phase_moba(fw, C, L):
    fw.begin_phase()
    banks = mk_psum(fw, 8)
    psT = Rot(banks[0:3]); psO = banks[3]; psD = banks[4]; psG = Rot(banks[5:7])
    kTs = mk_slots(fw, "kT", [128, S], BF16, 2)
    vhs = mk_slots(fw, "vh", [128, 128, 128], BF16, 2)
    qTs = mk_slots(fw, "qT", [128, TL], BF16, 2)
    kmf = fw.sbuf("kmf", [128, NH, 64], F32); kmf_r = Res()
    kmb = fw.sbuf("kmb", [128, NH, 64], BF16); kmb_r = Res()
    ident = fw.sbuf("ident", [128, 128], BF16); ident_r = Res()
    ones = fw.sbuf("ones", [128, 128], BF16); ones_r = Res()
    pastneg = fw.sbuf("pastneg", [128, 8, 64], F32); pastneg_r = Res()
    pastind = fw.sbuf("pastind", [128, 8, 64], F32); pastind_r = Res()
    alq = fw.sbuf("alq", [128, 2, NH], F32); alq_r = Res()
    zc = fw.sbuf("zc", [128, 32, 128], BF16); zc_r = Res()
    kbm = fw.sbuf("kbm", [128, NH, 128], F32); kbm_r = Res()
    gss = mk_slots(fw, "gs", [128, 64 + 64 + 16], F32, 2)
    sbs = mk_slots(fw, "sb", [128, 2, 64], BF16, 2)
    bzs = mk_slots(fw, "bz", [128, 128], BF16, 4)
    pts = mk_slots(fw, "pt", [128, 256], BF16, 3)
    recs = mk_slots(fw, "rec", [128, 256], F32, 2)
    obs = mk_slots(fw, "ob", [128, 256], BF16, 2)
    for t, r, src in [(kmf, kmf_r, C.k_meanT_g[:, :, :]), (ident, ident_r, C.d_ident[:, :]), (ones, ones_r, C.d_ones[:, :]),
                      (pastneg, pastneg_r, C.d_pastneg[:, :, :]), (pastind, pastind_r, C.d_pastind[:, :, :]), (alq, alq_r, C.d_alq[:, :, :]),
                      (zc, zc_r, C.d_zc[:, :, :]), (kbm, kbm_r, C.d_kbm[:, :, :])]:
        fw.dma(out=t[:], in_=src, slot=r)
    fw.op("dve", lambda e: e.tensor_copy(out=kmb[:], in_=kmf[:]), reads=[kmf_r], writes=[kmb_r])
    for h in range(C.nheads):
        kT, kT_r = kTs.next(); vh, vh_r = vhs.next(); qT, qT_r = qTs.next()
        fw.dma(out=kT[:], in_=C.k_mT_g[h, :, :], slot=kT_r)
        vsrc = C.v_m_g.rearrange("(b p) (h d) -> p b h d", p=128, h=NH)
        for half in range(2):
            fw.dma(out=vh[:, half * 64:(half + 1) * 64, :], in_=vsrc[:, half * 64:(half + 1) * 64, h, :], slot=vh_r)
        fw.dma(out=qT[:], in_=C.q_mT[h, :, :], slot=qT_r)
        for k in range(C.nslots):
            sb, sb_r = sbs.next()
            for qh in range(2):
                pg, pg_r = psG.next()
                t0 = 256 * k + 128 * qh
                fw.op("pe", lambda e: e.matmul(pg[:, 0:64], lhsT=qT[:, t0:t0 + 128], rhs=kmb[:, h, :], start=True, stop=True), reads=[qT_r, kmb_r], writes=[pg_r])
                gs, gs_r = gss.next()
                fw.op("dve", lambda e: e.tensor_tensor(out=gs[:, 0:64], in0=pg[:, 0:64], in1=pastneg[:, k, :], op=ALU.add), reads=[pg_r, pastneg_r], writes=[gs_r])
                fw.op("dve", lambda e: e.max(out=gs[:, 128:136], in_=gs[:, 0:64]), reads=[gs_r], writes=[gs_r])
                fw.op("dve", lambda e: e.tensor_scalar(out=gs[:, 136:137], in0=gs[:, 130:131], scalar1=-1e29, scalar2=None, op0=ALU.max), reads=[gs_r], writes=[gs_r])
                fw.op("dve", lambda e: e.scalar_tensor_tensor(out=gs[:, 64:128], in0=gs[:, 0:64], scalar=gs[:, 136:137], in1=pastind[:, k, :], op0=ALU.is_lt, op1=ALU.mult),
                      reads=[gs_r, pastind_r], writes=[gs_r])
                fw.op("dve", lambda e: e.tensor_scalar(out=sb[:, qh, :], in0=gs[:, 64:128], scalar1=NEG, scalar2=alq[:, qh, h:h + 1], op0=ALU.mult, op1=ALU.add),
                      reads=[gs_r, alq_r], writes=[sb_r])
            nblk = 8 * k + 8
            for n in range(nblk):
                for j in range(2):
                    j2 = 2 * n + j
                    ps, ps_r = psT.next()
                    fw.op("pe", lambda e: e.matmul(ps[:, 0:256], lhsT=kT[:, j2 * 128:(j2 + 1) * 128], rhs=qT[:, 256 * k:256 * k + 256], start=True, stop=False),
                          reads=[kT_r, qT_r], writes=[ps_r])
                    for qh in range(2):
                        if n < 8 * k:
                            fw.op("pe", lambda e: e.matmul(ps[:, qh * 128:(qh + 1) * 128], lhsT=sb[:, qh, n:n + 1].to_broadcast([128, 128]), rhs=ident[:], start=False, stop=(qh == 1)),
                                  reads=[sb_r, ident_r], writes=[ps_r])
                        else:
                            cp = n - 8 * k
                            bz, bz_r = bzs.next()
                            fw.op("dve", lambda e: e.tensor_scalar(out=bz[:], in0=zc[:, cp * 4 + qh * 2 + j, :], scalar1=sb[:, qh, n:n + 1], scalar2=None, op0=ALU.add),
                                  reads=[zc_r, sb_r], writes=[bz_r])
                            fw.op("pe", lambda e: e.matmul(ps[:, qh * 128:(qh + 1) * 128], lhsT=bz[:], rhs=ident[:], start=False, stop=(qh == 1)),
                                  reads=[bz_r, ident_r], writes=[ps_r])
                    p, p_r = pts.next()
                    mi = j2 - 16 * k + 112
                    fw.op("act", lambda e: e.activation(out=p[:], in_=ps[:, 0:256], func=AF.Exp, bias=kbm[:, h, mi:mi + 1]), reads=[ps_r, kbm_r], writes=[p_r])
                    first = (j2 == 0); last = (j2 == 2 * nblk - 1)
                    fw.op("pe", lambda e: e.matmul(psO[0][:, 0:256], lhsT=vh[:, j2, :], rhs=p[:], start=first, stop=last), reads=[vh_r, p_r], writes=[psO[1]])
                    fw.op("pe", lambda e: e.matmul(psD[0][:, 0:256], lhsT=ones[:], rhs=p[:], start=first, stop=last), reads=[ones_r, p_r], writes=[psD[1]])
            rc, rc_r = recs.next()
            fw.op("dve", lambda e: e.reciprocal(out=rc[:], in_=psD[0][:, 0:256]), reads=[psD[1]], writes=[rc_r])
            o, o_r = obs.next()
            fw.op("dve", lambda e: e.tensor_tensor(out=o[:], in0=psO[0][:, 0:256], in1=rc[:], op=ALU.mult), reads=[psO[1], rc_r], writes=[o_r])
            fw.dma(out=C.o_bT[h, :, 256 * k:256 * k + 256], in_=o[:], slot=o_r)
    fw.end_phase()


TT = 256


def declare_post_io(fw, C, ext):
    nl = C.nl
    declare(fw, C, "w_o_dsa", [nl, 1024, D], F32, ext)
    declare(fw, C, "w_o_moba", [nl, 1024, D], F32, ext)
    declare(fw, C, "w_out", [nl, D, D], F32, ext)
    declare(fw, C, "w_q_mem", [nl, D, 512], F32, ext)
    declare(fw, C, "w_kv_mem", [nl, D, 1024], F32, ext)
    declare(fw, C, "w_o_mem", [nl, 512, D], F32, ext)
    declare(fw, C, "w_ffn_in", [nl, D, 2 * DFF], F32, ext)
    declare(fw, C, "w_ffn_out", [nl, DFF, D], F32, ext)
    declare(fw, C, "d_lng", [nl, 128, 3, 16], F32, ext)
    declare(fw, C, "d_lnb", [nl, 128, 3, 16], F32, ext)
    declare(fw, C, "memT", [D, 256], F32, ext)
    declare(fw, C, "d_onesf", [128, 128], F32, ext)
    declare(fw, C, "xT_out", [D, TL], F32, ext)


def host_consts_post(inp, layers):
    out = {}
    nl = len(layers)
    out["d_lng"] = np.ascontiguousarray(inp["ln_g"][layers].reshape(nl, 3, 16, 128).transpose(0, 3, 1, 2)).astype(np.float32)
    out["d_lnb"] = np.ascontiguousarray(inp["ln_b"][layers].reshape(nl, 3, 16, 128).transpose(0, 3, 1, 2)).astype(np.float32)
    out["memT"] = np.ascontiguousarray(inp["mem"][0].T).astype(np.float32)
    out["d_onesf"] = np.full((128, 128), 1.0 / D, np.float32)
    return out


def phase_post(fw, C, L, x_in, x_out):
    fw.begin_phase()
    banks = mk_psum(fw, 8)
    psb = Rot(banks[0:4]); psM = banks[4]; psQ = banks[5]; psO = banks[6]; psD = banks[7]
    wst = mk_slots(fw, "wst2", [128, 44, 128], F32, 2)
    wbf = mk_slots(fw, "wbf2", [128, 44, 128], BF16, 2)
    xres = fw.sbuf("xres", [128, 16, TT], F32); xres_r = Res()
    xb = fw.sbuf("xb", [128, 16, TT], BF16); xb_r = Res()
    z = fw.sbuf("z", [128, 16, TT], F32); z_r = Res()
    zsq = fw.sbuf("zsq", [128, 16, TT], F32); zsq_r = Res()
    oa = fw.sbuf("oa", [128, 8, TT], BF16); oa_r = Res()
    obm = fw.sbuf("obm", [128, 8, TT], BF16); obm_r = Res()
    mT = fw.sbuf("mT", [128, 16, TT], BF16); mT_r = Res()
    aT = fw.sbuf("aT", [128, 44, TT], BF16); aT_r = Res()
    gts = mk_slots(fw, "gt", [128, 2, TT], F32, 2)
    t12 = mk_slots(fw, "t12", [128, 2, TT], F32, 2)
    sgs = mk_slots(fw, "sg", [128, TT], F32, 2)
    stat = fw.sbuf("stat", [128, 4, TT], F32); stat_r = Res()
    qmT = fw.sbuf("qmT", [128, 4, TT], BF16); qmT_r = Res()
    omT = fw.sbuf("omT", [128, 4, TT], BF16); omT_r = Res()
    pts = mk_slots(fw, "ptm", [128, TT], BF16, 2)
    recs = mk_slots(fw, "recm", [128, TT], F32, 2)
    kmemT = fw.sbuf("kmemT", [128, 4, 256], BF16); kmemT_r = Res()
    vmem = fw.sbuf("vmem", [128, 2, 512], BF16); vmem_r = Res()
    memst = z; memst_r = z_r
    memb = mT; memb_r = mT_r
    lng = fw.sbuf("lng", [128, 3, 16], F32); lng_r = Res()
    lnb = fw.sbuf("lnb", [128, 3, 16], F32); lnb_r = Res()
    onesf = fw.sbuf("onesf", [128, 128], F32); onesf_r = Res()
    ones = fw.sbuf("ones", [128, 128], BF16); ones_r = Res()
    fw.dma(out=lng[:], in_=C.d_lng[L], slot=lng_r)
    fw.dma(out=lnb[:], in_=C.d_lnb[L], slot=lnb_r)
    fw.dma(out=onesf[:], in_=C.d_onesf[:, :], slot=onesf_r)
    fw.dma(out=ones[:], in_=C.d_ones[:, :], slot=ones_r)
    fw.dma(out=memst[:], in_=C.memT.rearrange("(kc p) m -> p kc m", p=128), slot=memst_r)
    fw.op("pool", lambda e: e.tensor_copy(out=memb[:], in_=memst[:]), reads=[memst_r], writes=[memb_r])

    def slab(src_ap, K):
        st, st_r = wst.next(); wb, wb_r = wbf.next()
        fw.dma(out=st[:, 0:K, :], in_=src_ap.rearrange("(kc p) n -> p kc n", p=128), slot=st_r)
        fw.op("pool", lambda e: e.tensor_copy(out=wb[:, 0:K, :], in_=st[:, 0:K, :]), reads=[st_r], writes=[wb_r])
        return wb, wb_r

    def mm(wb, wb_r, K, rhs_t, rhs_r, n=TT):
        ps, ps_r = psb.next()
        for kc in range(K):
            fw.op("pe", lambda e: e.matmul(ps[:, 0:n], lhsT=wb[:, kc, :], rhs=rhs_t[:, kc, 0:n], start=(kc == 0), stop=(kc == K - 1)),
                  reads=[wb_r, rhs_r], writes=[ps_r])
        return ps, ps_r

    wkv = C.w_kv_mem[L]
    for hq in range(4):
        wb, wb_r = slab(wkv[:, hq * 128:(hq + 1) * 128], 16)
        ps, ps_r = mm(wb, wb_r, 16, memb, memb_r, 256)
        fw.op("act", lambda e: e.copy(out=kmemT[:, hq, :], in_=ps[:, 0:256]), reads=[ps_r], writes=[kmemT_r])
    for hq in range(4):
        wb, wb_r = slab(wkv[:, 512 + hq * 128:512 + (hq + 1) * 128], 16)
        for ms in range(2):
            ps, ps_r = psb.next()
            for kc in range(16):
                fw.op("pe", lambda e: e.matmul(ps[:, 0:128], lhsT=memb[:, kc, ms * 128:(ms + 1) * 128], rhs=wb[:, kc, :], start=(kc == 0), stop=(kc == 15)),
                      reads=[memb_r, wb_r], writes=[ps_r])
            fw.op("act", lambda e: e.copy(out=vmem[:, ms, hq * 128:(hq + 1) * 128], in_=ps[:, 0:128]), reads=[ps_r], writes=[vmem_r])

    def layer_norm(idx):
        fw.op("act", lambda e: e.activation(out=zsq[:], in_=z[:], func=AF.Square), reads=[z_r], writes=[zsq_r])
        for nb in range(16):
            fw.op("pe", lambda e: e.matmul(psM[0][:, 0:TT], lhsT=onesf[:], rhs=z[:, nb, :], start=(nb == 0), stop=(nb == 15)), reads=[onesf_r, z_r], writes=[psM[1]])
        for nb in range(16):
            fw.op("pe", lambda e: e.matmul(psQ[0][:, 0:TT], lhsT=onesf[:], rhs=zsq[:, nb, :], start=(nb == 0), stop=(nb == 15)), reads=[onesf_r, zsq_r], writes=[psQ[1]])
        fw.op("act", lambda e: e.copy(out=stat[:, 0, :], in_=psM[0][:, 0:TT]), reads=[psM[1]], writes=[stat_r])
        fw.op("dve", lambda e: e.tensor_tensor(out=stat[:, 1, :], in0=stat[:, 0, :], in1=stat[:, 0, :], op=ALU.mult), reads=[stat_r], writes=[stat_r])
        fw.op("dve", lambda e: e.tensor_tensor(out=stat[:, 2, :], in0=psQ[0][:, 0:TT], in1=stat[:, 1, :], op=ALU.subtract), reads=[psQ[1], stat_r], writes=[stat_r])
        fw.op("dve", lambda e: e.tensor_scalar(out=stat[:, 2, :], in0=stat[:, 2, :], scalar1=EPS, scalar2=None, op0=ALU.add), reads=[stat_r], writes=[stat_r])
        fw.op("act", lambda e: e.activation(out=stat[:, 1, :], in_=stat[:, 2, :], func=AF.Sqrt), reads=[stat_r], writes=[stat_r])
        fw.op("dve", lambda e: e.reciprocal(out=stat[:, 3, :], in_=stat[:, 1, :]), reads=[stat_r], writes=[stat_r])
        for nb in range(16):
            fw.op("dve", lambda e: e.tensor_tensor(out=z[:, nb, :], in0=z[:, nb, :], in1=stat[:, 0, :], op=ALU.subtract), reads=[z_r, stat_r], writes=[z_r])
            fw.op("dve", lambda e: e.tensor_tensor(out=z[:, nb, :], in0=z[:, nb, :], in1=stat[:, 3, :], op=ALU.mult), reads=[z_r, stat_r], writes=[z_r])
            fw.op("act", lambda e: e.activation(out=xres[:, nb, :], in_=z[:, nb, :], func=AF.Identity, scale=lng[:, idx, nb:nb + 1], bias=lnb[:, idx, nb:nb + 1]),
                  reads=[z_r, lng_r, lnb_r], writes=[xres_r])
        fw.op("pool", lambda e: e.tensor_copy(out=xb[:], in_=xres[:]), reads=[xres_r], writes=[xb_r])

    def resid(ps, ps_r, nb):
        fw.op("dve", lambda e: e.scalar_tensor_tensor(out=z[:, nb, :], in0=xres[:, nb, :], scalar=float(ALPHA), in1=ps[:, 0:TT], op0=ALU.mult, op1=ALU.add),
              reads=[xres_r, ps_r], writes=[z_r])

    for j in range(C.ntiles):
        t0 = j * TT
        fw.dma(out=xres[:], in_=x_in.rearrange("(kc p) t -> p kc t", p=128)[:, :, t0:t0 + TT], slot=xres_r)
        fw.dma(out=oa[:], in_=C.o_aT[:, :, t0:t0 + TT].rearrange("h p t -> p h t"), slot=oa_r)
        fw.dma(out=obm[:], in_=C.o_bT[:, :, t0:t0 + TT].rearrange("h p t -> p h t"), slot=obm_r)
        for fb in range(16):
            wa, wa_r = slab(C.w_o_dsa[L][:, fb * 128:(fb + 1) * 128], 8)
            psA, psA_r = mm(wa, wa_r, 8, oa, oa_r)
            wb_, wb_r = slab(C.w_o_moba[L][:, fb * 128:(fb + 1) * 128], 8)
            psB, psB_r = mm(wb_, wb_r, 8, obm, obm_r)
            gt, gt_r = gts.next()
            fw.dma(out=gt[:, 0, :], in_=C.g_aT[fb, :, t0:t0 + TT], slot=gt_r)
            fw.dma(out=gt[:, 1, :], in_=C.g_bT[fb, :, t0:t0 + TT], slot=gt_r)
            tt_, tt_r = t12.next()
            fw.op("dve", lambda e: e.tensor_tensor(out=tt_[:, 0, :], in0=psA[:, 0:TT], in1=gt[:, 0, :], op=ALU.mult), reads=[psA_r, gt_r], writes=[tt_r])
            fw.op("dve", lambda e: e.tensor_tensor(out=tt_[:, 1, :], in0=psB[:, 0:TT], in1=gt[:, 1, :], op=ALU.mult), reads=[psB_r, gt_r], writes=[tt_r])
            fw.op("dve", lambda e: e.tensor_tensor(out=mT[:, fb, :], in0=tt_[:, 0, :], in1=tt_[:, 1, :], op=ALU.add), reads=[tt_r], writes=[mT_r])
        for nb in range(16):
            wb, wb_r = slab(C.w_out[L][:, nb * 128:(nb + 1) * 128], 16)
            ps, ps_r = mm(wb, wb_r, 16, mT, mT_r)
            resid(ps, ps_r, nb)
        layer_norm(0)
        for hq in range(4):
            wb, wb_r = slab(C.w_q_mem[L][:, hq * 128:(hq + 1) * 128], 16)
            ps, ps_r = mm(wb, wb_r, 16, xb, xb_r)
            fw.op("act", lambda e: e.mul(out=qmT[:, hq, :], in_=ps[:, 0:TT], mul=float(QSCALE)), reads=[ps_r], writes=[qmT_r])
        for hq in range(4):
            for ms in range(2):
                ps, ps_r = psb.next()
                fw.op("pe", lambda e: e.matmul(ps[:, 0:TT], lhsT=kmemT[:, hq, ms * 128:(ms + 1) * 128], rhs=qmT[:, hq, :], start=True, stop=True), reads=[kmemT_r, qmT_r], writes=[ps_r])
                p, p_r = pts.next()
                fw.op("act", lambda e: e.activation(out=p[:], in_=ps[:, 0:TT], func=AF.Exp), reads=[ps_r], writes=[p_r])
                fw.op("pe", lambda e: e.matmul(psO[0][:, 0:TT], lhsT=vmem[:, ms, hq * 128:(hq + 1) * 128], rhs=p[:], start=(ms == 0), stop=(ms == 1)), reads=[vmem_r, p_r], writes=[psO[1]])
                fw.op("pe", lambda e: e.matmul(psD[0][:, 0:TT], lhsT=ones[:], rhs=p[:], start=(ms == 0), stop=(ms == 1)), reads=[ones_r, p_r], writes=[psD[1]])
            rc, rc_r = recs.next()
            fw.op("dve", lambda e: e.reciprocal(out=rc[:], in_=psD[0][:, 0:TT]), reads=[psD[1]], writes=[rc_r])
            fw.op("dve", lambda e: e.tensor_tensor(out=omT[:, hq, :], in0=psO[0][:, 0:TT], in1=rc[:], op=ALU.mult), reads=[psO[1], rc_r], writes=[omT_r])
        for nb in range(16):
            wb, wb_r = slab(C.w_o_mem[L][:, nb * 128:(nb + 1) * 128], 4)
            ps, ps_r = mm(wb, wb_r, 4, omT, omT_r)
            resid(ps, ps_r, nb)
        layer_norm(1)
        for i in range(44):
            wg, wg_r = slab(C.w_ffn_in[L][:, i * 128:(i + 1) * 128], 16)
            psg, psg_r = mm(wg, wg_r, 16, xb, xb_r)
            wu, wu_r = slab(C.w_ffn_in[L][:, DFF + i * 128:DFF + (i + 1) * 128], 16)
            psu, psu_r = mm(wu, wu_r, 16, xb, xb_r)
            sg, sg_r = sgs.next()
            fw.op("act", lambda e: e.activation(out=sg[:], in_=psg[:, 0:TT], func=AF.Silu), reads=[psg_r], writes=[sg_r])
            fw.op("dve", lambda e: e.tensor_tensor(out=aT[:, i, :], in0=psu[:, 0:TT], in1=sg[:], op=ALU.mult), reads=[psu_r, sg_r], writes=[aT_r])
        for nb in range(16):
            wb, wb_r = slab(C.w_ffn_out[L][:, nb * 128:(nb + 1) * 128], 44)
            ps, ps_r = mm(wb, wb_r, 44, aT, aT_r)
            resid(ps, ps_r, nb)
        layer_norm(2)
        fw.dma(out=x_out.rearrange("(kc p) t -> p kc t", p=128)[:, :, t0:t0 + TT], in_=xres[:], slot=xres_r)
    fw.end_phase()


from concourse.bass_utils import run_bass_kernel_spmd

ACT_SET = {
    "q_latT": ([2, 128, NH, TL], BF16), "q_idxT": ([8, 128, TL], BF16), "w_idx": ([TL, IH], F32),
    "k_idxT_own": ([ID_, TL], BF16), "c_kv_own": ([TL, KVL], BF16), "c_kvT_own": ([2, 128, TL], BF16),
    "q_mT": ([NH, 128, TL], BF16), "k_mT_own": ([NH, 128, TL], BF16), "k_meanT_own": ([128, NH, 8], F32),
    "v_m_own": ([TL, NH * HD], BF16), "g_aT": ([16, 128, TL], F32), "g_bT": ([16, 128, TL], F32),
}
KSIDE_OWN = ["k_idxT_own", "c_kv_own", "c_kvT_own", "k_mT_own", "k_meanT_own", "v_m_own"]
QSIDE = ["q_latT", "q_idxT", "w_idx", "q_mT", "g_aT", "g_bT"]
KSIDE_G = {"k_idxT_g": ([ID_, S], BF16), "c_kvT_g": ([2, 128, S], BF16), "c_kv_g": ([S, KVL], BF16),
           "k_mT_g": ([NH, 128, S], BF16), "v_m_g": ([S, NH * HD], BF16), "k_meanT_g": ([128, NH, 64], F32)}
PROJ_W = {"w_in": [1, D, INW], "w_uk": [1, NH, HD, KVL], "d_kvg": [1, 128, KVL], "d_kig": [1, 128, ID_], "d_kib": [1, 128, ID_], "d_bg": [1, 128, 2, 16]}
POST_W = {"w_uv": [1, NH, KVL, HD], "w_o_dsa": [1, 1024, D], "w_o_moba": [1, 1024, D], "w_out": [1, D, D], "w_q_mem": [1, D, 512], "w_kv_mem": [1, D, 1024],
          "w_o_mem": [1, 512, D], "w_ffn_in": [1, D, 2 * DFF], "w_ffn_out": [1, DFF, D], "d_lng": [1, 128, 3, 16], "d_lnb": [1, 128, 3, 16]}
CONSTS = {"d_ident": ([128, 128], BF16), "d_ones": ([128, 128], BF16), "d_onesf": ([128, 128], F32), "memT": ([D, 256], F32),
          "d_tloc": ([128, 16], F32), "d_iota": ([128, 2048], F32), "d_pow2": ([128, NIT], F32), "d_slopes": ([128, NH], F32), "d_kb": ([128, 128, NH], F32),
          "d_pastneg": ([128, 8, 64], F32), "d_pastind": ([128, 8, 64], F32), "d_alq": ([128, 2, NH], F32), "d_zc": ([128, 32, 128], BF16), "d_kbm": ([128, NH, 128], F32)}


def build_program(do_attn_post, do_proj, debug=False):
    nc = bass.Bass("TRN2", target_bir_lowering=False)
    ins, outs = [], []
    with contextlib.ExitStack() as es:
        fw = Fw(nc, es)
        C = Ctx(); C.cst = None; C.nl = 1
        C.nqb = 16; C.nslots = 8; C.nheads = NH; C.ntiles = TL // TT

        def dt(name, shape, dtype, kind):
            t = nc.dram_tensor(name, list(shape), dtype, kind=kind).ap()
            if kind == "ExternalInput":
                ins.append(name)
            elif kind == "ExternalOutput":
                outs.append(name)
            return t
        for n, (sh, d_) in CONSTS.items():
            setattr(C, n, dt(n, sh, d_, "ExternalInput"))
        xin = dt("xT_in", [D, TL], F32, "ExternalInput")
        if do_attn_post:
            for n, sh in POST_W.items():
                setattr(C, n, dt(n, sh, F32, "ExternalInput"))
            for n, (sh, d_) in KSIDE_G.items():
                setattr(C, n, dt(n, sh, d_, "ExternalInput"))
            for n in QSIDE:
                sh, d_ = ACT_SET[n]
                setattr(C, n, dt("in_" + n, sh, d_, "ExternalInput"))
            C.maskb = dt("maskb", [16, 128, S], BF16, "Internal")
            C.ddall = dt("ddall", [128, 16], F32, "Internal")
            C.o_aT = dt("o_aT", [NH, 128, TL], BF16, "ExternalOutput" if debug else "Internal")
            C.o_bT = dt("o_bT", [NH, 128, TL], BF16, "ExternalOutput" if debug else "Internal")
            xout = dt("xT_out", [D, TL], F32, "ExternalOutput")
            phase_dsa_select(fw, C, 0)
            phase_dsa_attn(fw, C, 0)
            phase_moba(fw, C, 0)
            phase_post(fw, C, 0, xin, xout)
        else:
            xout = xin
        if do_proj:
            for n, sh in PROJ_W.items():
                setattr(C, n, dt(n, sh, F32, "ExternalInput"))
            for n, (sh, d_) in ACT_SET.items():
                setattr(C, n, dt("out_" + n, sh, d_, "ExternalOutput"))
            C.xT_res = xout
            phase_proj(fw, C, 0)
    return nc, ins, outs


def core_consts(c):
    d = {}
    d.update(host_consts_attn(c))
    d.update(host_consts_moba(c))
    d["d_ident"] = np.eye(128, dtype=np.float32).astype(NPBF)
    d["d_onesf"] = np.full((128, 128), 1.0 / D, np.float32)
    return d


def gather_kside(outs_per_core):
    g = {n: np.zeros(sh, NPBF if d_ == BF16 else np.float32) for n, (sh, d_) in KSIDE_G.items()}
    for c in range(NCORE):
        tok = own_tokens(c)
        o = outs_per_core[c]
        g["k_idxT_g"][:, tok] = o["out_k_idxT_own"]
        g["c_kvT_g"][:, :, tok] = o["out_c_kvT_own"]
        g["c_kv_g"][tok, :] = o["out_c_kv_own"]
        g["k_mT_g"][:, :, tok] = o["out_k_mT_own"]
        g["v_m_g"][tok, :] = o["out_v_m_own"]
        for k in range(8):
            g["k_meanT_g"][:, :, 8 * k + c] = o["out_k_meanT_own"][:, :, k]
    return g


def kernel(**inp):
    x = np.asarray(inp["x"], np.float32)[0]
    consts = [core_consts(c) for c in range(NCORE)]
    memT = np.ascontiguousarray(np.asarray(inp["mem"], np.float32)[0].T)

    def proj_w(l):
        d = {"w_in": inp["w_in"][l:l + 1], "w_uk": inp["w_uk"][l:l + 1]}
        d.update({k: v for k, v in host_consts_proj(inp, [l]).items() if k != "d_ident"})
        return d

    def post_w(l):
        d = {n: inp[n][l:l + 1] for n in ["w_uv", "w_o_dsa", "w_o_moba", "w_out", "w_q_mem", "w_kv_mem", "w_o_mem", "w_ffn_in", "w_ffn_out"]}
        hp = host_consts_post(inp, [l])
        d["d_lng"] = hp["d_lng"]; d["d_lnb"] = hp["d_lnb"]
        return d

    def run(nc, ins, maps):
        maps = [{k: np.ascontiguousarray(m[k]) for k in ins} for m in maps]
        return run_bass_kernel_spmd(nc, maps, core_ids=list(range(NCORE))).results

    xT = [np.ascontiguousarray(x[own_tokens(c)].T) for c in range(NCORE)]
    ncP, insP, _ = build_program(False, True)
    maps = []
    for c in range(NCORE):
        m = dict(consts[c]); m["memT"] = memT; m["xT_in"] = xT[c]; m.update(proj_w(0)); maps.append(m)
    res = run(ncP, insP, maps)
    ncM = None
    for l in range(DEPTH):
        last = (l == DEPTH - 1)
        if last:
            ncX, insX, _ = build_program(True, False)
        else:
            if ncM is None:
                ncM = build_program(True, True)
            ncX, insX, _ = ncM
        kg = gather_kside(res)
        maps = []
        for c in range(NCORE):
            m = dict(consts[c]); m["memT"] = memT; m["xT_in"] = xT[c]
            m.update(kg)
            for n in QSIDE:
                m["in_" + n] = res[c]["out_" + n]
            m.update(post_w(l))
            if not last:
                m.update(proj_w(l + 1))
            maps.append(m)
        res = run(ncX, insX, maps)
        xT = [np.asarray(res[c]["xT_out"]) for c in range(NCORE)]
    out = np.zeros((1, S, D), np.float32)
    for c in range(NCORE):
        out[0, own_tokens(c), :] = xT[c].T
    return out
```

```python
import contextlib
import numpy as np
import concourse.bass as bass
import concourse.mybir as mybir

F32 = mybir.dt.float32
BF16 = mybir.dt.bfloat16
AF = mybir.ActivationFunctionType
ALU = mybir.AluOpType
AX = mybir.AxisListType


class Eng:
    def __init__(self, name, h, sem, is_pe=False):
        self.name = name
        self.h = h
        self.sem = sem
        self.count = 0
        self.seen = {}
        self.is_pe = is_pe


class Res:
    __slots__ = ("name", "last_w", "readers", "sem", "semval")

    def __init__(self, name=""):
        self.name = name
        self.last_w = None
        self.readers = {}
        self.sem = None
        self.semval = 0


class Fw:
    def __init__(self, nc, es):
        self.nc = nc
        self.es = es
        self.engs = {}
        for name, h, pe in [("pe", nc.tensor, True), ("act", nc.scalar, False),
                            ("dve", nc.vector, False), ("pool", nc.gpsimd, False),
                            ("sp", nc.sync, False)]:
            sem = es.enter_context(nc.semaphore("sem_" + name))
            self.engs[name] = Eng(name, h, sem, pe)
        self.nsem = 5
        self.dpool = []
        self.dnext = 0
        self.phase_slots = []
        self.pes = None

    def begin_phase(self):
        self.pes = contextlib.ExitStack()
        self.pes.__enter__()
        self.dnext = 0
        self.phase_slots = []

    def end_phase(self):
        self.barrier()
        self.pes.__exit__(None, None, None)
        self.pes = None

    def barrier(self):
        sp = self.engs["sp"]
        for r in self.phase_slots:
            if r.last_w is not None and r.last_w[0] == "d":
                self._wait(sp, r.last_w)
            for t in r.readers.values():
                if t[0] == "d":
                    self._wait(sp, t)
        for e2 in self.engs.values():
            if e2 is not sp and e2.count > 0:
                self._wait(sp, ("e", e2, e2.count))
        inst = sp.h.nop()
        sp.count += 1
        inst.then_inc(sp.sem, 1)
        for e2 in self.engs.values():
            if e2 is not sp:
                self._wait(e2, ("e", sp, sp.count))

    def sbuf(self, name, shape, dtype):
        self.uid = getattr(self, "uid", 0) + 1
        t = self.pes.enter_context(self.nc.sbuf_tensor("%s_u%d" % (name, self.uid), list(shape), dtype))
        return t

    def psum(self, name, shape, dtype):
        self.uid = getattr(self, "uid", 0) + 1
        return self.pes.enter_context(self.nc.psum_tensor("%s_u%d" % (name, self.uid), list(shape), dtype))

    def dram(self, name, shape, dtype, kind="Internal"):
        return self.nc.dram_tensor(name, list(shape), dtype, kind=kind).ap()

    def _dma_sem(self, r):
        if r.sem is None:
            if self.dnext >= len(self.dpool):
                h = self.es.enter_context(self.nc.semaphore("dsem_%d" % self.nsem))
                self.nsem += 1
                self.dpool.append([h, 0])
            r.sem = self.dpool[self.dnext]
            self.dnext += 1
            self.phase_slots.append(r)
        return r.sem

    def _wait(self, eng, tok):
        kind = tok[0]
        if kind == "e":
            _, e2, n = tok
            if e2 is eng and eng.is_pe:
                return
            if eng.seen.get(e2.name, 0) >= n:
                return
            eng.h.wait_ge(e2.sem, n)
            eng.seen[e2.name] = n
        else:
            _, r, v = tok
            key = ("d", id(r.sem))
            if eng.seen.get(key, 0) >= v:
                return
            eng.h.wait_ge(r.sem[0], v)
            eng.seen[key] = v

    def _deps(self, eng, reads, writes):
        for r in reads:
            if r.last_w is not None:
                self._wait(eng, r.last_w)
        for w in writes:
            if w.last_w is not None:
                self._wait(eng, w.last_w)
            for t in w.readers.values():
                self._wait(eng, t)

    def _commit(self, tok, reads, writes):
        key = tok[1].name if tok[0] == "e" else ("d", id(tok[1]))
        for r in reads:
            r.readers[key] = tok
        for w in writes:
            w.last_w = tok
            w.readers = {}

    def op(self, engname, fn, reads=(), writes=()):
        eng = self.engs[engname]
        self._deps(eng, reads, writes)
        inst = fn(eng.h)
        eng.count += 1
        inst.then_inc(eng.sem, 1)
        tok = ("e", eng, eng.count)
        self._commit(tok, reads, writes)
        return inst

    def dma(self, out, in_, slot, reads=(), writes=(), q="sp", **kw):
        eng = self.engs[q]
        writes = list(writes)
        if slot not in writes:
            writes.append(slot)
        reads = [r for r in reads if r is not slot]
        self._deps(eng, reads, writes)
        sem = self._dma_sem(slot)
        inst = eng.h.dma_start(out=out, in_=in_, **kw)
        sem[1] += 16
        inst.then_inc(sem[0], 16)
        tok = ("d", slot, sem[1])
        self._commit(tok, reads, writes)
        return tok

    def wait_all(self, q="sp"):
        eng = self.engs[q]
        for e2 in self.engs.values():
            if e2.count > 0 and e2 is not eng:
                self._wait(eng, ("e", e2, e2.count))

    def wait_tok(self, q, tok):
        self._wait(self.engs[q], tok)


D = 2048
S = 16384
NCORE = 8
TL = S // NCORE
DEPTH = 4
HD = 128
NH = 8
KVL = 256
IH = 16
ID_ = 64
DFF = 5632
INW = 9552
ALPHA = (2.0 * DEPTH) ** 0.25
EPS = 1e-5
QSCALE = HD ** -0.5
NEG = -30000.0
BIG = 1.0e30
O_QD, O_CKV, O_QI, O_KI, O_WI, O_QM, O_KM, O_VM, O_GA, O_GB = 0, 1024, 1280, 2304, 2368, 2384, 3408, 4432, 5456, 7504


class Ctx:
    pass


def mk_psum(fw, n=8):
    banks = []
    for i in range(n):
        t = fw.psum("ps%d" % i, [128, 512], F32)
        banks.append((t, Res("ps%d" % i)))
    return banks


class Rot:
    def __init__(self, items):
        self.items = items
        self.i = 0

    def next(self):
        it = self.items[self.i % len(self.items)]
        self.i += 1
        return it


def mk_slots(fw, name, shape, dtype, n):
    return Rot([(fw.sbuf("%s%d" % (name, i), shape, dtype), Res("%s%d" % (name, i))) for i in range(n)])


def load_weight_slab(fw, C, src_ap, K, ncols):
    kc = K
    st, st_r = C.wst.next()
    wb, wb_r = C.wbf.next()
    fw.dma(out=st[:, 0:kc, 0:ncols], in_=src_ap.rearrange("(kc p) n -> p kc n", p=128), slot=st_r)
    fw.op("pool", lambda e: e.tensor_copy(out=wb[:, 0:kc, 0:ncols], in_=st[:, 0:kc, 0:ncols]),
          reads=[st_r], writes=[wb_r])
    return wb, wb_r


def phase_proj(fw, C, L):
    nc = fw.nc
    fw.begin_phase()
    banks = mk_psum(fw, 7)
    psb = Rot(banks)
    pst = fw.psum("pst", [128, 1024], BF16)
    pst_r = Res("pst")
    xb = fw.sbuf("xb", [128, 16, TL], BF16)
    xb_r = Res("xb")
    C.wst = mk_slots(fw, "wst", [128, 16, 256], F32, 2)
    C.wbf = mk_slots(fw, "wbf", [128, 16, 256], BF16, 2)
    xst = mk_slots(fw, "xst", [128, 16, 256], F32, 2)
    ob = mk_slots(fw, "ob", [128, 512], BF16, 4)
    of = mk_slots(fw, "of", [128, 512], F32, 3)
    qd = mk_slots(fw, "qd", [128, 512], BF16, 2)
    small = mk_slots(fw, "small", [128, 16], F32, 4)
    junk = fw.sbuf("junk", [128, 256], BF16); junk_r = Res("junk")
    tmpf = mk_slots(fw, "tmpf", [128, 256], F32, 2)
    wuk_st = fw.sbuf("wuk_st", [128, 8, 256], F32); wuk_st_r = Res()
    wuk = fw.sbuf("wuk", [128, 8, 256], BF16); wuk_r = Res()
    kmean = fw.sbuf("kmean", [128, 8, 8], F32); kmean_r = Res()
    cst = C.cst
    ident = fw.sbuf("ident", [128, 128], BF16); ident_r = Res()
    kvg = fw.sbuf("kvg", [128, 256], F32); kvg_r = Res()
    kig = fw.sbuf("kig", [128, 64], F32); kig_r = Res()
    kib = fw.sbuf("kib", [128, 64], F32); kib_r = Res()
    bg = fw.sbuf("bg", [128, 2, 16], F32); bg_r = Res()
    fw.dma(out=ident[:], in_=C.d_ident[:, :], slot=ident_r)
    fw.dma(out=kvg[:], in_=C.d_kvg[L], slot=kvg_r)
    fw.dma(out=kig[:], in_=C.d_kig[L], slot=kig_r)
    fw.dma(out=kib[:], in_=C.d_kib[L], slot=kib_r)
    fw.dma(out=bg[:], in_=C.d_bg[L], slot=bg_r)
    fw.dma(out=wuk_st[:], in_=C.w_uk[L].rearrange("h d c -> d h c"), slot=wuk_st_r)
    fw.op("pool", lambda e: e.tensor_copy(out=wuk[:], in_=wuk_st[:]), reads=[wuk_st_r], writes=[wuk_r])

    for j in range(TL // 256):
        st, st_r = xst.next()
        fw.dma(out=st[:], in_=C.xT_res.rearrange("(kc p) t -> p kc t", p=128)[:, :, j * 256:(j + 1) * 256], slot=st_r)
        eng = "dve" if j % 2 == 0 else "act"
        if eng == "dve":
            fw.op("dve", lambda e: e.tensor_copy(out=xb[:, :, j * 256:(j + 1) * 256], in_=st[:]), reads=[st_r], writes=[xb_r])
        else:
            fw.op("act", lambda e: e.copy(out=xb[:, :, j * 256:(j + 1) * 256], in_=st[:]), reads=[st_r], writes=[xb_r])

    w_in = C.w_in[L]
    evac_i = [0]

    def evac_copy(dst, dst_r, src, src_r, scale=None):
        i = evac_i[0]; evac_i[0] += 1
        if i % 2 == 0:
            if scale is None:
                fw.op("act", lambda e: e.copy(out=dst, in_=src), reads=[src_r], writes=[dst_r])
            else:
                fw.op("act", lambda e: e.mul(out=dst, in_=src, mul=float(scale)), reads=[src_r], writes=[dst_r])
        else:
            if scale is None:
                fw.op("dve", lambda e: e.tensor_copy(out=dst, in_=src), reads=[src_r], writes=[dst_r])
            else:
                fw.op("dve", lambda e: e.tensor_scalar(out=dst, in0=src, scalar1=float(scale), scalar2=None, op0=ALU.mult),
                      reads=[src_r], writes=[dst_r])

    def fm_matmul(wb, wb_r, blk, j):
        ps, ps_r = psb.next()
        for kc in range(16):
            fw.op("pe", lambda e: e.matmul(ps[:], lhsT=wb[:, kc, blk * 128:(blk + 1) * 128], rhs=xb[:, kc, j * 512:(j + 1) * 512],
                                           start=(kc == 0), stop=(kc == 15)),
                  reads=[wb_r, xb_r], writes=[ps_r])
        return ps, ps_r

    def tm_matmul(wb, wb_r, ncols, ts):
        ps, ps_r = psb.next()
        for kc in range(16):
            fw.op("pe", lambda e: e.matmul(ps[:, 0:ncols], lhsT=xb[:, kc, ts * 128:(ts + 1) * 128], rhs=wb[:, kc, 0:ncols],
                                           start=(kc == 0), stop=(kc == 15)),
                  reads=[wb_r, xb_r], writes=[ps_r])
        return ps, ps_r

    def sec0():
        for sl in range(4):
            wb, wb_r = load_weight_slab(fw, C, w_in[:, O_QD + sl * 256:O_QD + (sl + 1) * 256], 16, 256)
            for j in range(4):
                for blk in range(2):
                    h = sl * 2 + blk
                    ps, ps_r = fm_matmul(wb, wb_r, blk, j)
                    q, q_r = qd.next()
                    evac_copy(q[:], q_r, ps[:], ps_r)
                    for cc in range(2):
                        ps2, ps2_r = psb.next()
                        fw.op("pe", lambda e: e.matmul(ps2[:], lhsT=wuk[:, h, cc * 128:(cc + 1) * 128], rhs=q[:], start=True, stop=True),
                              reads=[wuk_r, q_r], writes=[ps2_r])
                        o, o_r = ob.next()
                        evac_copy(o[:], o_r, ps2[:], ps2_r, scale=QSCALE)
                        fw.dma(out=C.q_latT[cc, :, h, j * 512:(j + 1) * 512], in_=o[:], slot=o_r)

    if getattr(C, 'stop', 99) >= 0:
        sec0()
    def sec1():
        wb, wb_r = load_weight_slab(fw, C, w_in[:, O_CKV:O_CKV + 256], 16, 256)
        for ts in range(16):
            ps, ps_r = tm_matmul(wb, wb_r, 256, ts)
            sm, sm_r = small.next()
            fw.op("act", lambda e: e.activation(out=junk[:], in_=ps[:, 0:256], func=AF.Square, accum_out=sm[:, 0:1]),
                  reads=[ps_r], writes=[junk_r, sm_r])
            fw.op("dve", lambda e: e.tensor_scalar(out=sm[:, 1:2], in0=sm[:, 0:1], scalar1=1.0 / 256, scalar2=EPS, op0=ALU.mult, op1=ALU.add),
                  reads=[sm_r], writes=[sm_r])
            fw.op("act", lambda e: e.activation(out=sm[:, 2:3], in_=sm[:, 1:2], func=AF.Sqrt), reads=[sm_r], writes=[sm_r])
            fw.op("dve", lambda e: e.reciprocal(out=sm[:, 3:4], in_=sm[:, 2:3]), reads=[sm_r], writes=[sm_r])
            tf, tf_r = tmpf.next()
            fw.op("dve", lambda e: e.tensor_scalar(out=tf[:], in0=ps[:, 0:256], scalar1=sm[:, 3:4], scalar2=None, op0=ALU.mult),
                  reads=[ps_r, sm_r], writes=[tf_r])
            o, o_r = ob.next()
            fw.op("dve", lambda e: e.tensor_tensor(out=o[:, 0:256], in0=tf[:], in1=kvg[:], op=ALU.mult), reads=[tf_r, kvg_r], writes=[o_r])
            for cc in range(2):
                fw.op("pe", lambda e: e.transpose(pst[:, cc * 128:(cc + 1) * 128], o[:, cc * 128:(cc + 1) * 128], ident[:]),
                      reads=[o_r, ident_r], writes=[pst_r])
            fw.op("act", lambda e: e.copy(out=o[:, 256:512], in_=pst[:, 0:256]), reads=[pst_r], writes=[o_r])
            fw.dma(out=C.c_kv_own[ts * 128:(ts + 1) * 128, :], in_=o[:, 0:256], slot=o_r)
            fw.dma(out=C.c_kvT_own[:, :, ts * 128:(ts + 1) * 128].rearrange("cc p t -> p cc t"),
                   in_=o[:, 256:512].rearrange("p (cc t) -> p cc t", cc=2), slot=o_r)

    if getattr(C, 'stop', 99) >= 1:
        sec1()
    def sec2():
        for sl in range(4):
            wb, wb_r = load_weight_slab(fw, C, w_in[:, O_QI + sl * 256:O_QI + (sl + 1) * 256], 16, 256)
            for j in range(4):
                for blk in range(2):
                    ps, ps_r = fm_matmul(wb, wb_r, blk, j)
                    o, o_r = ob.next()
                    evac_copy(o[:], o_r, ps[:], ps_r)
                    fw.dma(out=C.q_idxT[sl * 2 + blk, :, j * 512:(j + 1) * 512], in_=o[:], slot=o_r)

    if getattr(C, 'stop', 99) >= 2:
        sec2()
    def sec3():
        wb, wb_r = load_weight_slab(fw, C, w_in[:, O_KI:O_KI + 80], 16, 80)
        for ts in range(16):
            ps, ps_r = tm_matmul(wb, wb_r, 80, ts)
            sm, sm_r = small.next()
            fw.op("dve", lambda e: e.bn_stats(out=sm[:, 0:6], in_=ps[:, 0:64]), reads=[ps_r], writes=[sm_r])
            fw.op("dve", lambda e: e.bn_aggr(out=sm[:, 6:8], in_=sm[:, 0:6]), reads=[sm_r], writes=[sm_r])
            fw.op("dve", lambda e: e.tensor_scalar(out=sm[:, 8:9], in0=sm[:, 7:8], scalar1=EPS, scalar2=None, op0=ALU.add), reads=[sm_r], writes=[sm_r])
            fw.op("act", lambda e: e.activation(out=sm[:, 9:10], in_=sm[:, 8:9], func=AF.Sqrt), reads=[sm_r], writes=[sm_r])
            fw.op("dve", lambda e: e.reciprocal(out=sm[:, 10:11], in_=sm[:, 9:10]), reads=[sm_r], writes=[sm_r])
            tf, tf_r = tmpf.next()
            fw.op("dve", lambda e: e.tensor_scalar(out=tf[:, 0:64], in0=ps[:, 0:64], scalar1=sm[:, 6:7], scalar2=sm[:, 10:11],
                                                   op0=ALU.subtract, op1=ALU.mult), reads=[ps_r, sm_r], writes=[tf_r])
            fw.op("dve", lambda e: e.tensor_tensor(out=tf[:, 64:128], in0=tf[:, 0:64], in1=kig[:], op=ALU.mult), reads=[tf_r, kig_r], writes=[tf_r])
            o, o_r = ob.next()
            fw.op("dve", lambda e: e.tensor_tensor(out=o[:, 0:64], in0=tf[:, 64:128], in1=kib[:], op=ALU.add), reads=[tf_r, kib_r], writes=[o_r])
            fw.op("pe", lambda e: e.transpose(pst[0:64, 0:128], o[:, 0:64], ident[:]), reads=[o_r, ident_r], writes=[pst_r])
            fw.op("act", lambda e: e.copy(out=o[0:64, 128:256], in_=pst[0:64, 0:128]), reads=[pst_r], writes=[o_r])
            fw.dma(out=C.k_idxT_own[:, ts * 128:(ts + 1) * 128], in_=o[0:64, 128:256], slot=o_r)
            f, f_r = of.next()
            fw.op("act", lambda e: e.mul(out=f[:, 0:16], in_=ps[:, 64:80], mul=1.0 / 32), reads=[ps_r], writes=[f_r])
            fw.dma(out=C.w_idx[ts * 128:(ts + 1) * 128, :], in_=f[:, 0:16], slot=f_r)

    if getattr(C, 'stop', 99) >= 3:
        sec3()
    def sec4():
        for sl in range(4):
            wb, wb_r = load_weight_slab(fw, C, w_in[:, O_QM + sl * 256:O_QM + (sl + 1) * 256], 16, 256)
            for j in range(4):
                for blk in range(2):
                    ps, ps_r = fm_matmul(wb, wb_r, blk, j)
                    o, o_r = ob.next()
                    evac_copy(o[:], o_r, ps[:], ps_r, scale=QSCALE)
                    fw.dma(out=C.q_mT[sl * 2 + blk, :, j * 512:(j + 1) * 512], in_=o[:], slot=o_r)
        for sl in range(4):
            wb, wb_r = load_weight_slab(fw, C, w_in[:, O_KM + sl * 256:O_KM + (sl + 1) * 256], 16, 256)
            for j in range(4):
                for blk in range(2):
                    h = sl * 2 + blk
                    ps, ps_r = fm_matmul(wb, wb_r, blk, j)
                    o, o_r = ob.next()
                    for s2 in range(2):
                        fw.op("act", lambda e: e.activation(out=o[:, s2 * 256:(s2 + 1) * 256], in_=ps[:, s2 * 256:(s2 + 1) * 256], func=AF.Copy,
                                                            accum_out=kmean[:, h, 2 * j + s2:2 * j + s2 + 1]),
                              reads=[ps_r], writes=[o_r, kmean_r])
                    fw.dma(out=C.k_mT_own[h, :, j * 512:(j + 1) * 512], in_=o[:], slot=o_r)
        fw.op("dve", lambda e: e.tensor_scalar(out=kmean[:], in0=kmean[:], scalar1=1.0 / 256, scalar2=None, op0=ALU.mult), reads=[kmean_r], writes=[kmean_r])
        fw.dma(out=C.k_meanT_own[:, :, :], in_=kmean[:], slot=kmean_r)

    if getattr(C, 'stop', 99) >= 4:
        sec4()
    def sec5():
        for sl in range(4):
            wb, wb_r = load_weight_slab(fw, C, w_in[:, O_VM + sl * 256:O_VM + (sl + 1) * 256], 16, 256)
            for ts in range(16):
                ps, ps_r = tm_matmul(wb, wb_r, 256, ts)
                o, o_r = ob.next()
                evac_copy(o[:, 0:256], o_r, ps[:, 0:256], ps_r)
                fw.dma(out=C.v_m_own[ts * 128:(ts + 1) * 128, sl * 256:(sl + 1) * 256], in_=o[:, 0:256], slot=o_r)

    if getattr(C, 'stop', 99) >= 5:
        sec5()
    def sec6():
        for gi, (off, dst) in enumerate([(O_GA, C.g_aT), (O_GB, C.g_bT)]):
            for sl in range(8):
                wb, wb_r = load_weight_slab(fw, C, w_in[:, off + sl * 256:off + (sl + 1) * 256], 16, 256)
                for j in range(4):
                    for blk in range(2):
                        fb = sl * 2 + blk
                        ps, ps_r = fm_matmul(wb, wb_r, blk, j)
                        f, f_r = of.next()
                        fw.op("act", lambda e: e.activation(out=f[:], in_=ps[:], func=AF.Sigmoid, bias=bg[:, gi, fb:fb + 1]),
                              reads=[ps_r, bg_r], writes=[f_r])
                        fw.dma(out=dst[fb, :, j * 512:(j + 1) * 512], in_=f[:], slot=f_r)

    if getattr(C, 'stop', 99) >= 6:
        sec6()
    fw.end_phase()


import ml_dtypes
NPBF = ml_dtypes.bfloat16


def own_tokens(c):
    return np.concatenate([np.arange(256 * (8 * k + c), 256 * (8 * k + c) + 256) for k in range(8)])


def declare(fw, C, name, shape, dtype, ext):
    kind = "Internal"
    if name in ext:
        kind = ext[name]
    t = fw.nc.dram_tensor(name, list(shape), dtype, kind=kind).ap()
    setattr(C, name, t)
    return t


def declare_proj_io(fw, C, nl, ext):
    declare(fw, C, "xT_res", [D, TL], F32, ext)
    declare(fw, C, "w_in", [nl, D, INW], F32, ext)
    declare(fw, C, "w_uk", [nl, NH, HD, KVL], F32, ext)
    declare(fw, C, "d_ident", [128, 128], BF16, ext)
    declare(fw, C, "d_kvg", [nl, 128, KVL], F32, ext)
    declare(fw, C, "d_kig", [nl, 128, ID_], F32, ext)
    declare(fw, C, "d_kib", [nl, 128, ID_], F32, ext)
    declare(fw, C, "d_bg", [nl, 128, 2, 16], F32, ext)
    declare(fw, C, "q_latT", [2, 128, NH, TL], BF16, ext)
    declare(fw, C, "q_idxT", [8, 128, TL], BF16, ext)
    declare(fw, C, "w_idx", [TL, IH], F32, ext)
    declare(fw, C, "k_idxT_own", [ID_, TL], BF16, ext)
    declare(fw, C, "c_kv_own", [TL, KVL], BF16, ext)
    declare(fw, C, "c_kvT_own", [2, 128, TL], BF16, ext)
    declare(fw, C, "q_mT", [NH, 128, TL], BF16, ext)
    declare(fw, C, "k_mT_own", [NH, 128, TL], BF16, ext)
    declare(fw, C, "k_meanT_own", [128, NH, 8], F32, ext)
    declare(fw, C, "v_m_own", [TL, NH * HD], BF16, ext)
    declare(fw, C, "g_aT", [16, 128, TL], F32, ext)
    declare(fw, C, "g_bT", [16, 128, TL], F32, ext)


def host_consts_proj(inp, layers):
    out = {}
    out["d_ident"] = np.eye(128, dtype=np.float32).astype(NPBF)
    out["d_kvg"] = np.ascontiguousarray(np.broadcast_to(inp["kv_norm_g"][layers][:, None, :], (len(layers), 128, KVL))).astype(np.float32)
    out["d_kig"] = np.ascontiguousarray(np.broadcast_to(inp["idx_k_norm_g"][layers][:, None, :], (len(layers), 128, ID_))).astype(np.float32)
    out["d_kib"] = np.ascontiguousarray(np.broadcast_to(inp["idx_k_norm_b"][layers][:, None, :], (len(layers), 128, ID_))).astype(np.float32)
    bg = inp["b_gate"][layers]
    out["d_bg"] = np.ascontiguousarray(bg.reshape(len(layers), 2, 16, 128).transpose(0, 3, 1, 2)).astype(np.float32)
    return out


NIT = 22


def declare_attn_io(fw, C, ext):
    declare(fw, C, "k_idxT_g", [ID_, S], BF16, ext)
    declare(fw, C, "c_kvT_g", [2, 128, S], BF16, ext)
    declare(fw, C, "c_kv_g", [S, KVL], BF16, ext)
    declare(fw, C, "k_mT_g", [NH, 128, S], BF16, ext)
    declare(fw, C, "v_m_g", [S, NH * HD], BF16, ext)
    declare(fw, C, "k_meanT_g", [128, NH, 64], F32, ext)
    declare(fw, C, "maskb", [16, 128, S], BF16, ext)
    declare(fw, C, "ddall", [128, 16], F32, ext)
    declare(fw, C, "o_aT", [NH, 128, TL], BF16, ext)
    declare(fw, C, "o_bT", [NH, 128, TL], BF16, ext)
    declare(fw, C, "d_tloc", [128, 16], F32, ext)
    declare(fw, C, "d_iota", [128, 2048], F32, ext)
    declare(fw, C, "d_pow2", [128, NIT], F32, ext)
    declare(fw, C, "d_slopes", [128, NH], F32, ext)
    declare(fw, C, "d_kb", [128, 128, NH], F32, ext)
    declare(fw, C, "d_ones", [128, 128], BF16, ext)
    declare(fw, C, "w_uv", [C.nl, NH, KVL, HD], F32, ext)


def host_consts_attn(c):
    out = {}
    tl = np.zeros((128, 16), np.float32)
    for qb in range(16):
        tl[:, qb] = 256 * c + 128 * (qb % 2) + np.arange(128)
    out["d_tloc"] = tl
    out["d_iota"] = np.ascontiguousarray(np.broadcast_to(np.arange(2048, dtype=np.float32)[None, :], (128, 2048)))
    p2 = np.array([2.0 ** (-i) for i in range(NIT)], np.float32); p2[0] = 1.0 + 1e-6
    out["d_pow2"] = np.ascontiguousarray(np.broadcast_to(p2[None, :], (128, NIT)))
    sl = np.array([2.0 ** (-(i + 1)) for i in range(NH)], np.float32)
    out["d_slopes"] = np.ascontiguousarray(np.broadcast_to(sl[None, :], (128, NH)))
    pos = (128 * np.arange(128)[None, :, None] + np.arange(128)[:, None, None]).astype(np.float32)
    out["d_kb"] = np.ascontiguousarray(pos * sl[None, None, :]).astype(np.float32)
    out["d_ones"] = np.ones((128, 128), np.float32).astype(NPBF)
    return out


def phase_dsa_select(fw, C, L):
    fw.begin_phase()
    banks = mk_psum(fw, 8)
    psL = Rot(banks[0:5]); psS = Rot(banks[5:8])
    kid = fw.sbuf("kid", [128, S], BF16); kid_r = Res()
    score = fw.sbuf("score", [128, S], F32); score_r = Res()
    mb = fw.sbuf("mb", [128, S], BF16); mb_r = Res()
    Rt = mk_slots(fw, "Rt", [128, 16, 256], BF16, 3)
    dg = mk_slots(fw, "dg", [128, 16, 128], BF16, 2)
    qi = mk_slots(fw, "qi", [128, 8, 128], BF16, 2)
    wi = mk_slots(fw, "wi", [128, 16], F32, 2)
    iota = fw.sbuf("iota", [128, 2048], F32); iota_r = Res()
    tmpz = fw.sbuf("tmpz", [128, 2048], F32); tmpz_r = Res()
    ident = fw.sbuf("ident", [128, 128], BF16); ident_r = Res()
    tloc = fw.sbuf("tloc", [128, 16], F32); tloc_r = Res()
    pow2 = fw.sbuf("pow2", [128, NIT], F32); pow2_r = Res()
    ddall = fw.sbuf("ddall", [128, 16], F32); ddall_r = Res()
    a32 = fw.sbuf("a32", [128, 512], F32); a32_r = Res()
    sm = mk_slots(fw, "sm", [128, 8 + NIT], F32, 2)
    fw.dma(out=kid[0:64, :], in_=C.k_idxT_g[:, :], slot=kid_r)
    kid2_r = Res()
    fw.dma(out=kid[64:128, :], in_=C.k_idxT_g[:, :], slot=kid2_r)
    fw.dma(out=iota[:], in_=C.d_iota[:, :], slot=iota_r)
    fw.dma(out=ident[:], in_=C.d_ident[:, :], slot=ident_r)
    fw.dma(out=tloc[:], in_=C.d_tloc[:, :], slot=tloc_r)
    fw.dma(out=pow2[:], in_=C.d_pow2[:, :], slot=pow2_r)
    fw.op("dve", lambda e: e.memset(ddall[:], 0.0), writes=[ddall_r])
    ev = [0]
    for qb in range(C.nqb):
        k = qb // 2; hf = qb % 2
        tl0 = 256 * k + 128 * hf
        NK = 2048 * (k + 1)
        q, q_r = qi.next(); w, w_r = wi.next(); d, d_r = dg.next()
        fw.dma(out=q[:], in_=C.q_idxT[:, :, tl0:tl0 + 128].rearrange("b p t -> p b t"), slot=q_r)
        fw.dma(out=w[:], in_=C.w_idx[tl0:tl0 + 128, :], slot=w_r)
        for h in range(16):
            fw.op("pool", lambda e: e.tensor_scalar(out=d[:, h, :], in0=ident[:], scalar1=w[:, h:h + 1], scalar2=None, op0=ALU.mult),
                  reads=[ident_r, w_r], writes=[d_r])
        def stageL(ck):
            R, R_r = Rt.next()
            for h in range(16):
                blk = h // 2; po = (h % 2) * 64
                ps, ps_r = psL.next()
                fw.op("pe", lambda e: e.matmul(ps[:, 0:256], lhsT=q[po:po + 64, blk, :], rhs=kid[po:po + 64, ck * 256:(ck + 1) * 256], start=True, stop=True),
                      reads=[q_r, kid_r, kid2_r], writes=[ps_r])
                ev[0] += 1
                if ev[0] % 2 == 0:
                    fw.op("act", lambda e: e.activation(out=R[:, h, :], in_=ps[:, 0:256], func=AF.Relu), reads=[ps_r], writes=[R_r])
                else:
                    fw.op("dve", lambda e: e.tensor_scalar(out=R[:, h, :], in0=ps[:, 0:256], scalar1=0.0, scalar2=None, op0=ALU.max), reads=[ps_r], writes=[R_r])
            return R, R_r

        def stageS(ck, R, R_r):
            ps, ps_r = psS.next()
            for h in range(16):
                fw.op("pe", lambda e: e.matmul(ps[:, 0:256], lhsT=d[:, h, :], rhs=R[:, h, :], start=(h == 0), stop=(h == 15)),
                      reads=[d_r, R_r], writes=[ps_r])
            fw.op("act", lambda e: e.copy(out=score[:, ck * 256:(ck + 1) * 256], in_=ps[:, 0:256]), reads=[ps_r], writes=[score_r])
        nck = NK // 256
        cur = stageL(0)
        for ck in range(nck):
            nxt = stageL(ck + 1) if ck + 1 < nck else None
            stageS(ck, *cur)
            cur = nxt
        s, s_r = sm.next()
        fw.op("dve", lambda e: e.tensor_reduce(out=s[:, 0:1], in_=score[:, 0:NK], axis=AX.X, op=ALU.max, apply_absolute_value=True), reads=[score_r], writes=[s_r])
        fw.op("dve", lambda e: e.tensor_scalar(out=s[:, 8:8 + NIT], in0=pow2[:], scalar1=s[:, 0:1], scalar2=None, op0=ALU.mult), reads=[pow2_r, s_r], writes=[s_r])
        fw.op("dve", lambda e: e.tensor_scalar(out=s[:, 1:2], in0=s[:, 0:1], scalar1=-1.0, scalar2=None, op0=ALU.mult), reads=[s_r], writes=[s_r])
        fw.op("dve", lambda e: e.tensor_scalar(out=tmpz[:], in0=iota[:], scalar1=tloc[:, qb:qb + 1], scalar2=-BIG, op0=ALU.is_gt, op1=ALU.mult),
              reads=[iota_r, tloc_r], writes=[tmpz_r])
        fw.op("dve", lambda e: e.tensor_tensor(out=score[:, NK - 2048:NK], in0=score[:, NK - 2048:NK], in1=tmpz[:], op=ALU.add), reads=[tmpz_r, score_r], writes=[score_r])
        for it in range(NIT):
            fw.op("dve", lambda e: e.tensor_tensor(out=s[:, 2:3], in0=s[:, 1:2], in1=s[:, 8 + it:9 + it], op=ALU.add), reads=[s_r], writes=[s_r])
            fw.op("dve", lambda e: e.tensor_scalar(out=mb[:, 0:NK], in0=score[:, 0:NK], scalar1=s[:, 2:3], scalar2=None, op0=ALU.is_ge, op1=ALU.add, accum_out=s[:, 3:4]),
                  reads=[score_r, s_r], writes=[mb_r, s_r])
            fw.op("dve", lambda e: e.scalar_tensor_tensor(out=s[:, 4:5], in0=s[:, 3:4], scalar=256.0, in1=s[:, 8 + it:9 + it], op0=ALU.is_ge, op1=ALU.mult), reads=[s_r], writes=[s_r])
            fw.op("dve", lambda e: e.tensor_tensor(out=s[:, 1:2], in0=s[:, 1:2], in1=s[:, 4:5], op=ALU.add), reads=[s_r], writes=[s_r])
        fw.op("dve", lambda e: e.tensor_scalar(out=mb[:, 0:NK], in0=score[:, 0:NK], scalar1=s[:, 1:2], scalar2=NEG, op0=ALU.is_lt, op1=ALU.mult),
              reads=[score_r, s_r], writes=[mb_r])
        fw.dma(out=C.maskb[qb, :, 0:NK], in_=mb[:, 0:NK], slot=mb_r)
        ng = NK // 32
        fw.op("dve", lambda e: e.tensor_reduce(out=a32[:, 0:ng], in_=mb[:, 0:NK].rearrange("p (g e) -> p g e", e=32), axis=AX.X, op=ALU.max), reads=[mb_r], writes=[a32_r])
        fw.op("dve", lambda e: e.tensor_scalar(out=tmpz[:, 0:ng], in0=iota[:, 0:ng], scalar1=1.0, scalar2=None, op0=ALU.add), reads=[iota_r], writes=[tmpz_r])
        fw.op("dve", lambda e: e.scalar_tensor_tensor(out=a32[:, 0:ng], in0=a32[:, 0:ng], scalar=-1.0, in1=tmpz[:, 0:ng], op0=ALU.is_ge, op1=ALU.mult), reads=[a32_r, tmpz_r], writes=[a32_r])
        fw.op("dve", lambda e: e.tensor_reduce(out=s[:, 5:6], in_=a32[:, 0:ng], axis=AX.X, op=ALU.max), reads=[a32_r], writes=[s_r])
        fw.op("dve", lambda e: e.tensor_scalar(out=ddall[:, qb:qb + 1], in0=s[:, 5:6], scalar1=-32.0, scalar2=1.0, op0=ALU.mult, op1=ALU.add), reads=[s_r], writes=[ddall_r])
    fw.dma(out=C.ddall[:, :], in_=ddall[:], slot=ddall_r)
    fw.end_phase()


def phase_dsa_attn(fw, C, L):
    fw.begin_phase()
    banks = mk_psum(fw, 8)
    psT = Rot([banks[0], banks[1], banks[7]]); psO = [banks[2], banks[3]]; psD = banks[4]; psU = Rot(banks[5:7])
    ckT = fw.sbuf("ckT", [128, 2, S], BF16); ckT_r = Res()
    ckv = fw.sbuf("ckv", [128, 128, KVL], BF16); ckv_r = Res()
    ql = mk_slots(fw, "ql", [128, 2, NH, 128], BF16, 2)
    mbt = mk_slots(fw, "mbt", [128, 2048], BF16, 3)
    pt = mk_slots(fw, "pt", [128, 512], BF16, 4)
    Er = mk_slots(fw, "Er", [128, 512], BF16, 2)
    E4 = fw.sbuf("E4", [128, 512], BF16); E4_r = Res()
    ident = fw.sbuf("ident", [128, 128], BF16); ident_r = Res()
    ones = fw.sbuf("ones", [128, 128], BF16); ones_r = Res()
    kb = fw.sbuf("kb", [128, 128, NH], F32); kb_r = Res()
    slopes = fw.sbuf("slopes", [128, NH], F32); slopes_r = Res()
    ddall = fw.sbuf("ddall", [128, 16], F32); ddall_r = Res()
    rp = mk_slots(fw, "rp", [128, NH], F32, 2)
    rec = mk_slots(fw, "rec", [128, 512], F32, 2)
    olat = mk_slots(fw, "olat", [128, 2, 512], BF16, 2)
    oa = mk_slots(fw, "oa", [128, 512], BF16, 2)
    wuv_st = fw.sbuf("wuv_st", [128, NH, 2, 128], F32); wuv_st_r = Res()
    wuv = fw.sbuf("wuv", [128, NH, 2, 128], BF16); wuv_r = Res()
    fw.dma(out=ckT[:], in_=C.c_kvT_g.rearrange("cc p s -> p cc s"), slot=ckT_r)
    fw.dma(out=ckv[:], in_=C.c_kv_g.rearrange("(b p) c -> p b c", p=128), slot=ckv_r)
    fw.dma(out=ident[:], in_=C.d_ident[:, :], slot=ident_r)
    fw.dma(out=ones[:], in_=C.d_ones[:, :], slot=ones_r)
    fw.dma(out=kb[:], in_=C.d_kb[:, :, :], slot=kb_r)
    fw.dma(out=slopes[:], in_=C.d_slopes[:, :], slot=slopes_r)
    fw.dma(out=ddall[:], in_=C.ddall[:, :], slot=ddall_r)
    fw.dma(out=wuv_st[:], in_=C.w_uv[L].rearrange("h (cc p) d -> p h cc d", p=128), slot=wuv_st_r)
    fw.op("pool", lambda e: e.tensor_copy(out=wuv[:], in_=wuv_st[:]), reads=[wuv_st_r], writes=[wuv_r])
    for i in range(4):
        fw.op("pool", lambda e: e.tensor_copy(out=E4[:, i * 128:(i + 1) * 128], in_=ident[:]), reads=[ident_r], writes=[E4_r])
    for qb in range(C.nqb):
        k = qb // 2; hf = qb % 2
        tl0 = 256 * k + 128 * hf
        NK = 2048 * (k + 1)
        q, q_r = ql.next()
        for cc in range(2):
            fw.dma(out=q[:, cc, :, :], in_=C.q_latT[cc, :, :, tl0:tl0 + 128], slot=q_r)
        r, r_r = rp.next()
        fw.op("dve", lambda e: e.tensor_scalar(out=r[:], in0=slopes[:], scalar1=ddall[:, qb:qb + 1], scalar2=None, op0=ALU.mult), reads=[slopes_r, ddall_r], writes=[r_r])
        for g in range(2):
            er, er_r = Er.next()
            for hh in range(4):
                h = g * 4 + hh
                fw.op("dve", lambda e: e.tensor_scalar(out=er[:, hh * 128:(hh + 1) * 128], in0=ident[:], scalar1=r[:, h:h + 1], scalar2=None, op0=ALU.mult),
                      reads=[ident_r, r_r], writes=[er_r])
            nkb = NK // 128
            mcur = [None]

            def stageA(jk):
                if jk % 16 == 0:
                    mcur[0] = mbt.next()
                    fw.dma(out=mcur[0][0][:], in_=C.maskb[qb, :, jk * 128:jk * 128 + 2048], slot=mcur[0][1])
                m, m_r = mcur[0]
                ps, ps_r = psT.next()
                rhs_q = [q[:, cc, g * 4:(g + 1) * 4, :] for cc in range(2)]
                fw.op("pe", lambda e: e.matmul(ps[:], lhsT=ckT[:, 0, jk * 128:(jk + 1) * 128], rhs=rhs_q[0], start=True, stop=False), reads=[ckT_r, q_r], writes=[ps_r])
                fw.op("pe", lambda e: e.matmul(ps[:], lhsT=ckT[:, 1, jk * 128:(jk + 1) * 128], rhs=rhs_q[1], start=False, stop=False), reads=[ckT_r, q_r], writes=[ps_r])
                fw.op("pe", lambda e: e.matmul(ps[:], lhsT=m[:, (jk % 16) * 128:(jk % 16 + 1) * 128], rhs=E4[:], start=False, stop=False), reads=[m_r, E4_r], writes=[ps_r])
                fw.op("pe", lambda e: e.matmul(ps[:], lhsT=ones[:], rhs=er[:], start=False, stop=True), reads=[ones_r, er_r], writes=[ps_r])
                p, p_r = pt.next()
                for hh in range(4):
                    h = g * 4 + hh
                    fw.op("act", lambda e: e.activation(out=p[:, hh * 128:(hh + 1) * 128], in_=ps[:, hh * 128:(hh + 1) * 128], func=AF.Exp, bias=kb[:, jk, h:h + 1]),
                          reads=[ps_r, kb_r], writes=[p_r])
                return p, p_r

            def stageB(jk, p, p_r):
                for cc in range(2):
                    fw.op("pe", lambda e: e.matmul(psO[cc][0][:], lhsT=ckv[:, jk, cc * 128:(cc + 1) * 128], rhs=p[:], start=(jk == 0), stop=(jk == nkb - 1)),
                          reads=[ckv_r, p_r], writes=[psO[cc][1]])
                fw.op("pe", lambda e: e.matmul(psD[0][:], lhsT=ones[:], rhs=p[:], start=(jk == 0), stop=(jk == nkb - 1)), reads=[ones_r, p_r], writes=[psD[1]])
            cur = stageA(0)
            for jk in range(nkb):
                nxt = stageA(jk + 1) if jk + 1 < nkb else None
                stageB(jk, *cur)
                cur = nxt
            rc, rc_r = rec.next()
            fw.op("dve", lambda e: e.reciprocal(out=rc[:], in_=psD[0][:]), reads=[psD[1]], writes=[rc_r])
            ol, ol_r = olat.next()
            for cc in range(2):
                fw.op("dve", lambda e: e.tensor_tensor(out=ol[:, cc, :], in0=psO[cc][0][:], in1=rc[:], op=ALU.mult), reads=[psO[cc][1], rc_r], writes=[ol_r])
            pu, pu_r = psU.next()
            for hh in range(4):
                h = g * 4 + hh
                for cc in range(2):
                    fw.op("pe", lambda e: e.matmul(pu[:, hh * 128:(hh + 1) * 128], lhsT=wuv[:, h, cc, :], rhs=ol[:, cc, hh * 128:(hh + 1) * 128], start=(cc == 0), stop=(cc == 1)),
                          reads=[wuv_r, ol_r], writes=[pu_r])
            o, o_r = oa.next()
            fw.op("act", lambda e: e.copy(out=o[:], in_=pu[:]), reads=[pu_r], writes=[o_r])
            fw.dma(out=C.o_aT[g * 4:(g + 1) * 4, :, tl0:tl0 + 128].rearrange("h p t -> p h t"), in_=o[:].rearrange("p (h t) -> p h t", h=4), slot=o_r)
    fw.end_phase()


def declare_moba_io(fw, C, ext):
    declare(fw, C, "d_pastneg", [128, 8, 64], F32, ext)
    declare(fw, C, "d_pastind", [128, 8, 64], F32, ext)
    declare(fw, C, "d_alq", [128, 2, NH], F32, ext)
    declare(fw, C, "d_zc", [128, 32, 128], BF16, ext)
    declare(fw, C, "d_kbm", [128, NH, 128], F32, ext)


def host_consts_moba(c):
    out = {}
    pn = np.zeros((128, 8, 64), np.float32); pi = np.zeros((128, 8, 64), np.float32)
    for k in range(8):
        own = 8 * k + c
        pn[:, k, own:] = -BIG
        pi[:, k, :own] = 1.0
    out["d_pastneg"] = pn; out["d_pastind"] = pi
    sl = np.array([2.0 ** (-(i + 1)) for i in range(NH)], np.float32)
    p = np.arange(128, dtype=np.float32)
    alq = np.zeros((128, 2, NH), np.float32)
    for qh in range(2):
        alq[:, qh, :] = (255 - 128 * qh - p)[:, None] * sl[None, :]
    out["d_alq"] = alq
    zc = np.zeros((128, 8, 2, 2, 128), np.float32)
    for cp in range(8):
        for qh in range(2):
            for j in range(2):
                if cp > c:
                    zc[:, cp, qh, j, :] = NEG
                elif cp == c:
                    kpos = 128 * j + np.arange(128)[None, :]
                    qpos = 128 * qh + np.arange(128)[:, None]
                    zc[:, cp, qh, j, :] = np.where(kpos <= qpos, 0.0, NEG)
    out["d_zc"] = zc.reshape(128, 32, 128).astype(NPBF)
    m = np.arange(128, dtype=np.float32)
    kbm = (p[:, None, None] + 128 * (m[None, None, :] - 112) - 256 * c - 255) * sl[None, :, None]
    out["d_kbm"] = kbm.astype(np.float32)
    return out


def phase_moba(fw, C, L):
    fw.begin_phase()
    banks = mk_psum(fw, 8)
    psT = Rot(banks[0:3]); psO = banks[3]; psD = banks[4]; psG = Rot(banks[5:7])
    kTs = mk_slots(fw, "kT", [128, S], BF16, 2)
    vhs = mk_slots(fw, "vh", [128, 128, 128], BF16, 2)
    qTs = mk_slots(fw, "qT", [128, TL], BF16, 2)
    kmf = fw.sbuf("kmf", [128, NH, 64], F32); kmf_r = Res()
    kmb = fw.sbuf("kmb", [128, NH, 64], BF16); kmb_r = Res()
    ident = fw.sbuf("ident", [128, 128], BF16); ident_r = Res()
    ones = fw.sbuf("ones", [128, 128], BF16); ones_r = Res()
    pastneg = fw.sbuf("pastneg", [128, 8, 64], F32); pastneg_r = Res()
    pastind = fw.sbuf("pastind", [128, 8, 64], F32); pastind_r = Res()
    alq = fw.sbuf("alq", [128, 2, NH], F32); alq_r = Res()
    zc = fw.sbuf("zc", [128, 32, 128], BF16); zc_r = Res()
    kbm = fw.sbuf("kbm", [128, NH, 128], F32); kbm_r = Res()
    gss = mk_slots(fw, "gs", [128, 64 + 64 + 16], F32, 2)
    sbs = mk_slots(fw, "sb", [128, 2, 64], BF16, 2)
    bzs = mk_slots(fw, "bz", [128, 128], BF16, 8)
    pts = mk_slots(fw, "pt", [128, 256], BF16, 4)
    recs = mk_slots(fw, "rec", [128, 256], F32, 2)
    obs = mk_slots(fw, "ob", [128, 256], BF16, 2)
    for t, r, src in [(kmf, kmf_r, C.k_meanT_g[:, :, :]), (ident, ident_r, C.d_ident[:, :]), (ones, ones_r, C.d_ones[:, :]),
                      (pastneg, pastneg_r, C.d_pastneg[:, :, :]), (pastind, pastind_r, C.d_pastind[:, :, :]), (alq, alq_r, C.d_alq[:, :, :]),
                      (zc, zc_r, C.d_zc[:, :, :]), (kbm, kbm_r, C.d_kbm[:, :, :])]:
        fw.dma(out=t[:], in_=src, slot=r)
    fw.op("dve", lambda e: e.tensor_copy(out=kmb[:], in_=kmf[:]), reads=[kmf_r], writes=[kmb_r])
    for h in range(C.nheads):
        kT, kT_r = kTs.next(); vh, vh_r = vhs.next(); qT, qT_r = qTs.next()
        fw.dma(out=kT[:], in_=C.k_mT_g[h, :, :], slot=kT_r)
        vsrc = C.v_m_g.rearrange("(b p) (h d) -> p b h d", p=128, h=NH)
        for half in range(2):
            fw.dma(out=vh[:, half * 64:(half + 1) * 64, :], in_=vsrc[:, half * 64:(half + 1) * 64, h, :], slot=vh_r)
        fw.dma(out=qT[:], in_=C.q_mT[h, :, :], slot=qT_r)
        for k in range(C.nslots):
            sb, sb_r = sbs.next()
            for qh in range(2):
                pg, pg_r = psG.next()
                t0 = 256 * k + 128 * qh
                fw.op("pe", lambda e: e.matmul(pg[:, 0:64], lhsT=qT[:, t0:t0 + 128], rhs=kmb[:, h, :], start=True, stop=True), reads=[qT_r, kmb_r], writes=[pg_r])
                gs, gs_r = gss.next()
                fw.op("dve", lambda e: e.tensor_tensor(out=gs[:, 0:64], in0=pg[:, 0:64], in1=pastneg[:, k, :], op=ALU.add), reads=[pg_r, pastneg_r], writes=[gs_r])
                fw.op("dve", lambda e: e.max(out=gs[:, 128:136], in_=gs[:, 0:64]), reads=[gs_r], writes=[gs_r])
                fw.op("dve", lambda e: e.tensor_scalar(out=gs[:, 136:137], in0=gs[:, 130:131], scalar1=-1e29, scalar2=None, op0=ALU.max), reads=[gs_r], writes=[gs_r])
                fw.op("dve", lambda e: e.scalar_tensor_tensor(out=gs[:, 64:128], in0=gs[:, 0:64], scalar=gs[:, 136:137], in1=pastind[:, k, :], op0=ALU.is_lt, op1=ALU.mult),
                      reads=[gs_r, pastind_r], writes=[gs_r])
                fw.op("dve", lambda e: e.tensor_scalar(out=sb[:, qh, :], in0=gs[:, 64:128], scalar1=NEG, scalar2=alq[:, qh, h:h + 1], op0=ALU.mult, op1=ALU.add),
                      reads=[gs_r, alq_r], writes=[sb_r])
            nblk = 8 * k + 8

            def stageA(j2):
                n = j2 // 2; j = j2 % 2
                ps, ps_r = psT.next()
                fw.op("pe", lambda e: e.matmul(ps[:, 0:256], lhsT=kT[:, j2 * 128:(j2 + 1) * 128], rhs=qT[:, 256 * k:256 * k + 256], start=True, stop=False),
                      reads=[kT_r, qT_r], writes=[ps_r])
                for qh in range(2):
                    if n < 8 * k:
                        fw.op("pe", lambda e: e.matmul(ps[:, qh * 128:(qh + 1) * 128], lhsT=sb[:, qh, n:n + 1].to_broadcast([128, 128]), rhs=ident[:], start=False, stop=(qh == 1)),
                              reads=[sb_r, ident_r], writes=[ps_r])
                    else:
                        cp = n - 8 * k
                        bz, bz_r = bzs.next()
                        fw.op("dve", lambda e: e.tensor_scalar(out=bz[:], in0=zc[:, cp * 4 + qh * 2 + j, :], scalar1=sb[:, qh, n:n + 1], scalar2=None, op0=ALU.add),
                              reads=[zc_r, sb_r], writes=[bz_r])
                        fw.op("pe", lambda e: e.matmul(ps[:, qh * 128:(qh + 1) * 128], lhsT=bz[:], rhs=ident[:], start=False, stop=(qh == 1)),
                              reads=[bz_r, ident_r], writes=[ps_r])
                p, p_r = pts.next()
                mi = j2 - 16 * k + 112
                fw.op("act", lambda e: e.activation(out=p[:], in_=ps[:, 0:256], func=AF.Exp, bias=kbm[:, h, mi:mi + 1]), reads=[ps_r, kbm_r], writes=[p_r])
                return p, p_r

            def stageB(j2, p, p_r):
                first = (j2 == 0); last = (j2 == 2 * nblk - 1)
                fw.op("pe", lambda e: e.matmul(psO[0][:, 0:256], lhsT=vh[:, j2, :], rhs=p[:], start=first, stop=last), reads=[vh_r, p_r], writes=[psO[1]])
                fw.op("pe", lambda e: e.matmul(psD[0][:, 0:256], lhsT=ones[:], rhs=p[:], start=first, stop=last), reads=[ones_r, p_r], writes=[psD[1]])
            cur = stageA(0)
            for j2 in range(2 * nblk):
                nxt = stageA(j2 + 1) if j2 + 1 < 2 * nblk else None
                stageB(j2, *cur)
                cur = nxt
            rc, rc_r = recs.next()
            fw.op("dve", lambda e: e.reciprocal(out=rc[:], in_=psD[0][:, 0:256]), reads=[psD[1]], writes=[rc_r])
            o, o_r = obs.next()
            fw.op("dve", lambda e: e.tensor_tensor(out=o[:], in0=psO[0][:, 0:256], in1=rc[:], op=ALU.mult), reads=[psO[1], rc_r], writes=[o_r])
            fw.dma(out=C.o_bT[h, :, 256 * k:256 * k + 256], in_=o[:], slot=o_r)
    fw.end_phase()


TT = 256


def declare_post_io(fw, C, ext):
    nl = C.nl
    declare(fw, C, "w_o_dsa", [nl, 1024, D], F32, ext)
    declare(fw, C, "w_o_moba", [nl, 1024, D], F32, ext)
    declare(fw, C, "w_out", [nl, D, D], F32, ext)
    declare(fw, C, "w_q_mem", [nl, D, 512], F32, ext)
    declare(fw, C, "w_kv_mem", [nl, D, 1024], F32, ext)
    declare(fw, C, "w_o_mem", [nl, 512, D], F32, ext)
    declare(fw, C, "w_ffn_in", [nl, D, 2 * DFF], F32, ext)
    declare(fw, C, "w_ffn_out", [nl, DFF, D], F32, ext)
    declare(fw, C, "d_lng", [nl, 128, 3, 16], F32, ext)
    declare(fw, C, "d_lnb", [nl, 128, 3, 16], F32, ext)
    declare(fw, C, "memT", [D, 256], F32, ext)
    declare(fw, C, "d_onesf", [128, 128], F32, ext)
    declare(fw, C, "xT_out", [D, TL], F32, ext)


def host_consts_post(inp, layers):
    out = {}
    nl = len(layers)
    out["d_lng"] = np.ascontiguousarray(inp["ln_g"][layers].reshape(nl, 3, 16, 128).transpose(0, 3, 1, 2)).astype(np.float32)
    out["d_lnb"] = np.ascontiguousarray(inp["ln_b"][layers].reshape(nl, 3, 16, 128).transpose(0, 3, 1, 2)).astype(np.float32)
    out["memT"] = np.ascontiguousarray(inp["mem"][0].T).astype(np.float32)
    out["d_onesf"] = np.full((128, 128), 1.0 / D, np.float32)
    return out


POST_MATS = [("w_o_dsa", 8, 16), ("w_o_moba", 8, 16), ("w_out", 16, 16), ("w_q_mem", 16, 4), ("w_kv_mem", 16, 8), ("w_o_mem", 4, 16),
             ("w_ffn_in", 16, 88), ("w_ffn_out", 44, 16)]


def declare_prep(fw, C, dt):
    for name, K, nb in POST_MATS:
        setattr(C, "p_" + name, dt("p_" + name, [nb, 128, K, 128], BF16, "Internal"))


def phase_prep(fw, C, L):
    fw.begin_phase()
    st = mk_slots(fw, "pst", [128, 16, 512], F32, 2)
    bf = mk_slots(fw, "pbf", [128, 16, 512], BF16, 2)
    i = 0
    for name, K, nb in POST_MATS:
        src = getattr(C, name)[L]
        dst = getattr(C, "p_" + name)
        ncols = nb * 128
        for c0 in range(0, ncols, 512):
            cw = min(512, ncols - c0)
            for k0 in range(0, K, 16):
                kw = min(16, K - k0)
                s_, s_r = st.next(); b_, b_r = bf.next()
                fw.dma(out=s_[:, 0:kw, 0:cw], in_=src[k0 * 128:(k0 + kw) * 128, c0:c0 + cw].rearrange("(kc p) n -> p kc n", p=128), slot=s_r)
                eng = ["pool", "dve", "act"][i % 3]; i += 1
                if eng == "act":
                    fw.op("act", lambda e: e.copy(out=b_[:, 0:kw, 0:cw], in_=s_[:, 0:kw, 0:cw]), reads=[s_r], writes=[b_r])
                else:
                    fw.op(eng, lambda e: e.tensor_copy(out=b_[:, 0:kw, 0:cw], in_=s_[:, 0:kw, 0:cw]), reads=[s_r], writes=[b_r])
                for j in range(cw // 128):
                    fw.dma(out=dst[c0 // 128 + j, :, k0:k0 + kw, :], in_=b_[:, 0:kw, j * 128:(j + 1) * 128], slot=b_r)
    fw.end_phase()


def phase_post(fw, C, L, x_in, x_out):
    fw.begin_phase()
    banks = mk_psum(fw, 8)
    psb = Rot(banks[0:4]); psM = banks[4]; psQ = banks[5]; psO = banks[6]; psD = banks[7]
    wbf = mk_slots(fw, "wbf2", [128, 44, 128], BF16, 4)
    xres = fw.sbuf("xres", [128, 16, TT], F32); xres_r = Res()
    xb = fw.sbuf("xb", [128, 16, TT], BF16); xb_r = Res()
    z = fw.sbuf("z", [128, 16, TT], F32); z_r = Res()
    zsq = fw.sbuf("zsq", [128, 16, TT], F32); zsq_r = Res()
    oa = fw.sbuf("oa", [128, 8, TT], BF16); oa_r = Res()
    obm = fw.sbuf("obm", [128, 8, TT], BF16); obm_r = Res()
    mT = fw.sbuf("mT", [128, 16, TT], BF16); mT_r = Res()
    aT = fw.sbuf("aT", [128, 44, TT], BF16); aT_r = Res()
    gts = mk_slots(fw, "gt", [128, 2, TT], F32, 2)
    t12 = mk_slots(fw, "t12", [128, 2, TT], F32, 2)
    sgs = mk_slots(fw, "sg", [128, TT], F32, 2)
    stat = fw.sbuf("stat", [128, 4, TT], F32); stat_r = Res()
    qmT = fw.sbuf("qmT", [128, 4, TT], BF16); qmT_r = Res()
    omT = fw.sbuf("omT", [128, 4, TT], BF16); omT_r = Res()
    pts = mk_slots(fw, "ptm", [128, TT], BF16, 2)
    recs = mk_slots(fw, "recm", [128, TT], F32, 2)
    kmemT = fw.sbuf("kmemT", [128, 4, 256], BF16); kmemT_r = Res()
    vmem = fw.sbuf("vmem", [128, 2, 512], BF16); vmem_r = Res()
    memst = z; memst_r = z_r
    memb = mT; memb_r = mT_r
    lng = fw.sbuf("lng", [128, 3, 16], F32); lng_r = Res()
    lnb = fw.sbuf("lnb", [128, 3, 16], F32); lnb_r = Res()
    onesf = fw.sbuf("onesf", [128, 128], F32); onesf_r = Res()
    ones = fw.sbuf("ones", [128, 128], BF16); ones_r = Res()
    fw.dma(out=lng[:], in_=C.d_lng[L], slot=lng_r)
    fw.dma(out=lnb[:], in_=C.d_lnb[L], slot=lnb_r)
    fw.dma(out=onesf[:], in_=C.d_onesf[:, :], slot=onesf_r)
    fw.dma(out=ones[:], in_=C.d_ones[:, :], slot=ones_r)
    fw.dma(out=memst[:], in_=C.memT.rearrange("(kc p) m -> p kc m", p=128), slot=memst_r)
    fw.op("pool", lambda e: e.tensor_copy(out=memb[:], in_=memst[:]), reads=[memst_r], writes=[memb_r])

    def slab(name, nb, K):
        wb, wb_r = wbf.next()
        fw.dma(out=wb[:, 0:K, :], in_=getattr(C, "p_" + name)[nb, :, :, :], slot=wb_r)
        return wb, wb_r

    def mm(wb, wb_r, K, rhs_t, rhs_r, n=TT):
        ps, ps_r = psb.next()
        for kc in range(K):
            fw.op("pe", lambda e: e.matmul(ps[:, 0:n], lhsT=wb[:, kc, :], rhs=rhs_t[:, kc, 0:n], start=(kc == 0), stop=(kc == K - 1)),
                  reads=[wb_r, rhs_r], writes=[ps_r])
        return ps, ps_r

    for hq in range(4):
        wb, wb_r = slab("w_kv_mem", hq, 16)
        ps, ps_r = mm(wb, wb_r, 16, memb, memb_r, 256)
        fw.op("act", lambda e: e.copy(out=kmemT[:, hq, :], in_=ps[:, 0:256]), reads=[ps_r], writes=[kmemT_r])
    for hq in range(4):
        wb, wb_r = slab("w_kv_mem", 4 + hq, 16)
        for ms in range(2):
            ps, ps_r = psb.next()
            for kc in range(16):
                fw.op("pe", lambda e: e.matmul(ps[:, 0:128], lhsT=memb[:, kc, ms * 128:(ms + 1) * 128], rhs=wb[:, kc, :], start=(kc == 0), stop=(kc == 15)),
                      reads=[memb_r, wb_r], writes=[ps_r])
            fw.op("act", lambda e: e.copy(out=vmem[:, ms, hq * 128:(hq + 1) * 128], in_=ps[:, 0:128]), reads=[ps_r], writes=[vmem_r])

    def layer_norm(idx):
        fw.op("act", lambda e: e.activation(out=zsq[:], in_=z[:], func=AF.Square), reads=[z_r], writes=[zsq_r])
        for nb in range(16):
            fw.op("pe", lambda e: e.matmul(psM[0][:, 0:TT], lhsT=onesf[:], rhs=z[:, nb, :], start=(nb == 0), stop=(nb == 15)), reads=[onesf_r, z_r], writes=[psM[1]])
        for nb in range(16):
            fw.op("pe", lambda e: e.matmul(psQ[0][:, 0:TT], lhsT=onesf[:], rhs=zsq[:, nb, :], start=(nb == 0), stop=(nb == 15)), reads=[onesf_r, zsq_r], writes=[psQ[1]])
        fw.op("act", lambda e: e.copy(out=stat[:, 0, :], in_=psM[0][:, 0:TT]), reads=[psM[1]], writes=[stat_r])
        fw.op("dve", lambda e: e.tensor_tensor(out=stat[:, 1, :], in0=stat[:, 0, :], in1=stat[:, 0, :], op=ALU.mult), reads=[stat_r], writes=[stat_r])
        fw.op("dve", lambda e: e.tensor_tensor(out=stat[:, 2, :], in0=psQ[0][:, 0:TT], in1=stat[:, 1, :], op=ALU.subtract), reads=[psQ[1], stat_r], writes=[stat_r])
        fw.op("dve", lambda e: e.tensor_scalar(out=stat[:, 2, :], in0=stat[:, 2, :], scalar1=EPS, scalar2=None, op0=ALU.add), reads=[stat_r], writes=[stat_r])
        fw.op("act", lambda e: e.activation(out=stat[:, 1, :], in_=stat[:, 2, :], func=AF.Sqrt), reads=[stat_r], writes=[stat_r])
        fw.op("dve", lambda e: e.reciprocal(out=stat[:, 3, :], in_=stat[:, 1, :]), reads=[stat_r], writes=[stat_r])
        for nb in range(16):
            fw.op("dve", lambda e: e.tensor_tensor(out=z[:, nb, :], in0=z[:, nb, :], in1=stat[:, 0, :], op=ALU.subtract), reads=[z_r, stat_r], writes=[z_r])
            fw.op("dve", lambda e: e.tensor_tensor(out=z[:, nb, :], in0=z[:, nb, :], in1=stat[:, 3, :], op=ALU.mult), reads=[z_r, stat_r], writes=[z_r])
            fw.op("act", lambda e: e.activation(out=xres[:, nb, :], in_=z[:, nb, :], func=AF.Identity, scale=lng[:, idx, nb:nb + 1], bias=lnb[:, idx, nb:nb + 1]),
                  reads=[z_r, lng_r, lnb_r], writes=[xres_r])
        fw.op("pool", lambda e: e.tensor_copy(out=xb[:], in_=xres[:]), reads=[xres_r], writes=[xb_r])

    def resid(ps, ps_r, nb):
        fw.op("dve", lambda e: e.scalar_tensor_tensor(out=z[:, nb, :], in0=xres[:, nb, :], scalar=float(ALPHA), in1=ps[:, 0:TT], op0=ALU.mult, op1=ALU.add),
              reads=[xres_r, ps_r], writes=[z_r])

    for j in range(C.ntiles):
        t0 = j * TT
        fw.dma(out=xres[:], in_=x_in.rearrange("(kc p) t -> p kc t", p=128)[:, :, t0:t0 + TT], slot=xres_r)
        fw.dma(out=oa[:], in_=C.o_aT[:, :, t0:t0 + TT].rearrange("h p t -> p h t"), slot=oa_r)
        fw.dma(out=obm[:], in_=C.o_bT[:, :, t0:t0 + TT].rearrange("h p t -> p h t"), slot=obm_r)
        for fb in range(16):
            wa, wa_r = slab("w_o_dsa", fb, 8)
            psA, psA_r = mm(wa, wa_r, 8, oa, oa_r)
            wb_, wb_r = slab("w_o_moba", fb, 8)
            psB, psB_r = mm(wb_, wb_r, 8, obm, obm_r)
            gt, gt_r = gts.next()
            fw.dma(out=gt[:, 0, :], in_=C.g_aT[fb, :, t0:t0 + TT], slot=gt_r)
            fw.dma(out=gt[:, 1, :], in_=C.g_bT[fb, :, t0:t0 + TT], slot=gt_r)
            tt_, tt_r = t12.next()
            fw.op("dve", lambda e: e.tensor_tensor(out=tt_[:, 0, :], in0=psA[:, 0:TT], in1=gt[:, 0, :], op=ALU.mult), reads=[psA_r, gt_r], writes=[tt_r])
            fw.op("dve", lambda e: e.tensor_tensor(out=tt_[:, 1, :], in0=psB[:, 0:TT], in1=gt[:, 1, :], op=ALU.mult), reads=[psB_r, gt_r], writes=[tt_r])
            fw.op("dve", lambda e: e.tensor_tensor(out=mT[:, fb, :], in0=tt_[:, 0, :], in1=tt_[:, 1, :], op=ALU.add), reads=[tt_r], writes=[mT_r])
        for nb in range(16):
            wb, wb_r = slab("w_out", nb, 16)
            ps, ps_r = mm(wb, wb_r, 16, mT, mT_r)
            resid(ps, ps_r, nb)
        layer_norm(0)
        for hq in range(4):
            wb, wb_r = slab("w_q_mem", hq, 16)
            ps, ps_r = mm(wb, wb_r, 16, xb, xb_r)
            fw.op("act", lambda e: e.mul(out=qmT[:, hq, :], in_=ps[:, 0:TT], mul=float(QSCALE)), reads=[ps_r], writes=[qmT_r])
        for hq in range(4):
            for ms in range(2):
                ps, ps_r = psb.next()
                fw.op("pe", lambda e: e.matmul(ps[:, 0:TT], lhsT=kmemT[:, hq, ms * 128:(ms + 1) * 128], rhs=qmT[:, hq, :], start=True, stop=True), reads=[kmemT_r, qmT_r], writes=[ps_r])
                p, p_r = pts.next()
                fw.op("act", lambda e: e.activation(out=p[:], in_=ps[:, 0:TT], func=AF.Exp), reads=[ps_r], writes=[p_r])
                fw.op("pe", lambda e: e.matmul(psO[0][:, 0:TT], lhsT=vmem[:, ms, hq * 128:(hq + 1) * 128], rhs=p[:], start=(ms == 0), stop=(ms == 1)), reads=[vmem_r, p_r], writes=[psO[1]])
                fw.op("pe", lambda e: e.matmul(psD[0][:, 0:TT], lhsT=ones[:], rhs=p[:], start=(ms == 0), stop=(ms == 1)), reads=[ones_r, p_r], writes=[psD[1]])
            rc, rc_r = recs.next()
            fw.op("dve", lambda e: e.reciprocal(out=rc[:], in_=psD[0][:, 0:TT]), reads=[psD[1]], writes=[rc_r])
            fw.op("dve", lambda e: e.tensor_tensor(out=omT[:, hq, :], in0=psO[0][:, 0:TT], in1=rc[:], op=ALU.mult), reads=[psO[1], rc_r], writes=[omT_r])
        for nb in range(16):
            wb, wb_r = slab("w_o_mem", nb, 4)
            ps, ps_r = mm(wb, wb_r, 4, omT, omT_r)
            resid(ps, ps_r, nb)
        layer_norm(1)
        for i in range(44):
            wg, wg_r = slab("w_ffn_in", i, 16)
            psg, psg_r = mm(wg, wg_r, 16, xb, xb_r)
            wu, wu_r = slab("w_ffn_in", 44 + i, 16)
            psu, psu_r = mm(wu, wu_r, 16, xb, xb_r)
            sg, sg_r = sgs.next()
            fw.op("act", lambda e: e.activation(out=sg[:], in_=psg[:, 0:TT], func=AF.Silu), reads=[psg_r], writes=[sg_r])
            fw.op("dve", lambda e: e.tensor_tensor(out=aT[:, i, :], in0=psu[:, 0:TT], in1=sg[:], op=ALU.mult), reads=[psu_r, sg_r], writes=[aT_r])
        for nb in range(16):
            wb, wb_r = slab("w_ffn_out", nb, 44)
            ps, ps_r = mm(wb, wb_r, 44, aT, aT_r)
            resid(ps, ps_r, nb)
        layer_norm(2)
        fw.dma(out=x_out.rearrange("(kc p) t -> p kc t", p=128)[:, :, t0:t0 + TT], in_=xres[:], slot=xres_r)
    fw.end_phase()


from concourse.bass_utils import run_bass_kernel_spmd

ACT_SET = {
    "q_latT": ([2, 128, NH, TL], BF16), "q_idxT": ([8, 128, TL], BF16), "w_idx": ([TL, IH], F32),
    "k_idxT_own": ([ID_, TL], BF16), "c_kv_own": ([TL, KVL], BF16), "c_kvT_own": ([2, 128, TL], BF16),
    "q_mT": ([NH, 128, TL], BF16), "k_mT_own": ([NH, 128, TL], BF16), "k_meanT_own": ([128, NH, 8], F32),
    "v_m_own": ([TL, NH * HD], BF16), "g_aT": ([16, 128, TL], F32), "g_bT": ([16, 128, TL], F32),
}
KSIDE_OWN = ["k_idxT_own", "c_kv_own", "c_kvT_own", "k_mT_own", "k_meanT_own", "v_m_own"]
QSIDE = ["q_latT", "q_idxT", "w_idx", "q_mT", "g_aT", "g_bT"]
KSIDE_G = {"k_idxT_g": ([ID_, S], BF16), "c_kvT_g": ([2, 128, S], BF16), "c_kv_g": ([S, KVL], BF16),
           "k_mT_g": ([NH, 128, S], BF16), "v_m_g": ([S, NH * HD], BF16), "k_meanT_g": ([128, NH, 64], F32)}
PROJ_W = {"w_in": [1, D, INW], "w_uk": [1, NH, HD, KVL], "d_kvg": [1, 128, KVL], "d_kig": [1, 128, ID_], "d_kib": [1, 128, ID_], "d_bg": [1, 128, 2, 16]}
POST_W = {"w_uv": [1, NH, KVL, HD], "w_o_dsa": [1, 1024, D], "w_o_moba": [1, 1024, D], "w_out": [1, D, D], "w_q_mem": [1, D, 512], "w_kv_mem": [1, D, 1024],
          "w_o_mem": [1, 512, D], "w_ffn_in": [1, D, 2 * DFF], "w_ffn_out": [1, DFF, D], "d_lng": [1, 128, 3, 16], "d_lnb": [1, 128, 3, 16]}
CONSTS = {"d_ident": ([128, 128], BF16), "d_ones": ([128, 128], BF16), "d_onesf": ([128, 128], F32), "memT": ([D, 256], F32),
          "d_tloc": ([128, 16], F32), "d_iota": ([128, 2048], F32), "d_pow2": ([128, NIT], F32), "d_slopes": ([128, NH], F32), "d_kb": ([128, 128, NH], F32),
          "d_pastneg": ([128, 8, 64], F32), "d_pastind": ([128, 8, 64], F32), "d_alq": ([128, 2, NH], F32), "d_zc": ([128, 32, 128], BF16), "d_kbm": ([128, NH, 128], F32)}


def build_program(do_attn_post, do_proj, debug=False):
    nc = bass.Bass("TRN2", target_bir_lowering=False)
    ins, outs = [], []
    with contextlib.ExitStack() as es:
        fw = Fw(nc, es)
        C = Ctx(); C.cst = None; C.nl = 1
        C.nqb = 16; C.nslots = 8; C.nheads = NH; C.ntiles = TL // TT

        def dt(name, shape, dtype, kind):
            t = nc.dram_tensor(name, list(shape), dtype, kind=kind).ap()
            if kind == "ExternalInput":
                ins.append(name)
            elif kind == "ExternalOutput":
                outs.append(name)
            return t
        for n, (sh, d_) in CONSTS.items():
            setattr(C, n, dt(n, sh, d_, "ExternalInput"))
        xin = dt("xT_in", [D, TL], F32, "ExternalInput")
        if do_attn_post:
            for n, sh in POST_W.items():
                setattr(C, n, dt(n, sh, F32, "ExternalInput"))
            for n, (sh, d_) in KSIDE_G.items():
                setattr(C, n, dt(n, sh, d_, "ExternalInput"))
            for n in QSIDE:
                sh, d_ = ACT_SET[n]
                setattr(C, n, dt("in_" + n, sh, d_, "ExternalInput"))
            C.maskb = dt("maskb", [16, 128, S], BF16, "Internal")
            C.ddall = dt("ddall", [128, 16], F32, "Internal")
            C.o_aT = dt("o_aT", [NH, 128, TL], BF16, "ExternalOutput" if debug else "Internal")
            C.o_bT = dt("o_bT", [NH, 128, TL], BF16, "ExternalOutput" if debug else "Internal")
            xout = dt("xT_out", [D, TL], F32, "ExternalOutput")
            declare_prep(fw, C, dt)
            phase_prep(fw, C, 0)
            phase_dsa_select(fw, C, 0)
            phase_dsa_attn(fw, C, 0)
            phase_moba(fw, C, 0)
            phase_post(fw, C, 0, xin, xout)
        else:
            xout = xin
        if do_proj:
            for n, sh in PROJ_W.items():
                setattr(C, n, dt(n, sh, F32, "ExternalInput"))
            for n, (sh, d_) in ACT_SET.items():
                setattr(C, n, dt("out_" + n, sh, d_, "ExternalOutput"))
            C.xT_res = xout
            phase_proj(fw, C, 0)
    return nc, ins, outs


def core_consts(c):
    d = {}
    d.update(host_consts_attn(c))
    d.update(host_consts_moba(c))
    d["d_ident"] = np.eye(128, dtype=np.float32).astype(NPBF)
    d["d_onesf"] = np.full((128, 128), 1.0 / D, np.float32)
    return d


def gather_kside(outs_per_core):
    g = {n: np.zeros(sh, NPBF if d_ == BF16 else np.float32) for n, (sh, d_) in KSIDE_G.items()}
    for c in range(NCORE):
        tok = own_tokens(c)
        o = outs_per_core[c]
        g["k_idxT_g"][:, tok] = o["out_k_idxT_own"]
        g["c_kvT_g"][:, :, tok] = o["out_c_kvT_own"]
        g["c_kv_g"][tok, :] = o["out_c_kv_own"]
        g["k_mT_g"][:, :, tok] = o["out_k_mT_own"]
        g["v_m_g"][tok, :] = o["out_v_m_own"]
        for k in range(8):
            g["k_meanT_g"][:, :, 8 * k + c] = o["out_k_meanT_own"][:, :, k]
    return g


def kernel(**inp):
    x = np.asarray(inp["x"], np.float32)[0]
    consts = [core_consts(c) for c in range(NCORE)]
    memT = np.ascontiguousarray(np.asarray(inp["mem"], np.float32)[0].T)

    def proj_w(l):
        d = {"w_in": inp["w_in"][l:l + 1], "w_uk": inp["w_uk"][l:l + 1]}
        d.update({k: v for k, v in host_consts_proj(inp, [l]).items() if k != "d_ident"})
        return d

    def post_w(l):
        d = {n: inp[n][l:l + 1] for n in ["w_uv", "w_o_dsa", "w_o_moba", "w_out", "w_q_mem", "w_kv_mem", "w_o_mem", "w_ffn_in", "w_ffn_out"]}
        hp = host_consts_post(inp, [l])
        d["d_lng"] = hp["d_lng"]; d["d_lnb"] = hp["d_lnb"]
        return d

    def run(nc, ins, maps):
        maps = [{k: np.ascontiguousarray(m[k]) for k in ins} for m in maps]
        return run_bass_kernel_spmd(nc, maps, core_ids=list(range(NCORE))).results

    xT = [np.ascontiguousarray(x[own_tokens(c)].T) for c in range(NCORE)]
    ncP, insP, _ = build_program(False, True)
    maps = []
    for c in range(NCORE):
        m = dict(consts[c]); m["memT"] = memT; m["xT_in"] = xT[c]; m.update(proj_w(0)); maps.append(m)
    res = run(ncP, insP, maps)
    ncM = None
    for l in range(DEPTH):
        last = (l == DEPTH - 1)
        if last:
            ncX, insX, _ = build_program(True, False)
        else:
            if ncM is None:
                ncM = build_program(True, True)
            ncX, insX, _ = ncM
        kg = gather_kside(res)
        maps = []
        for c in range(NCORE):
            m = dict(consts[c]); m["memT"] = memT; m["xT_in"] = xT[c]
            m.update(kg)
            for n in QSIDE:
                m["in_" + n] = res[c]["out_" + n]
            m.update(post_w(l))
            if not last:
                m.update(proj_w(l + 1))
            maps.append(m)
        res = run(ncX, insX, maps)
        xT = [np.asarray(res[c]["xT_out"]) for c in range(NCORE)]
    out = np.zeros((1, S, D), np.float32)
    for c in range(NCORE):
        out[0, own_tokens(c), :] = xT[c].T
    return out
```

```python
import contextlib
import numpy as np
import concourse.bass as bass
import concourse.mybir as mybir

F32 = mybir.dt.float32
BF16 = mybir.dt.bfloat16
AF = mybir.ActivationFunctionType
ALU = mybir.AluOpType
AX = mybir.AxisListType


class Eng:
    def __init__(self, name, h, sem, is_pe=False):
        self.name = name
        self.h = h
        self.sem = sem
        self.count = 0
        self.seen = {}
        self.is_pe = is_pe


class Res:
    __slots__ = ("name", "last_w", "readers", "sem", "semval")

    def __init__(self, name=""):
        self.name = name
        self.last_w = None
        self.readers = {}
        self.sem = None
        self.semval = 0


class Fw:
    def __init__(self, nc, es):
        self.nc = nc
        self.es = es
        self.engs = {}
        for name, h, pe in [("pe", nc.tensor, True), ("act", nc.scalar, False),
                            ("dve", nc.vector, False), ("pool", nc.gpsimd, False),
                            ("sp", nc.sync, False)]:
            sem = es.enter_context(nc.semaphore("sem_" + name))
            self.engs[name] = Eng(name, h, sem, pe)
        self.nsem = 5
        self.dpool = []
        self.dnext = 0
        self.phase_slots = []
        self.pes = None

    def begin_phase(self):
        self.pes = contextlib.ExitStack()
        self.pes.__enter__()
        self.dnext = 0
        self.phase_slots = []

    def end_phase(self):
        self.barrier()
        self.pes.__exit__(None, None, None)
        self.pes = None

    def barrier(self):
        sp = self.engs["sp"]
        for r in self.phase_slots:
            if r.last_w is not None and r.last_w[0] == "d":
                self._wait(sp, r.last_w)
            for t in r.readers.values():
                if t[0] == "d":
                    self._wait(sp, t)
        for e2 in self.engs.values():
            if e2 is not sp and e2.count > 0:
                self._wait(sp, ("e", e2, e2.count))
        inst = sp.h.nop()
        sp.count += 1
        inst.then_inc(sp.sem, 1)
        for e2 in self.engs.values():
            if e2 is not sp:
                self._wait(e2, ("e", sp, sp.count))

    def sbuf(self, name, shape, dtype):
        self.uid = getattr(self, "uid", 0) + 1
        t = self.pes.enter_context(self.nc.sbuf_tensor("%s_u%d" % (name, self.uid), list(shape), dtype))
        return t

    def psum(self, name, shape, dtype):
        self.uid = getattr(self, "uid", 0) + 1
        return self.pes.enter_context(self.nc.psum_tensor("%s_u%d" % (name, self.uid), list(shape), dtype))

    def dram(self, name, shape, dtype, kind="Internal"):
        return self.nc.dram_tensor(name, list(shape), dtype, kind=kind).ap()

    def _dma_sem(self, r):
        if r.sem is None:
            if self.dnext >= len(self.dpool):
                h = self.es.enter_context(self.nc.semaphore("dsem_%d" % self.nsem))
                self.nsem += 1
                self.dpool.append([h, 0])
            r.sem = self.dpool[self.dnext]
            self.dnext += 1
            self.phase_slots.append(r)
        return r.sem

    def _wait(self, eng, tok):
        kind = tok[0]
        if kind == "e":
            _, e2, n = tok
            if e2 is eng and eng.is_pe:
                return
            if eng.seen.get(e2.name, 0) >= n:
                return
            eng.h.wait_ge(e2.sem, n)
            eng.seen[e2.name] = n
        else:
            _, r, v = tok
            key = ("d", id(r.sem))
            if eng.seen.get(key, 0) >= v:
                return
            eng.h.wait_ge(r.sem[0], v)
            eng.seen[key] = v

    def _deps(self, eng, reads, writes):
        for r in reads:
            if r.last_w is not None:
                self._wait(eng, r.last_w)
        for w in writes:
            if w.last_w is not None:
                self._wait(eng, w.last_w)
            for t in w.readers.values():
                self._wait(eng, t)

    def _commit(self, tok, reads, writes):
        key = tok[1].name if tok[0] == "e" else ("d", id(tok[1]))
        for r in reads:
            r.readers[key] = tok
        for w in writes:
            w.last_w = tok
            w.readers = {}

    def op(self, engname, fn, reads=(), writes=()):
        eng = self.engs[engname]
        self._deps(eng, reads, writes)
        inst = fn(eng.h)
        eng.count += 1
        inst.then_inc(eng.sem, 1)
        tok = ("e", eng, eng.count)
        self._commit(tok, reads, writes)
        return inst

    def dma(self, out, in_, slot, reads=(), writes=(), q="sp", **kw):
        eng = self.engs[q]
        writes = list(writes)
        if slot not in writes:
            writes.append(slot)
        reads = [r for r in reads if r is not slot]
        self._deps(eng, reads, writes)
        sem = self._dma_sem(slot)
        inst = eng.h.dma_start(out=out, in_=in_, **kw)
        sem[1] += 16
        inst.then_inc(sem[0], 16)
        tok = ("d", slot, sem[1])
        self._commit(tok, reads, writes)
        return tok

    def wait_all(self, q="sp"):
        eng = self.engs[q]
        for e2 in self.engs.values():
            if e2.count > 0 and e2 is not eng:
                self._wait(eng, ("e", e2, e2.count))

    def wait_tok(self, q, tok):
        self._wait(self.engs[q], tok)


D = 2048
S = 16384
NCORE = 8
TL = S // NCORE
DEPTH = 4
HD = 128
NH = 8
KVL = 256
IH = 16
ID_ = 64
DFF = 5632
INW = 9552
ALPHA = (2.0 * DEPTH) ** 0.25
EPS = 1e-5
QSCALE = HD ** -0.5
NEG = -30000.0
BIG = 1.0e30
O_QD, O_CKV, O_QI, O_KI, O_WI, O_QM, O_KM, O_VM, O_GA, O_GB = 0, 1024, 1280, 2304, 2368, 2384, 3408, 4432, 5456, 7504


class Ctx:
    pass


def mk_psum(fw, n=8):
    banks = []
    for i in range(n):
        t = fw.psum("ps%d" % i, [128, 512], F32)
        banks.append((t, Res("ps%d" % i)))
    return banks


class Rot:
    def __init__(self, items):
        self.items = items
        self.i = 0

    def next(self):
        it = self.items[self.i % len(self.items)]
        self.i += 1
        return it


def mk_slots(fw, name, shape, dtype, n):
    return Rot([(fw.sbuf("%s%d" % (name, i), shape, dtype), Res("%s%d" % (name, i))) for i in range(n)])


def load_weight_slab(fw, C, src_ap, K, ncols):
    kc = K
    st, st_r = C.wst.next()
    wb, wb_r = C.wbf.next()
    fw.dma(out=st[:, 0:kc, 0:ncols], in_=src_ap.rearrange("(kc p) n -> p kc n", p=128), slot=st_r)
    fw.op("pool", lambda e: e.tensor_copy(out=wb[:, 0:kc, 0:ncols], in_=st[:, 0:kc, 0:ncols]),
          reads=[st_r], writes=[wb_r])
    return wb, wb_r


def phase_proj(fw, C, L):
    nc = fw.nc
    fw.begin_phase()
    banks = mk_psum(fw, 7)
    psb = Rot(banks)
    pst = fw.psum("pst", [128, 1024], BF16)
    pst_r = Res("pst")
    xb = fw.sbuf("xb", [128, 16, TL], BF16)
    xb_r = Res("xb")
    C.wst = mk_slots(fw, "wst", [128, 16, 256], F32, 2)
    C.wbf = mk_slots(fw, "wbf", [128, 16, 256], BF16, 2)
    xst = mk_slots(fw, "xst", [128, 16, 256], F32, 2)
    ob = mk_slots(fw, "ob", [128, 512], BF16, 4)
    of = mk_slots(fw, "of", [128, 512], F32, 3)
    qd = mk_slots(fw, "qd", [128, 512], BF16, 2)
    small = mk_slots(fw, "small", [128, 16], F32, 4)
    junk = fw.sbuf("junk", [128, 256], BF16); junk_r = Res("junk")
    tmpf = mk_slots(fw, "tmpf", [128, 256], F32, 2)
    wuk_st = fw.sbuf("wuk_st", [128, 8, 256], F32); wuk_st_r = Res()
    wuk = fw.sbuf("wuk", [128, 8, 256], BF16); wuk_r = Res()
    kmean = fw.sbuf("kmean", [128, 8, 8], F32); kmean_r = Res()
    cst = C.cst
    ident = fw.sbuf("ident", [128, 128], BF16); ident_r = Res()
    kvg = fw.sbuf("kvg", [128, 256], F32); kvg_r = Res()
    kig = fw.sbuf("kig", [128, 64], F32); kig_r = Res()
    kib = fw.sbuf("kib", [128, 64], F32); kib_r = Res()
    bg = fw.sbuf("bg", [128, 2, 16], F32); bg_r = Res()
    fw.dma(out=ident[:], in_=C.d_ident[:, :], slot=ident_r)
    fw.dma(out=kvg[:], in_=C.d_kvg[L], slot=kvg_r)
    fw.dma(out=kig[:], in_=C.d_kig[L], slot=kig_r)
    fw.dma(out=kib[:], in_=C.d_kib[L], slot=kib_r)
    fw.dma(out=bg[:], in_=C.d_bg[L], slot=bg_r)
    fw.dma(out=wuk_st[:], in_=C.w_uk[L].rearrange("h d c -> d h c"), slot=wuk_st_r)
    fw.op("pool", lambda e: e.tensor_copy(out=wuk[:], in_=wuk_st[:]), reads=[wuk_st_r], writes=[wuk_r])

    for j in range(TL // 256):
        st, st_r = xst.next()
        fw.dma(out=st[:], in_=C.xT_res.rearrange("(kc p) t -> p kc t", p=128)[:, :, j * 256:(j + 1) * 256], slot=st_r)
        eng = "dve" if j % 2 == 0 else "act"
        if eng == "dve":
            fw.op("dve", lambda e: e.tensor_copy(out=xb[:, :, j * 256:(j + 1) * 256], in_=st[:]), reads=[st_r], writes=[xb_r])
        else:
            fw.op("act", lambda e: e.copy(out=xb[:, :, j * 256:(j + 1) * 256], in_=st[:]), reads=[st_r], writes=[xb_r])

    w_in = C.w_in[L]
    evac_i = [0]

    def evac_copy(dst, dst_r, src, src_r, scale=None):
        i = evac_i[0]; evac_i[0] += 1
        if i % 2 == 0:
            if scale is None:
                fw.op("act", lambda e: e.copy(out=dst, in_=src), reads=[src_r], writes=[dst_r])
            else:
                fw.op("act", lambda e: e.mul(out=dst, in_=src, mul=float(scale)), reads=[src_r], writes=[dst_r])
        else:
            if scale is None:
                fw.op("dve", lambda e: e.tensor_copy(out=dst, in_=src), reads=[src_r], writes=[dst_r])
            else:
                fw.op("dve", lambda e: e.tensor_scalar(out=dst, in0=src, scalar1=float(scale), scalar2=None, op0=ALU.mult),
                      reads=[src_r], writes=[dst_r])

    def fm_matmul(wb, wb_r, blk, j):
        ps, ps_r = psb.next()
        for kc in range(16):
            fw.op("pe", lambda e: e.matmul(ps[:], lhsT=wb[:, kc, blk * 128:(blk + 1) * 128], rhs=xb[:, kc, j * 512:(j + 1) * 512],
                                           start=(kc == 0), stop=(kc == 15)),
                  reads=[wb_r, xb_r], writes=[ps_r])
        return ps, ps_r

    def tm_matmul(wb, wb_r, ncols, ts):
        ps, ps_r = psb.next()
        for kc in range(16):
            fw.op("pe", lambda e: e.matmul(ps[:, 0:ncols], lhsT=xb[:, kc, ts * 128:(ts + 1) * 128], rhs=wb[:, kc, 0:ncols],
                                           start=(kc == 0), stop=(kc == 15)),
                  reads=[wb_r, xb_r], writes=[ps_r])
        return ps, ps_r

    def sec0():
        for sl in range(4):
            wb, wb_r = load_weight_slab(fw, C, w_in[:, O_QD + sl * 256:O_QD + (sl + 1) * 256], 16, 256)
            for j in range(4):
                for blk in range(2):
                    h = sl * 2 + blk
                    ps, ps_r = fm_matmul(wb, wb_r, blk, j)
                    q, q_r = qd.next()
                    evac_copy(q[:], q_r, ps[:], ps_r)
                    for cc in range(2):
                        ps2, ps2_r = psb.next()
                        fw.op("pe", lambda e: e.matmul(ps2[:], lhsT=wuk[:, h, cc * 128:(cc + 1) * 128], rhs=q[:], start=True, stop=True),
                              reads=[wuk_r, q_r], writes=[ps2_r])
                        o, o_r = ob.next()
                        evac_copy(o[:], o_r, ps2[:], ps2_r, scale=QSCALE)
                        fw.dma(out=C.q_latT[cc, :, h, j * 512:(j + 1) * 512], in_=o[:], slot=o_r)

    if getattr(C, 'stop', 99) >= 0:
        sec0()
    def sec1():
        wb, wb_r = load_weight_slab(fw, C, w_in[:, O_CKV:O_CKV + 256], 16, 256)
        for ts in range(16):
            ps, ps_r = tm_matmul(wb, wb_r, 256, ts)
            sm, sm_r = small.next()
            fw.op("act", lambda e: e.activation(out=junk[:], in_=ps[:, 0:256], func=AF.Square, accum_out=sm[:, 0:1]),
                  reads=[ps_r], writes=[junk_r, sm_r])
            fw.op("dve", lambda e: e.tensor_scalar(out=sm[:, 1:2], in0=sm[:, 0:1], scalar1=1.0 / 256, scalar2=EPS, op0=ALU.mult, op1=ALU.add),
                  reads=[sm_r], writes=[sm_r])
            fw.op("act", lambda e: e.activation(out=sm[:, 2:3], in_=sm[:, 1:2], func=AF.Sqrt), reads=[sm_r], writes=[sm_r])
            fw.op("dve", lambda e: e.reciprocal(out=sm[:, 3:4], in_=sm[:, 2:3]), reads=[sm_r], writes=[sm_r])
            tf, tf_r = tmpf.next()
            fw.op("dve", lambda e: e.tensor_scalar(out=tf[:], in0=ps[:, 0:256], scalar1=sm[:, 3:4], scalar2=None, op0=ALU.mult),
                  reads=[ps_r, sm_r], writes=[tf_r])
            o, o_r = ob.next()
            fw.op("dve", lambda e: e.tensor_tensor(out=o[:, 0:256], in0=tf[:], in1=kvg[:], op=ALU.mult), reads=[tf_r, kvg_r], writes=[o_r])
            for cc in range(2):
                fw.op("pe", lambda e: e.transpose(pst[:, cc * 128:(cc + 1) * 128], o[:, cc * 128:(cc + 1) * 128], ident[:]),
                      reads=[o_r, ident_r], writes=[pst_r])
            fw.op("act", lambda e: e.copy(out=o[:, 256:512], in_=pst[:, 0:256]), reads=[pst_r], writes=[o_r])
            fw.dma(out=C.c_kv_own[ts * 128:(ts + 1) * 128, :], in_=o[:, 0:256], slot=o_r)
            fw.dma(out=C.c_kvT_own[:, :, ts * 128:(ts + 1) * 128].rearrange("cc p t -> p cc t"),
                   in_=o[:, 256:512].rearrange("p (cc t) -> p cc t", cc=2), slot=o_r)

    if getattr(C, 'stop', 99) >= 1:
        sec1()
    def sec2():
        for sl in range(4):
            wb, wb_r = load_weight_slab(fw, C, w_in[:, O_QI + sl * 256:O_QI + (sl + 1) * 256], 16, 256)
            for j in range(4):
                for blk in range(2):
                    ps, ps_r = fm_matmul(wb, wb_r, blk, j)
                    o, o_r = ob.next()
                    evac_copy(o[:], o_r, ps[:], ps_r)
                    fw.dma(out=C.q_idxT[sl * 2 + blk, :, j * 512:(j + 1) * 512], in_=o[:], slot=o_r)

    if getattr(C, 'stop', 99) >= 2:
        sec2()
    def sec3():
        wb, wb_r = load_weight_slab(fw, C, w_in[:, O_KI:O_KI + 80], 16, 80)
        for ts in range(16):
            ps, ps_r = tm_matmul(wb, wb_r, 80, ts)
            sm, sm_r = small.next()
            fw.op("dve", lambda e: e.bn_stats(out=sm[:, 0:6], in_=ps[:, 0:64]), reads=[ps_r], writes=[sm_r])
            fw.op("dve", lambda e: e.bn_aggr(out=sm[:, 6:8], in_=sm[:, 0:6]), reads=[sm_r], writes=[sm_r])
            fw.op("dve", lambda e: e.tensor_scalar(out=sm[:, 8:9], in0=sm[:, 7:8], scalar1=EPS, scalar2=None, op0=ALU.add), reads=[sm_r], writes=[sm_r])
            fw.op("act", lambda e: e.activation(out=sm[:, 9:10], in_=sm[:, 8:9], func=AF.Sqrt), reads=[sm_r], writes=[sm_r])
            fw.op("dve", lambda e: e.reciprocal(out=sm[:, 10:11], in_=sm[:, 9:10]), reads=[sm_r], writes=[sm_r])
            tf, tf_r = tmpf.next()
            fw.op("dve", lambda e: e.tensor_scalar(out=tf[:, 0:64], in0=ps[:, 0:64], scalar1=sm[:, 6:7], scalar2=sm[:, 10:11],
                                                   op0=ALU.subtract, op1=ALU.mult), reads=[ps_r, sm_r], writes=[tf_r])
            fw.op("dve", lambda e: e.tensor_tensor(out=tf[:, 64:128], in0=tf[:, 0:64], in1=kig[:], op=ALU.mult), reads=[tf_r, kig_r], writes=[tf_r])
            o, o_r = ob.next()
            fw.op("dve", lambda e: e.tensor_tensor(out=o[:, 0:64], in0=tf[:, 64:128], in1=kib[:], op=ALU.add), reads=[tf_r, kib_r], writes=[o_r])
            fw.op("pe", lambda e: e.transpose(pst[0:64, 0:128], o[:, 0:64], ident[:]), reads=[o_r, ident_r], writes=[pst_r])
            fw.op("act", lambda e: e.copy(out=o[0:64, 128:256], in_=pst[0:64, 0:128]), reads=[pst_r], writes=[o_r])
            fw.dma(out=C.k_idxT_own[:, ts * 128:(ts + 1) * 128], in_=o[0:64, 128:256], slot=o_r)
            f, f_r = of.next()
            fw.op("act", lambda e: e.mul(out=f[:, 0:16], in_=ps[:, 64:80], mul=1.0 / 32), reads=[ps_r], writes=[f_r])
            fw.dma(out=C.w_idx[ts * 128:(ts + 1) * 128, :], in_=f[:, 0:16], slot=f_r)

    if getattr(C, 'stop', 99) >= 3:
        sec3()
    def sec4():
        for sl in range(4):
            wb, wb_r = load_weight_slab(fw, C, w_in[:, O_QM + sl * 256:O_QM + (sl + 1) * 256], 16, 256)
            for j in range(4):
                for blk in range(2):
                    ps, ps_r = fm_matmul(wb, wb_r, blk, j)
                    o, o_r = ob.next()
                    evac_copy(o[:], o_r, ps[:], ps_r, scale=QSCALE)
                    fw.dma(out=C.q_mT[sl * 2 + blk, :, j * 512:(j + 1) * 512], in_=o[:], slot=o_r)
        for sl in range(4):
            wb, wb_r = load_weight_slab(fw, C, w_in[:, O_KM + sl * 256:O_KM + (sl + 1) * 256], 16, 256)
            for j in range(4):
                for blk in range(2):
                    h = sl * 2 + blk
                    ps, ps_r = fm_matmul(wb, wb_r, blk, j)
                    o, o_r = ob.next()
                    for s2 in range(2):
                        fw.op("act", lambda e: e.activation(out=o[:, s2 * 256:(s2 + 1) * 256], in_=ps[:, s2 * 256:(s2 + 1) * 256], func=AF.Copy,
                                                            accum_out=kmean[:, h, 2 * j + s2:2 * j + s2 + 1]),
                              reads=[ps_r], writes=[o_r, kmean_r])
                    fw.dma(out=C.k_mT_own[h, :, j * 512:(j + 1) * 512], in_=o[:], slot=o_r)
        fw.op("dve", lambda e: e.tensor_scalar(out=kmean[:], in0=kmean[:], scalar1=1.0 / 256, scalar2=None, op0=ALU.mult), reads=[kmean_r], writes=[kmean_r])
        fw.dma(out=C.k_meanT_own[:, :, :], in_=kmean[:], slot=kmean_r)

    if getattr(C, 'stop', 99) >= 4:
        sec4()
    def sec5():
        for sl in range(4):
            wb, wb_r = load_weight_slab(fw, C, w_in[:, O_VM + sl * 256:O_VM + (sl + 1) * 256], 16, 256)
            for ts in range(16):
                ps, ps_r = tm_matmul(wb, wb_r, 256, ts)
                o, o_r = ob.next()
                evac_copy(o[:, 0:256], o_r, ps[:, 0:256], ps_r)
                fw.dma(out=C.v_m_own[ts * 128:(ts + 1) * 128, sl * 256:(sl + 1) * 256], in_=o[:, 0:256], slot=o_r)

    if getattr(C, 'stop', 99) >= 5:
        sec5()
    def sec6():
        for gi, (off, dst) in enumerate([(O_GA, C.g_aT), (O_GB, C.g_bT)]):
            for sl in range(8):
                wb, wb_r = load_weight_slab(fw, C, w_in[:, off + sl * 256:off + (sl + 1) * 256], 16, 256)
                for j in range(4):
                    for blk in range(2):
                        fb = sl * 2 + blk
                        ps, ps_r = fm_matmul(wb, wb_r, blk, j)
                        f, f_r = of.next()
                        fw.op("act", lambda e: e.activation(out=f[:], in_=ps[:], func=AF.Sigmoid, bias=bg[:, gi, fb:fb + 1]),
                              reads=[ps_r, bg_r], writes=[f_r])
                        fw.dma(out=dst[fb, :, j * 512:(j + 1) * 512], in_=f[:], slot=f_r)

    if getattr(C, 'stop', 99) >= 6:
        sec6()
    fw.end_phase()


import ml_dtypes
NPBF = ml_dtypes.bfloat16


def own_tokens(c):
    return np.concatenate([np.arange(256 * (8 * k + c), 256 * (8 * k + c) + 256) for k in range(8)])


def declare(fw, C, name, shape, dtype, ext):
    kind = "Internal"
    if name in ext:
        kind = ext[name]
    t = fw.nc.dram_tensor(name, list(shape), dtype, kind=kind).ap()
    setattr(C, name, t)
    return t


def declare_proj_io(fw, C, nl, ext):
    declare(fw, C, "xT_res", [D, TL], F32, ext)
    declare(fw, C, "w_in", [nl, D, INW], F32, ext)
    declare(fw, C, "w_uk", [nl, NH, HD, KVL], F32, ext)
    declare(fw, C, "d_ident", [128, 128], BF16, ext)
    declare(fw, C, "d_kvg", [nl, 128, KVL], F32, ext)
    declare(fw, C, "d_kig", [nl, 128, ID_], F32, ext)
    declare(fw, C, "d_kib", [nl, 128, ID_], F32, ext)
    declare(fw, C, "d_bg", [nl, 128, 2, 16], F32, ext)
    declare(fw, C, "q_latT", [2, 128, NH, TL], BF16, ext)
    declare(fw, C, "q_idxT", [8, 128, TL], BF16, ext)
    declare(fw, C, "w_idx", [TL, IH], F32, ext)
    declare(fw, C, "k_idxT_own", [ID_, TL], BF16, ext)
    declare(fw, C, "c_kv_own", [TL, KVL], BF16, ext)
    declare(fw, C, "c_kvT_own", [2, 128, TL], BF16, ext)
    declare(fw, C, "q_mT", [NH, 128, TL], BF16, ext)
    declare(fw, C, "k_mT_own", [NH, 128, TL], BF16, ext)
    declare(fw, C, "k_meanT_own", [128, NH, 8], F32, ext)
    declare(fw, C, "v_m_own", [TL, NH * HD], BF16, ext)
    declare(fw, C, "g_aT", [16, 128, TL], F32, ext)
    declare(fw, C, "g_bT", [16, 128, TL], F32, ext)


def host_consts_proj(inp, layers):
    out = {}
    out["d_ident"] = np.eye(128, dtype=np.float32).astype(NPBF)
    out["d_kvg"] = np.ascontiguousarray(np.broadcast_to(inp["kv_norm_g"][layers][:, None, :], (len(layers), 128, KVL))).astype(np.float32)
    out["d_kig"] = np.ascontiguousarray(np.broadcast_to(inp["idx_k_norm_g"][layers][:, None, :], (len(layers), 128, ID_))).astype(np.float32)
    out["d_kib"] = np.ascontiguousarray(np.broadcast_to(inp["idx_k_norm_b"][layers][:, None, :], (len(layers), 128, ID_))).astype(np.float32)
    bg = inp["b_gate"][layers]
    out["d_bg"] = np.ascontiguousarray(bg.reshape(len(layers), 2, 16, 128).transpose(0, 3, 1, 2)).astype(np.float32)
    return out


NIT = 22


def declare_attn_io(fw, C, ext):
    declare(fw, C, "k_idxT_g", [ID_, S], BF16, ext)
    declare(fw, C, "c_kvT_g", [2, 128, S], BF16, ext)
    declare(fw, C, "c_kv_g", [S, KVL], BF16, ext)
    declare(fw, C, "k_mT_g", [NH, 128, S], BF16, ext)
    declare(fw, C, "v_m_g", [S, NH * HD], BF16, ext)
    declare(fw, C, "k_meanT_g", [128, NH, 64], F32, ext)
    declare(fw, C, "maskb", [16, 128, S], BF16, ext)
    declare(fw, C, "ddall", [128, 16], F32, ext)
    declare(fw, C, "o_aT", [NH, 128, TL], BF16, ext)
    declare(fw, C, "o_bT", [NH, 128, TL], BF16, ext)
    declare(fw, C, "d_tloc", [128, 16], F32, ext)
    declare(fw, C, "d_iota", [128, 2048], F32, ext)
    declare(fw, C, "d_pow2", [128, NIT], F32, ext)
    declare(fw, C, "d_slopes", [128, NH], F32, ext)
    declare(fw, C, "d_kb", [128, 128, NH], F32, ext)
    declare(fw, C, "d_ones", [128, 128], BF16, ext)
    declare(fw, C, "w_uv", [C.nl, NH, KVL, HD], F32, ext)


def host_consts_attn(c):
    out = {}
    tl = np.zeros((128, 16), np.float32)
    for qb in range(16):
        tl[:, qb] = 256 * c + 128 * (qb % 2) + np.arange(128)
    out["d_tloc"] = tl
    out["d_iota"] = np.ascontiguousarray(np.broadcast_to(np.arange(2048, dtype=np.float32)[None, :], (128, 2048)))
    p2 = np.array([2.0 ** (-i) for i in range(NIT)], np.float32); p2[0] = 1.0 + 1e-6
    out["d_pow2"] = np.ascontiguousarray(np.broadcast_to(p2[None, :], (128, NIT)))
    sl = np.array([2.0 ** (-(i + 1)) for i in range(NH)], np.float32)
    out["d_slopes"] = np.ascontiguousarray(np.broadcast_to(sl[None, :], (128, NH)))
    pos = (128 * np.arange(128)[None, :, None] + np.arange(128)[:, None, None]).astype(np.float32)
    out["d_kb"] = np.ascontiguousarray(pos * sl[None, None, :]).astype(np.float32)
    out["d_ones"] = np.ones((128, 128), np.float32).astype(NPBF)
    return out


def phase_dsa_select(fw, C, L):
    fw.begin_phase()
    banks = mk_psum(fw, 8)
    psL = Rot(banks[0:5]); psS = Rot(banks[5:8])
    kid = fw.sbuf("kid", [128, S], BF16); kid_r = Res()
    score = fw.sbuf("score", [128, S], F32); score_r = Res()
    mb = fw.sbuf("mb", [128, S], BF16); mb_r = Res()
    Rt = mk_slots(fw, "Rt", [128, 16, 256], BF16, 3)
    dg = mk_slots(fw, "dg", [128, 16, 128], BF16, 2)
    qi = mk_slots(fw, "qi", [128, 8, 128], BF16, 2)
    wi = mk_slots(fw, "wi", [128, 16], F32, 2)
    iota = fw.sbuf("iota", [128, 2048], F32); iota_r = Res()
    tmpz = fw.sbuf("tmpz", [128, 2048], F32); tmpz_r = Res()
    ident = fw.sbuf("ident", [128, 128], BF16); ident_r = Res()
    tloc = fw.sbuf("tloc", [128, 16], F32); tloc_r = Res()
    pow2 = fw.sbuf("pow2", [128, NIT], F32); pow2_r = Res()
    ddall = fw.sbuf("ddall", [128, 16], F32); ddall_r = Res()
    a32 = fw.sbuf("a32", [128, 512], F32); a32_r = Res()
    sm = mk_slots(fw, "sm", [128, 8 + NIT], F32, 2)
    negs = mk_slots(fw, "negs", [128, 2], F32, 3)
    mb2_r = Res()
    fw.dma(out=kid[0:64, :], in_=C.k_idxT_g[:, :], slot=kid_r)
    kid2_r = Res()
    fw.dma(out=kid[64:128, :], in_=C.k_idxT_g[:, :], slot=kid2_r)
    fw.dma(out=iota[:], in_=C.d_iota[:, :], slot=iota_r)
    fw.dma(out=ident[:], in_=C.d_ident[:, :], slot=ident_r)
    fw.dma(out=tloc[:], in_=C.d_tloc[:, :], slot=tloc_r)
    fw.dma(out=pow2[:], in_=C.d_pow2[:, :], slot=pow2_r)
    fw.op("dve", lambda e: e.memset(ddall[:], 0.0), writes=[ddall_r])
    ev = [0]
    for qb in range(C.nqb):
        k = qb // 2; hf = qb % 2
        tl0 = 256 * k + 128 * hf
        NK = 2048 * (k + 1)
        q, q_r = qi.next(); w, w_r = wi.next(); d, d_r = dg.next()
        fw.dma(out=q[:], in_=C.q_idxT[:, :, tl0:tl0 + 128].rearrange("b p t -> p b t"), slot=q_r)
        fw.dma(out=w[:], in_=C.w_idx[tl0:tl0 + 128, :], slot=w_r)
        for h in range(16):
            fw.op("pool", lambda e: e.tensor_scalar(out=d[:, h, :], in0=ident[:], scalar1=w[:, h:h + 1], scalar2=None, op0=ALU.mult),
                  reads=[ident_r, w_r], writes=[d_r])
        def stageL(ck):
            R, R_r = Rt.next()
            for h in range(16):
                blk = h // 2; po = (h % 2) * 64
                ps, ps_r = psL.next()
                fw.op("pe", lambda e: e.matmul(ps[:, 0:256], lhsT=q[po:po + 64, blk, :], rhs=kid[po:po + 64, ck * 256:(ck + 1) * 256], start=True, stop=True),
                      reads=[q_r, kid_r, kid2_r], writes=[ps_r])
                ev[0] += 1
                if ev[0] % 2 == 0:
                    fw.op("act", lambda e: e.activation(out=R[:, h, :], in_=ps[:, 0:256], func=AF.Relu), reads=[ps_r], writes=[R_r])
                else:
                    fw.op("dve", lambda e: e.tensor_scalar(out=R[:, h, :], in0=ps[:, 0:256], scalar1=0.0, scalar2=None, op0=ALU.max), reads=[ps_r], writes=[R_r])
            return R, R_r

        def stageS(ck, R, R_r):
            ps, ps_r = psS.next()
            for h in range(16):
                fw.op("pe", lambda e: e.matmul(ps[:, 0:256], lhsT=d[:, h, :], rhs=R[:, h, :], start=(h == 0), stop=(h == 15)),
                      reads=[d_r, R_r], writes=[ps_r])
            fw.op("act", lambda e: e.copy(out=score[:, ck * 256:(ck + 1) * 256], in_=ps[:, 0:256]), reads=[ps_r], writes=[score_r])
        nck = NK // 256
        cur = stageL(0)
        for ck in range(nck):
            nxt = stageL(ck + 1) if ck + 1 < nck else None
            stageS(ck, *cur)
            cur = nxt
        s, s_r = sm.next()
        fw.op("dve", lambda e: e.tensor_reduce(out=s[:, 0:1], in_=score[:, 0:NK], axis=AX.X, op=ALU.max, apply_absolute_value=True), reads=[score_r], writes=[s_r])
        fw.op("dve", lambda e: e.tensor_scalar(out=s[:, 8:8 + NIT], in0=pow2[:], scalar1=s[:, 0:1], scalar2=None, op0=ALU.mult), reads=[pow2_r, s_r], writes=[s_r])
        fw.op("dve", lambda e: e.tensor_scalar(out=s[:, 1:2], in0=s[:, 0:1], scalar1=-1.0, scalar2=None, op0=ALU.mult), reads=[s_r], writes=[s_r])
        fw.op("dve", lambda e: e.tensor_scalar(out=tmpz[:], in0=iota[:], scalar1=tloc[:, qb:qb + 1], scalar2=-BIG, op0=ALU.is_gt, op1=ALU.mult),
              reads=[iota_r, tloc_r], writes=[tmpz_r])
        fw.op("dve", lambda e: e.tensor_tensor(out=score[:, NK - 2048:NK], in0=score[:, NK - 2048:NK], in1=tmpz[:], op=ALU.add), reads=[tmpz_r, score_r], writes=[score_r])
        H = (int(0.42 * NK) // 32) * 32
        thrc = float(512 - (NK - H))
        for it in range(NIT):
            fw.op("dve", lambda e: e.tensor_tensor(out=s[:, 2:3], in0=s[:, 1:2], in1=s[:, 8 + it:9 + it], op=ALU.add), reads=[s_r], writes=[s_r])
            ng_, ng_r = negs.next()
            fw.op("dve", lambda e: e.tensor_scalar(out=ng_[:, 0:1], in0=s[:, 2:3], scalar1=-1.0, scalar2=None, op0=ALU.mult), reads=[s_r], writes=[ng_r])
            fw.op("act", lambda e: e.activation(out=mb[:, H:NK], in_=score[:, H:NK], func=AF.Sign, bias=ng_[:, 0:1], accum_out=ng_[:, 1:2]),
                  reads=[score_r, ng_r], writes=[mb2_r, ng_r])
            fw.op("dve", lambda e: e.tensor_scalar(out=mb[:, 0:H], in0=score[:, 0:H], scalar1=s[:, 2:3], scalar2=None, op0=ALU.is_ge, op1=ALU.add, accum_out=s[:, 3:4]),
                  reads=[score_r, s_r], writes=[mb_r, s_r])
            fw.op("dve", lambda e: e.scalar_tensor_tensor(out=s[:, 6:7], in0=s[:, 3:4], scalar=2.0, in1=ng_[:, 1:2], op0=ALU.mult, op1=ALU.add), reads=[s_r, ng_r], writes=[s_r])
            fw.op("dve", lambda e: e.scalar_tensor_tensor(out=s[:, 4:5], in0=s[:, 6:7], scalar=thrc, in1=s[:, 8 + it:9 + it], op0=ALU.is_ge, op1=ALU.mult), reads=[s_r], writes=[s_r])
            fw.op("dve", lambda e: e.tensor_tensor(out=s[:, 1:2], in0=s[:, 1:2], in1=s[:, 4:5], op=ALU.add), reads=[s_r], writes=[s_r])
        fw.op("dve", lambda e: e.tensor_scalar(out=mb[:, 0:NK], in0=score[:, 0:NK], scalar1=s[:, 1:2], scalar2=NEG, op0=ALU.is_lt, op1=ALU.mult),
              reads=[score_r, s_r], writes=[mb_r, mb2_r])
        fw.dma(out=C.maskb[qb, :, 0:NK], in_=mb[:, 0:NK], slot=mb_r, reads=[mb2_r])
        ng = NK // 32
        fw.op("dve", lambda e: e.tensor_reduce(out=a32[:, 0:ng], in_=mb[:, 0:NK].rearrange("p (g e) -> p g e", e=32), axis=AX.X, op=ALU.max), reads=[mb_r, mb2_r], writes=[a32_r])
        fw.op("dve", lambda e: e.tensor_scalar(out=tmpz[:, 0:ng], in0=iota[:, 0:ng], scalar1=1.0, scalar2=None, op0=ALU.add), reads=[iota_r], writes=[tmpz_r])
        fw.op("dve", lambda e: e.scalar_tensor_tensor(out=a32[:, 0:ng], in0=a32[:, 0:ng], scalar=-1.0, in1=tmpz[:, 0:ng], op0=ALU.is_ge, op1=ALU.mult), reads=[a32_r, tmpz_r], writes=[a32_r])
        fw.op("dve", lambda e: e.tensor_reduce(out=s[:, 5:6], in_=a32[:, 0:ng], axis=AX.X, op=ALU.max), reads=[a32_r], writes=[s_r])
        fw.op("dve", lambda e: e.tensor_scalar(out=ddall[:, qb:qb + 1], in0=s[:, 5:6], scalar1=-32.0, scalar2=1.0, op0=ALU.mult, op1=ALU.add), reads=[s_r], writes=[ddall_r])
    fw.dma(out=C.ddall[:, :], in_=ddall[:], slot=ddall_r)
    fw.end_phase()


def phase_dsa_attn(fw, C, L):
    fw.begin_phase()
    banks = mk_psum(fw, 8)
    psT = Rot([banks[0], banks[1], banks[7]]); psO = [banks[2], banks[3]]; psD = banks[4]; psU = Rot(banks[5:7])
    ckT = fw.sbuf("ckT", [128, 2, S], BF16); ckT_r = Res()
    ckv = fw.sbuf("ckv", [128, 128, KVL], BF16); ckv_r = Res()
    ql = mk_slots(fw, "ql", [128, 2, NH, 128], BF16, 2)
    mbt = mk_slots(fw, "mbt", [128, 2048], BF16, 3)
    pt = mk_slots(fw, "pt", [128, 512], BF16, 4)
    Er = mk_slots(fw, "Er", [128, 512], BF16, 2)
    E4 = fw.sbuf("E4", [128, 512], BF16); E4_r = Res()
    ident = fw.sbuf("ident", [128, 128], BF16); ident_r = Res()
    ones = fw.sbuf("ones", [128, 128], BF16); ones_r = Res()
    kb = fw.sbuf("kb", [128, 128, NH], F32); kb_r = Res()
    slopes = fw.sbuf("slopes", [128, NH], F32); slopes_r = Res()
    ddall = fw.sbuf("ddall", [128, 16], F32); ddall_r = Res()
    rp = mk_slots(fw, "rp", [128, NH], F32, 2)
    rec = mk_slots(fw, "rec", [128, 512], F32, 2)
    olat = mk_slots(fw, "olat", [128, 2, 512], BF16, 2)
    oa = mk_slots(fw, "oa", [128, 512], BF16, 2)
    wuv_st = fw.sbuf("wuv_st", [128, NH, 2, 128], F32); wuv_st_r = Res()
    wuv = fw.sbuf("wuv", [128, NH, 2, 128], BF16); wuv_r = Res()
    fw.dma(out=ckT[:], in_=C.c_kvT_g.rearrange("cc p s -> p cc s"), slot=ckT_r)
    fw.dma(out=ckv[:], in_=C.c_kv_g.rearrange("(b p) c -> p b c", p=128), slot=ckv_r)
    fw.dma(out=ident[:], in_=C.d_ident[:, :], slot=ident_r)
    fw.dma(out=ones[:], in_=C.d_ones[:, :], slot=ones_r)
    fw.dma(out=kb[:], in_=C.d_kb[:, :, :], slot=kb_r)
    fw.dma(out=slopes[:], in_=C.d_slopes[:, :], slot=slopes_r)
    fw.dma(out=ddall[:], in_=C.ddall[:, :], slot=ddall_r)
    fw.dma(out=wuv_st[:], in_=C.w_uv[L].rearrange("h (cc p) d -> p h cc d", p=128), slot=wuv_st_r)
    fw.op("pool", lambda e: e.tensor_copy(out=wuv[:], in_=wuv_st[:]), reads=[wuv_st_r], writes=[wuv_r])
    for i in range(4):
        fw.op("pool", lambda e: e.tensor_copy(out=E4[:, i * 128:(i + 1) * 128], in_=ident[:]), reads=[ident_r], writes=[E4_r])
    for qb in range(C.nqb):
        k = qb // 2; hf = qb % 2
        tl0 = 256 * k + 128 * hf
        NK = 2048 * (k + 1)
        q, q_r = ql.next()
        for cc in range(2):
            fw.dma(out=q[:, cc, :, :], in_=C.q_latT[cc, :, :, tl0:tl0 + 128], slot=q_r)
        r, r_r = rp.next()
        fw.op("dve", lambda e: e.tensor_scalar(out=r[:], in0=slopes[:], scalar1=ddall[:, qb:qb + 1], scalar2=None, op0=ALU.mult), reads=[slopes_r, ddall_r], writes=[r_r])
        for g in range(2):
            er, er_r = Er.next()
            for hh in range(4):
                h = g * 4 + hh
                fw.op("dve", lambda e: e.tensor_scalar(out=er[:, hh * 128:(hh + 1) * 128], in0=ident[:], scalar1=r[:, h:h + 1], scalar2=None, op0=ALU.mult),
                      reads=[ident_r, r_r], writes=[er_r])
            nkb = NK // 128
            mcur = [None]

            def stageA(jk):
                if jk % 16 == 0:
                    mcur[0] = mbt.next()
                    fw.dma(out=mcur[0][0][:], in_=C.maskb[qb, :, jk * 128:jk * 128 + 2048], slot=mcur[0][1])
                m, m_r = mcur[0]
                ps, ps_r = psT.next()
                rhs_q = [q[:, cc, g * 4:(g + 1) * 4, :] for cc in range(2)]
                fw.op("pe", lambda e: e.matmul(ps[:], lhsT=ckT[:, 0, jk * 128:(jk + 1) * 128], rhs=rhs_q[0], start=True, stop=False), reads=[ckT_r, q_r], writes=[ps_r])
                fw.op("pe", lambda e: e.matmul(ps[:], lhsT=ckT[:, 1, jk * 128:(jk + 1) * 128], rhs=rhs_q[1], start=False, stop=False), reads=[ckT_r, q_r], writes=[ps_r])
                fw.op("pe", lambda e: e.matmul(ps[:], lhsT=m[:, (jk % 16) * 128:(jk % 16 + 1) * 128], rhs=E4[:], start=False, stop=False), reads=[m_r, E4_r], writes=[ps_r])
                fw.op("pe", lambda e: e.matmul(ps[:], lhsT=ones[:], rhs=er[:], start=False, stop=True), reads=[ones_r, er_r], writes=[ps_r])
                p, p_r = pt.next()
                for hh in range(4):
                    h = g * 4 + hh
                    fw.op("act", lambda e: e.activation(out=p[:, hh * 128:(hh + 1) * 128], in_=ps[:, hh * 128:(hh + 1) * 128], func=AF.Exp, bias=kb[:, jk, h:h + 1]),
                          reads=[ps_r, kb_r], writes=[p_r])
                return p, p_r

            def stageB(jk, p, p_r):
                for cc in range(2):
                    fw.op("pe", lambda e: e.matmul(psO[cc][0][:], lhsT=ckv[:, jk, cc * 128:(cc + 1) * 128], rhs=p[:], start=(jk == 0), stop=(jk == nkb - 1)),
                          reads=[ckv_r, p_r], writes=[psO[cc][1]])
                fw.op("pe", lambda e: e.matmul(psD[0][:], lhsT=ones[:], rhs=p[:], start=(jk == 0), stop=(jk == nkb - 1)), reads=[ones_r, p_r], writes=[psD[1]])
            cur = stageA(0)
            for jk in range(nkb):
                nxt = stageA(jk + 1) if jk + 1 < nkb else None
                stageB(jk, *cur)
                cur = nxt
            rc, rc_r = rec.next()
            fw.op("dve", lambda e: e.reciprocal(out=rc[:], in_=psD[0][:]), reads=[psD[1]], writes=[rc_r])
            ol, ol_r = olat.next()
            for cc in range(2):
                fw.op("dve", lambda e: e.tensor_tensor(out=ol[:, cc, :], in0=psO[cc][0][:], in1=rc[:], op=ALU.mult), reads=[psO[cc][1], rc_r], writes=[ol_r])
            pu, pu_r = psU.next()
            for hh in range(4):
                h = g * 4 + hh
                for cc in range(2):
                    fw.op("pe", lambda e: e.matmul(pu[:, hh * 128:(hh + 1) * 128], lhsT=wuv[:, h, cc, :], rhs=ol[:, cc, hh * 128:(hh + 1) * 128], start=(cc == 0), stop=(cc == 1)),
                          reads=[wuv_r, ol_r], writes=[pu_r])
            o, o_r = oa.next()
            fw.op("act", lambda e: e.copy(out=o[:], in_=pu[:]), reads=[pu_r], writes=[o_r])
            fw.dma(out=C.o_aT[g * 4:(g + 1) * 4, :, tl0:tl0 + 128].rearrange("h p t -> p h t"), in_=o[:].rearrange("p (h t) -> p h t", h=4), slot=o_r)
    fw.end_phase()


def declare_moba_io(fw, C, ext):
    declare(fw, C, "d_pastneg", [128, 8, 64], F32, ext)
    declare(fw, C, "d_pastind", [128, 8, 64], F32, ext)
    declare(fw, C, "d_alq", [128, 2, NH], F32, ext)
    declare(fw, C, "d_zc", [128, 32, 128], BF16, ext)
    declare(fw, C, "d_kbm", [128, NH, 128], F32, ext)


def host_consts_moba(c):
    out = {}
    pn = np.zeros((128, 8, 64), np.float32); pi = np.zeros((128, 8, 64), np.float32)
    for k in range(8):
        own = 8 * k + c
        pn[:, k, own:] = -BIG
        pi[:, k, :own] = 1.0
    out["d_pastneg"] = pn; out["d_pastind"] = pi
    sl = np.array([2.0 ** (-(i + 1)) for i in range(NH)], np.float32)
    p = np.arange(128, dtype=np.float32)
    alq = np.zeros((128, 2, NH), np.float32)
    for qh in range(2):
        alq[:, qh, :] = (255 - 128 * qh - p)[:, None] * sl[None, :]
    out["d_alq"] = alq
    zc = np.zeros((128, 8, 2, 2, 128), np.float32)
    for cp in range(8):
        for qh in range(2):
            for j in range(2):
                if cp > c:
                    zc[:, cp, qh, j, :] = NEG
                elif cp == c:
                    kpos = 128 * j + np.arange(128)[None, :]
                    qpos = 128 * qh + np.arange(128)[:, None]
                    zc[:, cp, qh, j, :] = np.where(kpos <= qpos, 0.0, NEG)
    out["d_zc"] = zc.reshape(128, 32, 128).astype(NPBF)
    m = np.arange(128, dtype=np.float32)
    kbm = (p[:, None, None] + 128 * (m[None, None, :] - 112) - 256 * c - 255) * sl[None, :, None]
    out["d_kbm"] = kbm.astype(np.float32)
    return out


def phase_moba(fw, C, L):
    fw.begin_phase()
    banks = mk_psum(fw, 8)
    psT = Rot(banks[0:3]); psO = banks[3]; psD = banks[4]; psG = Rot(banks[5:7])
    kTs = mk_slots(fw, "kT", [128, S], BF16, 2)
    vhs = mk_slots(fw, "vh", [128, 128, 128], BF16, 2)
    qTs = mk_slots(fw, "qT", [128, TL], BF16, 2)
    kmf = fw.sbuf("kmf", [128, NH, 64], F32); kmf_r = Res()
    kmb = fw.sbuf("kmb", [128, NH, 64], BF16); kmb_r = Res()
    ident = fw.sbuf("ident", [128, 128], BF16); ident_r = Res()
    ones = fw.sbuf("ones", [128, 128], BF16); ones_r = Res()
    pastneg = fw.sbuf("pastneg", [128, 8, 64], F32); pastneg_r = Res()
    pastind = fw.sbuf("pastind", [128, 8, 64], F32); pastind_r = Res()
    alq = fw.sbuf("alq", [128, 2, NH], F32); alq_r = Res()
    zc = fw.sbuf("zc", [128, 32, 128], BF16); zc_r = Res()
    kbm = fw.sbuf("kbm", [128, NH, 128], F32); kbm_r = Res()
    gss = mk_slots(fw, "gs", [128, 64 + 64 + 16], F32, 2)
    sbs = mk_slots(fw, "sb", [128, 2, 64], BF16, 2)
    bzs = mk_slots(fw, "bz", [128, 128], BF16, 8)
    pts = mk_slots(fw, "pt", [128, 256], BF16, 4)
    recs = mk_slots(fw, "rec", [128, 256], F32, 2)
    obs = mk_slots(fw, "ob", [128, 256], BF16, 2)
    for t, r, src in [(kmf, kmf_r, C.k_meanT_g[:, :, :]), (ident, ident_r, C.d_ident[:, :]), (ones, ones_r, C.d_ones[:, :]),
                      (pastneg, pastneg_r, C.d_pastneg[:, :, :]), (pastind, pastind_r, C.d_pastind[:, :, :]), (alq, alq_r, C.d_alq[:, :, :]),
                      (zc, zc_r, C.d_zc[:, :, :]), (kbm, kbm_r, C.d_kbm[:, :, :])]:
        fw.dma(out=t[:], in_=src, slot=r)
    fw.op("dve", lambda e: e.tensor_copy(out=kmb[:], in_=kmf[:]), reads=[kmf_r], writes=[kmb_r])
    for h in range(C.nheads):
        kT, kT_r = kTs.next(); vh, vh_r = vhs.next(); qT, qT_r = qTs.next()
        fw.dma(out=kT[:], in_=C.k_mT_g[h, :, :], slot=kT_r)
        vsrc = C.v_m_g.rearrange("(b p) (h d) -> p b h d", p=128, h=NH)
        for half in range(2):
            fw.dma(out=vh[:, half * 64:(half + 1) * 64, :], in_=vsrc[:, half * 64:(half + 1) * 64, h, :], slot=vh_r)
        fw.dma(out=qT[:], in_=C.q_mT[h, :, :], slot=qT_r)
        for k in range(C.nslots):
            sb, sb_r = sbs.next()
            for qh in range(2):
                pg, pg_r = psG.next()
                t0 = 256 * k + 128 * qh
                fw.op("pe", lambda e: e.matmul(pg[:, 0:64], lhsT=qT[:, t0:t0 + 128], rhs=kmb[:, h, :], start=True, stop=True), reads=[qT_r, kmb_r], writes=[pg_r])
                gs, gs_r = gss.next()
                fw.op("dve", lambda e: e.tensor_tensor(out=gs[:, 0:64], in0=pg[:, 0:64], in1=pastneg[:, k, :], op=ALU.add), reads=[pg_r, pastneg_r], writes=[gs_r])
                fw.op("dve", lambda e: e.max(out=gs[:, 128:136], in_=gs[:, 0:64]), reads=[gs_r], writes=[gs_r])
                fw.op("dve", lambda e: e.tensor_scalar(out=gs[:, 136:137], in0=gs[:, 130:131], scalar1=-1e29, scalar2=None, op0=ALU.max), reads=[gs_r], writes=[gs_r])
                fw.op("dve", lambda e: e.scalar_tensor_tensor(out=gs[:, 64:128], in0=gs[:, 0:64], scalar=gs[:, 136:137], in1=pastind[:, k, :], op0=ALU.is_lt, op1=ALU.mult),
                      reads=[gs_r, pastind_r], writes=[gs_r])
                fw.op("dve", lambda e: e.tensor_scalar(out=sb[:, qh, :], in0=gs[:, 64:128], scalar1=NEG, scalar2=alq[:, qh, h:h + 1], op0=ALU.mult, op1=ALU.add),
                      reads=[gs_r, alq_r], writes=[sb_r])
            nblk = 8 * k + 8

            def stageA(j2):
                n = j2 // 2; j = j2 % 2
                ps, ps_r = psT.next()
                fw.op("pe", lambda e: e.matmul(ps[:, 0:256], lhsT=kT[:, j2 * 128:(j2 + 1) * 128], rhs=qT[:, 256 * k:256 * k + 256], start=True, stop=False),
                      reads=[kT_r, qT_r], writes=[ps_r])
                for qh in range(2):
                    if n < 8 * k:
                        fw.op("pe", lambda e: e.matmul(ps[:, qh * 128:(qh + 1) * 128], lhsT=sb[:, qh, n:n + 1].to_broadcast([128, 128]), rhs=ident[:], start=False, stop=(qh == 1)),
                              reads=[sb_r, ident_r], writes=[ps_r])
                    else:
                        cp = n - 8 * k
                        bz, bz_r = bzs.next()
                        fw.op("dve", lambda e: e.tensor_scalar(out=bz[:], in0=zc[:, cp * 4 + qh * 2 + j, :], scalar1=sb[:, qh, n:n + 1], scalar2=None, op0=ALU.add),
                              reads=[zc_r, sb_r], writes=[bz_r])
                        fw.op("pe", lambda e: e.matmul(ps[:, qh * 128:(qh + 1) * 128], lhsT=bz[:], rhs=ident[:], start=False, stop=(qh == 1)),
                              reads=[bz_r, ident_r], writes=[ps_r])
                p, p_r = pts.next()
                mi = j2 - 16 * k + 112
                fw.op("act", lambda e: e.activation(out=p[:], in_=ps[:, 0:256], func=AF.Exp, bias=kbm[:, h, mi:mi + 1]), reads=[ps_r, kbm_r], writes=[p_r])
                return p, p_r

            def stageB(j2, p, p_r):
                first = (j2 == 0); last = (j2 == 2 * nblk - 1)
                fw.op("pe", lambda e: e.matmul(psO[0][:, 0:256], lhsT=vh[:, j2, :], rhs=p[:], start=first, stop=last), reads=[vh_r, p_r], writes=[psO[1]])
                fw.op("pe", lambda e: e.matmul(psD[0][:, 0:256], lhsT=ones[:], rhs=p[:], start=first, stop=last), reads=[ones_r, p_r], writes=[psD[1]])
            cur = stageA(0)
            for j2 in range(2 * nblk):
                nxt = stageA(j2 + 1) if j2 + 1 < 2 * nblk else None
                stageB(j2, *cur)
                cur = nxt
            rc, rc_r = recs.next()
            fw.op("dve", lambda e: e.reciprocal(out=rc[:], in_=psD[0][:, 0:256]), reads=[psD[1]], writes=[rc_r])
            o, o_r = obs.next()
            fw.op("dve", lambda e: e.tensor_tensor(out=o[:], in0=psO[0][:, 0:256], in1=rc[:], op=ALU.mult), reads=[psO[1], rc_r], writes=[o_r])
            fw.dma(out=C.o_bT[h, :, 256 * k:256 * k + 256], in_=o[:], slot=o_r)
    fw.end_phase()


TT = 256


def declare_post_io(fw, C, ext):
    nl = C.nl
    declare(fw, C, "w_o_dsa", [nl, 1024, D], F32, ext)
    declare(fw, C, "w_o_moba", [nl, 1024, D], F32, ext)
    declare(fw, C, "w_out", [nl, D, D], F32, ext)
    declare(fw, C, "w_q_mem", [nl, D, 512], F32, ext)
    declare(fw, C, "w_kv_mem", [nl, D, 1024], F32, ext)
    declare(fw, C, "w_o_mem", [nl, 512, D], F32, ext)
    declare(fw, C, "w_ffn_in", [nl, D, 2 * DFF], F32, ext)
    declare(fw, C, "w_ffn_out", [nl, DFF, D], F32, ext)
    declare(fw, C, "d_lng", [nl, 128, 3, 16], F32, ext)
    declare(fw, C, "d_lnb", [nl, 128, 3, 16], F32, ext)
    declare(fw, C, "memT", [D, 256], F32, ext)
    declare(fw, C, "d_onesf", [128, 128], F32, ext)
    declare(fw, C, "xT_out", [D, TL], F32, ext)


def host_consts_post(inp, layers):
    out = {}
    nl = len(layers)
    out["d_lng"] = np.ascontiguousarray(inp["ln_g"][layers].reshape(nl, 3, 16, 128).transpose(0, 3, 1, 2)).astype(np.float32)
    out["d_lnb"] = np.ascontiguousarray(inp["ln_b"][layers].reshape(nl, 3, 16, 128).transpose(0, 3, 1, 2)).astype(np.float32)
    out["memT"] = np.ascontiguousarray(inp["mem"][0].T).astype(np.float32)
    out["d_onesf"] = np.full((128, 128), 1.0 / D, np.float32)
    return out


POST_MATS = [("w_o_dsa", 8, 16), ("w_o_moba", 8, 16), ("w_out", 16, 16), ("w_q_mem", 16, 4), ("w_kv_mem", 16, 8), ("w_o_mem", 4, 16),
             ("w_ffn_in", 16, 88), ("w_ffn_out", 44, 16)]


def declare_prep(fw, C, dt):
    for name, K, nb in POST_MATS:
        setattr(C, "p_" + name, dt("p_" + name, [nb, 128, K, 128], BF16, "Internal"))


def phase_prep(fw, C, L):
    fw.begin_phase()
    st = mk_slots(fw, "pst", [128, 16, 512], F32, 2)
    bf = mk_slots(fw, "pbf", [128, 16, 512], BF16, 2)
    i = 0
    for name, K, nb in POST_MATS:
        src = getattr(C, name)[L]
        dst = getattr(C, "p_" + name)
        ncols = nb * 128
        for c0 in range(0, ncols, 512):
            cw = min(512, ncols - c0)
            for k0 in range(0, K, 16):
                kw = min(16, K - k0)
                s_, s_r = st.next(); b_, b_r = bf.next()
                fw.dma(out=s_[:, 0:kw, 0:cw], in_=src[k0 * 128:(k0 + kw) * 128, c0:c0 + cw].rearrange("(kc p) n -> p kc n", p=128), slot=s_r)
                eng = ["pool", "dve", "act"][i % 3]; i += 1
                if eng == "act":
                    fw.op("act", lambda e: e.copy(out=b_[:, 0:kw, 0:cw], in_=s_[:, 0:kw, 0:cw]), reads=[s_r], writes=[b_r])
                else:
                    fw.op(eng, lambda e: e.tensor_copy(out=b_[:, 0:kw, 0:cw], in_=s_[:, 0:kw, 0:cw]), reads=[s_r], writes=[b_r])
                for j in range(cw // 128):
                    fw.dma(out=dst[c0 // 128 + j, :, k0:k0 + kw, :], in_=b_[:, 0:kw, j * 128:(j + 1) * 128], slot=b_r)
    fw.end_phase()


def phase_post(fw, C, L, x_in, x_out):
    fw.begin_phase()
    banks = mk_psum(fw, 8)
    psb = Rot(banks[0:4]); psM = banks[4]; psQ = banks[5]; psO = banks[6]; psD = banks[7]
    wbf = mk_slots(fw, "wbf2", [128, 44, 128], BF16, 4)
    xres = fw.sbuf("xres", [128, 16, TT], F32); xres_r = Res()
    xb = fw.sbuf("xb", [128, 16, TT], BF16); xb_r = Res()
    z = fw.sbuf("z", [128, 16, TT], F32); z_r = Res()
    zsq = fw.sbuf("zsq", [128, 16, TT], F32); zsq_r = Res()
    oa = fw.sbuf("oa", [128, 8, TT], BF16); oa_r = Res()
    obm = fw.sbuf("obm", [128, 8, TT], BF16); obm_r = Res()
    mT = fw.sbuf("mT", [128, 16, TT], BF16); mT_r = Res()
    aT = fw.sbuf("aT", [128, 44, TT], BF16); aT_r = Res()
    gts = mk_slots(fw, "gt", [128, 2, TT], F32, 2)
    t12 = mk_slots(fw, "t12", [128, 2, TT], F32, 2)
    sgs = mk_slots(fw, "sg", [128, TT], F32, 2)
    stat = fw.sbuf("stat", [128, 4, TT], F32); stat_r = Res()
    qmT = fw.sbuf("qmT", [128, 4, TT], BF16); qmT_r = Res()
    omT = fw.sbuf("omT", [128, 4, TT], BF16); omT_r = Res()
    pts = mk_slots(fw, "ptm", [128, TT], BF16, 2)
    recs = mk_slots(fw, "recm", [128, TT], F32, 2)
    kmemT = fw.sbuf("kmemT", [128, 4, 256], BF16); kmemT_r = Res()
    vmem = fw.sbuf("vmem", [128, 2, 512], BF16); vmem_r = Res()
    memst = z; memst_r = z_r
    memb = mT; memb_r = mT_r
    lng = fw.sbuf("lng", [128, 3, 16], F32); lng_r = Res()
    lnb = fw.sbuf("lnb", [128, 3, 16], F32); lnb_r = Res()
    onesf = fw.sbuf("onesf", [128, 128], F32); onesf_r = Res()
    ones = fw.sbuf("ones", [128, 128], BF16); ones_r = Res()
    fw.dma(out=lng[:], in_=C.d_lng[L], slot=lng_r)
    fw.dma(out=lnb[:], in_=C.d_lnb[L], slot=lnb_r)
    fw.dma(out=onesf[:], in_=C.d_onesf[:, :], slot=onesf_r)
    fw.dma(out=ones[:], in_=C.d_ones[:, :], slot=ones_r)
    fw.dma(out=memst[:], in_=C.memT.rearrange("(kc p) m -> p kc m", p=128), slot=memst_r)
    fw.op("pool", lambda e: e.tensor_copy(out=memb[:], in_=memst[:]), reads=[memst_r], writes=[memb_r])

    def slab(name, nb, K):
        wb, wb_r = wbf.next()
        fw.dma(out=wb[:, 0:K, :], in_=getattr(C, "p_" + name)[nb, :, :, :], slot=wb_r)
        return wb, wb_r

    def mm(wb, wb_r, K, rhs_t, rhs_r, n=TT):
        ps, ps_r = psb.next()
        for kc in range(K):
            fw.op("pe", lambda e: e.matmul(ps[:, 0:n], lhsT=wb[:, kc, :], rhs=rhs_t[:, kc, 0:n], start=(kc == 0), stop=(kc == K - 1)),
                  reads=[wb_r, rhs_r], writes=[ps_r])
        return ps, ps_r

    for hq in range(4):
        wb, wb_r = slab("w_kv_mem", hq, 16)
        ps, ps_r = mm(wb, wb_r, 16, memb, memb_r, 256)
        fw.op("act", lambda e: e.copy(out=kmemT[:, hq, :], in_=ps[:, 0:256]), reads=[ps_r], writes=[kmemT_r])
    for hq in range(4):
        wb, wb_r = slab("w_kv_mem", 4 + hq, 16)
        for ms in range(2):
            ps, ps_r = psb.next()
            for kc in range(16):
                fw.op("pe", lambda e: e.matmul(ps[:, 0:128], lhsT=memb[:, kc, ms * 128:(ms + 1) * 128], rhs=wb[:, kc, :], start=(kc == 0), stop=(kc == 15)),
                      reads=[memb_r, wb_r], writes=[ps_r])
            fw.op("act", lambda e: e.copy(out=vmem[:, ms, hq * 128:(hq + 1) * 128], in_=ps[:, 0:128]), reads=[ps_r], writes=[vmem_r])

    def layer_norm(idx):
        fw.op("act", lambda e: e.activation(out=zsq[:], in_=z[:], func=AF.Square), reads=[z_r], writes=[zsq_r])
        for nb in range(16):
            fw.op("pe", lambda e: e.matmul(psM[0][:, 0:TT], lhsT=onesf[:], rhs=z[:, nb, :], start=(nb == 0), stop=(nb == 15)), reads=[onesf_r, z_r], writes=[psM[1]])
        for nb in range(16):
            fw.op("pe", lambda e: e.matmul(psQ[0][:, 0:TT], lhsT=onesf[:], rhs=zsq[:, nb, :], start=(nb == 0), stop=(nb == 15)), reads=[onesf_r, zsq_r], writes=[psQ[1]])
        fw.op("act", lambda e: e.copy(out=stat[:, 0, :], in_=psM[0][:, 0:TT]), reads=[psM[1]], writes=[stat_r])
        fw.op("dve", lambda e: e.tensor_tensor(out=stat[:, 1, :], in0=stat[:, 0, :], in1=stat[:, 0, :], op=ALU.mult), reads=[stat_r], writes=[stat_r])
        fw.op("dve", lambda e: e.tensor_tensor(out=stat[:, 2, :], in0=psQ[0][:, 0:TT], in1=stat[:, 1, :], op=ALU.subtract), reads=[psQ[1], stat_r], writes=[stat_r])
        fw.op("dve", lambda e: e.tensor_scalar(out=stat[:, 2, :], in0=stat[:, 2, :], scalar1=EPS, scalar2=None, op0=ALU.add), reads=[stat_r], writes=[stat_r])
        fw.op("act", lambda e: e.activation(out=stat[:, 1, :], in_=stat[:, 2, :], func=AF.Sqrt), reads=[stat_r], writes=[stat_r])
        fw.op("dve", lambda e: e.reciprocal(out=stat[:, 3, :], in_=stat[:, 1, :]), reads=[stat_r], writes=[stat_r])
        for nb in range(16):
            fw.op("dve", lambda e: e.tensor_tensor(out=z[:, nb, :], in0=z[:, nb, :], in1=stat[:, 0, :], op=ALU.subtract), reads=[z_r, stat_r], writes=[z_r])
            fw.op("dve", lambda e: e.tensor_tensor(out=z[:, nb, :], in0=z[:, nb, :], in1=stat[:, 3, :], op=ALU.mult), reads=[z_r, stat_r], writes=[z_r])
            fw.op("act", lambda e: e.activation(out=xres[:, nb, :], in_=z[:, nb, :], func=AF.Identity, scale=lng[:, idx, nb:nb + 1], bias=lnb[:, idx, nb:nb + 1]),
                  reads=[z_r, lng_r, lnb_r], writes=[xres_r])
        fw.op("pool", lambda e: e.tensor_copy(out=xb[:], in_=xres[:]), reads=[xres_r], writes=[xb_r])

    def resid(ps, ps_r, nb):
        fw.op("dve", lambda e: e.scalar_tensor_tensor(out=z[:, nb, :], in0=xres[:, nb, :], scalar=float(ALPHA), in1=ps[:, 0:TT], op0=ALU.mult, op1=ALU.add),
              reads=[xres_r, ps_r], writes=[z_r])

    for j in range(C.ntiles):
        t0 = j * TT
        fw.dma(out=xres[:], in_=x_in.rearrange("(kc p) t -> p kc t", p=128)[:, :, t0:t0 + TT], slot=xres_r)
        fw.dma(out=oa[:], in_=C.o_aT[:, :, t0:t0 + TT].rearrange("h p t -> p h t"), slot=oa_r)
        fw.dma(out=obm[:], in_=C.o_bT[:, :, t0:t0 + TT].rearrange("h p t -> p h t"), slot=obm_r)
        for fb in range(16):
            wa, wa_r = slab("w_o_dsa", fb, 8)
            psA, psA_r = mm(wa, wa_r, 8, oa, oa_r)
            wb_, wb_r = slab("w_o_moba", fb, 8)
            psB, psB_r = mm(wb_, wb_r, 8, obm, obm_r)
            gt, gt_r = gts.next()
            fw.dma(out=gt[:, 0, :], in_=C.g_aT[fb, :, t0:t0 + TT], slot=gt_r)
            fw.dma(out=gt[:, 1, :], in_=C.g_bT[fb, :, t0:t0 + TT], slot=gt_r)
            tt_, tt_r = t12.next()
            fw.op("dve", lambda e: e.tensor_tensor(out=tt_[:, 0, :], in0=psA[:, 0:TT], in1=gt[:, 0, :], op=ALU.mult), reads=[psA_r, gt_r], writes=[tt_r])
            fw.op("dve", lambda e: e.tensor_tensor(out=tt_[:, 1, :], in0=psB[:, 0:TT], in1=gt[:, 1, :], op=ALU.mult), reads=[psB_r, gt_r], writes=[tt_r])
            fw.op("dve", lambda e: e.tensor_tensor(out=mT[:, fb, :], in0=tt_[:, 0, :], in1=tt_[:, 1, :], op=ALU.add), reads=[tt_r], writes=[mT_r])
        for nb in range(16):
            wb, wb_r = slab("w_out", nb, 16)
            ps, ps_r = mm(wb, wb_r, 16, mT, mT_r)
            resid(ps, ps_r, nb)
        layer_norm(0)
        for hq in range(4):
            wb, wb_r = slab("w_q_mem", hq, 16)
            ps, ps_r = mm(wb, wb_r, 16, xb, xb_r)
            fw.op("act", lambda e: e.mul(out=qmT[:, hq, :], in_=ps[:, 0:TT], mul=float(QSCALE)), reads=[ps_r], writes=[qmT_r])
        for hq in range(4):
            for ms in range(2):
                ps, ps_r = psb.next()
                fw.op("pe", lambda e: e.matmul(ps[:, 0:TT], lhsT=kmemT[:, hq, ms * 128:(ms + 1) * 128], rhs=qmT[:, hq, :], start=True, stop=True), reads=[kmemT_r, qmT_r], writes=[ps_r])
                p, p_r = pts.next()
                fw.op("act", lambda e: e.activation(out=p[:], in_=ps[:, 0:TT], func=AF.Exp), reads=[ps_r], writes=[p_r])
                fw.op("pe", lambda e: e.matmul(psO[0][:, 0:TT], lhsT=vmem[:, ms, hq * 128:(hq + 1) * 128], rhs=p[:], start=(ms == 0), stop=(ms == 1)), reads=[vmem_r, p_r], writes=[psO[1]])
                fw.op("pe", lambda e: e.matmul(psD[0][:, 0:TT], lhsT=ones[:], rhs=p[:], start=(ms == 0), stop=(ms == 1)), reads=[ones_r, p_r], writes=[psD[1]])
            rc, rc_r = recs.next()
            fw.op("dve", lambda e: e.reciprocal(out=rc[:], in_=psD[0][:, 0:TT]), reads=[psD[1]], writes=[rc_r])
            fw.op("dve", lambda e: e.tensor_tensor(out=omT[:, hq, :], in0=psO[0][:, 0:TT], in1=rc[:], op=ALU.mult), reads=[psO[1], rc_r], writes=[omT_r])
        for nb in range(16):
            wb, wb_r = slab("w_o_mem", nb, 4)
            ps, ps_r = mm(wb, wb_r, 4, omT, omT_r)
            resid(ps, ps_r, nb)
        layer_norm(1)
        for i in range(44):
            wg, wg_r = slab("w_ffn_in", i, 16)
            psg, psg_r = mm(wg, wg_r, 16, xb, xb_r)
            wu, wu_r = slab("w_ffn_in", 44 + i, 16)
            psu, psu_r = mm(wu, wu_r, 16, xb, xb_r)
            sg, sg_r = sgs.next()
            fw.op("act", lambda e: e.activation(out=sg[:], in_=psg[:, 0:TT], func=AF.Silu), reads=[psg_r], writes=[sg_r])
            fw.op("dve", lambda e: e.tensor_tensor(out=aT[:, i, :], in0=psu[:, 0:TT], in1=sg[:], op=ALU.mult), reads=[psu_r, sg_r], writes=[aT_r])
        for nb in range(16):
            wb, wb_r = slab("w_ffn_out", nb, 44)
            ps, ps_r = mm(wb, wb_r, 44, aT, aT_r)
            resid(ps, ps_r, nb)
        layer_norm(2)
        fw.dma(out=x_out.rearrange("(kc p) t -> p kc t", p=128)[:, :, t0:t0 + TT], in_=xres[:], slot=xres_r)
    fw.end_phase()


from concourse.bass_utils import run_bass_kernel_spmd

ACT_SET = {
    "q_latT": ([2, 128, NH, TL], BF16), "q_idxT": ([8, 128, TL], BF16), "w_idx": ([TL, IH], F32),
    "k_idxT_own": ([ID_, TL], BF16), "c_kv_own": ([TL, KVL], BF16), "c_kvT_own": ([2, 128, TL], BF16),
    "q_mT": ([NH, 128, TL], BF16), "k_mT_own": ([NH, 128, TL], BF16), "k_meanT_own": ([128, NH, 8], F32),
    "v_m_own": ([TL, NH * HD], BF16), "g_aT": ([16, 128, TL], F32), "g_bT": ([16, 128, TL], F32),
}
KSIDE_OWN = ["k_idxT_own", "c_kv_own", "c_kvT_own", "k_mT_own", "k_meanT_own", "v_m_own"]
QSIDE = ["q_latT", "q_idxT", "w_idx", "q_mT", "g_aT", "g_bT"]
KSIDE_G = {"k_idxT_g": ([ID_, S], BF16), "c_kvT_g": ([2, 128, S], BF16), "c_kv_g": ([S, KVL], BF16),
           "k_mT_g": ([NH, 128, S], BF16), "v_m_g": ([S, NH * HD], BF16), "k_meanT_g": ([128, NH, 64], F32)}
PROJ_W = {"w_in": [1, D, INW], "w_uk": [1, NH, HD, KVL], "d_kvg": [1, 128, KVL], "d_kig": [1, 128, ID_], "d_kib": [1, 128, ID_], "d_bg": [1, 128, 2, 16]}
POST_W = {"w_uv": [1, NH, KVL, HD], "w_o_dsa": [1, 1024, D], "w_o_moba": [1, 1024, D], "w_out": [1, D, D], "w_q_mem": [1, D, 512], "w_kv_mem": [1, D, 1024],
          "w_o_mem": [1, 512, D], "w_ffn_in": [1, D, 2 * DFF], "w_ffn_out": [1, DFF, D], "d_lng": [1, 128, 3, 16], "d_lnb": [1, 128, 3, 16]}
CONSTS = {"d_ident": ([128, 128], BF16), "d_ones": ([128, 128], BF16), "d_onesf": ([128, 128], F32), "memT": ([D, 256], F32),
          "d_tloc": ([128, 16], F32), "d_iota": ([128, 2048], F32), "d_pow2": ([128, NIT], F32), "d_slopes": ([128, NH], F32), "d_kb": ([128, 128, NH], F32),
          "d_pastneg": ([128, 8, 64], F32), "d_pastind": ([128, 8, 64], F32), "d_alq": ([128, 2, NH], F32), "d_zc": ([128, 32, 128], BF16), "d_kbm": ([128, NH, 128], F32)}


def build_program(do_attn_post, do_proj, debug=False):
    nc = bass.Bass("TRN2", target_bir_lowering=False)
    ins, outs = [], []
    with contextlib.ExitStack() as es:
        fw = Fw(nc, es)
        C = Ctx(); C.cst = None; C.nl = 1
        C.nqb = 16; C.nslots = 8; C.nheads = NH; C.ntiles = TL // TT

        def dt(name, shape, dtype, kind):
            t = nc.dram_tensor(name, list(shape), dtype, kind=kind).ap()
            if kind == "ExternalInput":
                ins.append(name)
            elif kind == "ExternalOutput":
                outs.append(name)
            return t
        for n, (sh, d_) in CONSTS.items():
            setattr(C, n, dt(n, sh, d_, "ExternalInput"))
        xin = dt("xT_in", [D, TL], F32, "ExternalInput")
        if do_attn_post:
            for n, sh in POST_W.items():
                setattr(C, n, dt(n, sh, F32, "ExternalInput"))
            for n, (sh, d_) in KSIDE_G.items():
                setattr(C, n, dt(n, sh, d_, "ExternalInput"))
            for n in QSIDE:
                sh, d_ = ACT_SET[n]
                setattr(C, n, dt("in_" + n, sh, d_, "ExternalInput"))
            C.maskb = dt("maskb", [16, 128, S], BF16, "Internal")
            C.ddall = dt("ddall", [128, 16], F32, "Internal")
            C.o_aT = dt("o_aT", [NH, 128, TL], BF16, "ExternalOutput" if debug else "Internal")
            C.o_bT = dt("o_bT", [NH, 128, TL], BF16, "ExternalOutput" if debug else "Internal")
            xout = dt("xT_out", [D, TL], F32, "ExternalOutput")
            declare_prep(fw, C, dt)
            phase_prep(fw, C, 0)
            phase_dsa_select(fw, C, 0)
            phase_dsa_attn(fw, C, 0)
            phase_moba(fw, C, 0)
            phase_post(fw, C, 0, xin, xout)
        else:
            xout = xin
        if do_proj:
            for n, sh in PROJ_W.items():
                setattr(C, n, dt(n, sh, F32, "ExternalInput"))
            for n, (sh, d_) in ACT_SET.items():
                setattr(C, n, dt("out_" + n, sh, d_, "ExternalOutput"))
            C.xT_res = xout
            phase_proj(fw, C, 0)
    return nc, ins, outs


def core_consts(c):
    d = {}
    d.update(host_consts_attn(c))
    d.update(host_consts_moba(c))
    d["d_ident"] = np.eye(128, dtype=np.float32).astype(NPBF)
    d["d_onesf"] = np.full((128, 128), 1.0 / D, np.float32)
    return d


def gather_kside(outs_per_core):
    g = {n: np.zeros(sh, NPBF if d_ == BF16 else np.float32) for n, (sh, d_) in KSIDE_G.items()}
    for c in range(NCORE):
        tok = own_tokens(c)
        o = outs_per_core[c]
        g["k_idxT_g"][:, tok] = o["out_k_idxT_own"]
        g["c_kvT_g"][:, :, tok] = o["out_c_kvT_own"]
        g["c_kv_g"][tok, :] = o["out_c_kv_own"]
        g["k_mT_g"][:, :, tok] = o["out_k_mT_own"]
        g["v_m_g"][tok, :] = o["out_v_m_own"]
        for k in range(8):
            g["k_meanT_g"][:, :, 8 * k + c] = o["out_k_meanT_own"][:, :, k]
    return g


def kernel(**inp):
    x = np.asarray(inp["x"], np.float32)[0]
    consts = [core_consts(c) for c in range(NCORE)]
    memT = np.ascontiguousarray(np.asarray(inp["mem"], np.float32)[0].T)

    def proj_w(l):
        d = {"w_in": inp["w_in"][l:l + 1], "w_uk": inp["w_uk"][l:l + 1]}
        d.update({k: v for k, v in host_consts_proj(inp, [l]).items() if k != "d_ident"})
        return d

    def post_w(l):
        d = {n: inp[n][l:l + 1] for n in ["w_uv", "w_o_dsa", "w_o_moba", "w_out", "w_q_mem", "w_kv_mem", "w_o_mem", "w_ffn_in", "w_ffn_out"]}
        hp = host_consts_post(inp, [l])
        d["d_lng"] = hp["d_lng"]; d["d_lnb"] = hp["d_lnb"]
        return d

    def run(nc, ins, maps):
        maps = [{k: np.ascontiguousarray(m[k]) for k in ins} for m in maps]
        return run_bass_kernel_spmd(nc, maps, core_ids=list(range(NCORE))).results

    xT = [np.ascontiguousarray(x[own_tokens(c)].T) for c in range(NCORE)]
    ncP, insP, _ = build_program(False, True)
    maps = []
    for c in range(NCORE):
        m = dict(consts[c]); m["memT"] = memT; m["xT_in"] = xT[c]; m.update(proj_w(0)); maps.append(m)
    res = run(ncP, insP, maps)
    ncM = None
    for l in range(DEPTH):
        last = (l == DEPTH - 1)
        if last:
            ncX, insX, _ = build_program(True, False)
        else:
            if ncM is None:
                ncM = build_program(True, True)
            ncX, insX, _ = ncM
        kg = gather_kside(res)
        maps = []
        for c in range(NCORE):
            m = dict(consts[c]); m["memT"] = memT; m["xT_in"] = xT[c]
            m.update(kg)
            for n in QSIDE:
                m["in_" + n] = res[c]["out_" + n]
            m.update(post_w(l))
            if not last:
                m.update(proj_w(l + 1))
            maps.append(m)
        res = run(ncX, insX, maps)
        xT = [np.asarray(res[c]["xT_out"]) for c in range(NCORE)]
    out = np.zeros((1, S, D), np.float32)
    for c in range(NCORE):
        out[0, own_tokens(c), :] = xT[c].T
    return out
```
